# Optimizing a Trainium2 kernel written in Bass

```python
import jax, jax.numpy as jnp
from jax import lax
import numpy as np

D_MODEL = 2048
BATCH = 4
SEQ = 4096
DEPTH = 4

D_MIX = D_MODEL
RET_HEAD_DIM = D_MODEL // 16
RET_HEADS = 6
RET_WIDTH = RET_HEADS * RET_HEAD_DIM
RET_CHUNK = 128
ROPE_BASE = 10000.0
POOL_WINDOWS = (2, 4, 8, 16)
POOL_GROUPS = 4
POOL_GROUP_DIM = D_MODEL // 16
POOL_WIDTH = POOL_GROUPS * POOL_GROUP_DIM
GLA_HEADS = 4
GLA_WIDTH = D_MIX - RET_WIDTH - POOL_WIDTH
GLA_V_DIM = GLA_WIDTH // GLA_HEADS
GLA_K_DIM = GLA_V_DIM // 2
GLA_KEY_WIDTH = GLA_HEADS * GLA_K_DIM
GLA_GATE_RANK = 16
GLA_GATE_TAU = 16.0
GLA_CHUNK = 64
IN_WIDTH = 4 * RET_WIDTH + POOL_WIDTH + 2 * GLA_KEY_WIDTH + 2 * GLA_WIDTH + GLA_GATE_RANK
D_FF = 4 * D_MODEL
N_MOD = 6
EPS = 1e-6

kernel_name = "hybrid_retention_pool_gla_block"


def rms_norm(x, g):
    xf = x.astype(jnp.float32)
    y = xf * lax.rsqrt(jnp.mean(jnp.square(xf), axis=-1, keepdims=True) + EPS)
    return (y * g.astype(jnp.float32)).astype(x.dtype)


def head_layer_norm(o):
    mu = jnp.mean(o, axis=-1, keepdims=True)
    var = jnp.mean(jnp.square(o - mu), axis=-1, keepdims=True)
    return (o - mu) * lax.rsqrt(var + EPS)


def head_rms_norm(o):
    return o * lax.rsqrt(jnp.mean(jnp.square(o), axis=-1, keepdims=True) + EPS)


def rotary(x, cos, sin):
    x1, x2 = jnp.split(x, 2, axis=-1)
    c = cos[None, :, None, :]
    s = sin[None, :, None, :]
    return jnp.concatenate([x1 * c - x2 * s, x1 * s + x2 * c], axis=-1)


def retention(q, k, v):
    B, S, H, Dh = q.shape
    C = RET_CHUNK
    N = S // C
    log_g = jnp.log1p(-jnp.exp2(-5.0 - jnp.arange(H, dtype=jnp.float32)))
    q = q.reshape(B, N, C, H, Dh) * (Dh ** -0.5)
    k = k.reshape(B, N, C, H, Dh)
    v = v.reshape(B, N, C, H, Dh)
    idx = jnp.arange(C, dtype=jnp.float32)
    rel = idx[:, None] - idx[None, :]
    decay = jnp.where(rel[None] >= 0,
                      jnp.exp(jnp.maximum(rel, 0.0)[None] * log_g[:, None, None]), 0.0)
    scores = jnp.einsum('bnihd,bnjhd->bnhij', q, k) * decay
    inner = jnp.einsum('bnhij,bnjhe->bnihe', scores, v)
    zeta = jnp.exp((C - 1.0 - idx)[None, :] * log_g[:, None])
    u = jnp.einsum('bnjhd,hj,bnjhe->nbhde', k, zeta, v)
    g_chunk = jnp.exp(C * log_g)[None, :, None, None]

    def step(state, u_n):
        return g_chunk * state + u_n, state

    _, r_prev = lax.scan(step, jnp.zeros((B, H, Dh, Dh), jnp.float32), u)
    xi = jnp.exp((idx + 1.0)[None, :] * log_g[:, None])
    cross = jnp.einsum('bnihd,hi,nbhde->bnihe', q, xi, r_prev)
    return (inner + cross).reshape(B, S, H, Dh)


def gated_linear_attention(q, k, v, log_a):
    B, S, H, Dk = q.shape
    Dv = v.shape[-1]
    C = GLA_CHUNK
    N = S // C
    q = q.reshape(B, N, C, H, Dk) * (Dk ** -0.5)
    k = k.reshape(B, N, C, H, Dk)
    v = v.reshape(B, N, C, H, Dv)
    b = jnp.cumsum(log_a.reshape(B, N, C, H, Dk), axis=2)
    b_last = b[:, :, -1:]
    q_dec = q * jnp.exp(b)
    k_inv = k * jnp.exp(-b)
    causal = jnp.tril(jnp.ones((C, C), dtype=bool))
    scores = jnp.where(causal, jnp.einsum('bnihd,bnjhd->bnhij', q_dec, k_inv), 0.0)
    inner = jnp.einsum('bnhij,bnjhe->bnihe', scores, v)
    k_state = k * jnp.exp(b_last - b)
    u = jnp.einsum('bnjhd,bnjhe->nbhde', k_state, v)
    g_chunk = jnp.moveaxis(jnp.exp(b_last[:, :, 0]), 1, 0)[..., None]

    def step(state, inp):
        g_n, u_n = inp
        return g_n * state + u_n, state

    _, s_prev = lax.scan(step, jnp.zeros((B, H, Dk, Dv), jnp.float32), (g_chunk, u))
    cross = jnp.einsum('bnihd,nbhde->bnihe', q_dec, s_prev)
    return (inner + cross).reshape(B, S, H, Dv)


def multiscale_pool(u, w_pool, s_pool):
    B, S, _ = u.shape
    ug = u.reshape(B, S, POOL_GROUPS, POOL_GROUP_DIM)
    cs = jnp.concatenate([jnp.zeros((B, 1, POOL_GROUPS, POOL_GROUP_DIM), jnp.float32),
                          jnp.cumsum(ug, axis=1)], axis=1)
    t = jnp.arange(S)
    means = []
    for grp, w in enumerate(POOL_WINDOWS):
        lo = jnp.maximum(t + 1 - w, 0)
        win_sum = cs[:, 1:, grp] - jnp.take(cs[:, :, grp], lo, axis=1)
        cnt = (t + 1 - lo).astype(jnp.float32)
        means.append(win_sum / cnt[None, :, None])
    pooled = jnp.stack(means, axis=2) - ug
    y = jnp.einsum('bsgc,gcd->bsgd', pooled, w_pool.astype(jnp.float32))
    return y.reshape(B, S, POOL_WIDTH) * s_pool.astype(jnp.float32)


def hybrid_layer(x, c, w_ada, b_ada, g_mix, g_mlp, w_in, w_gate_up, b_gate,
                 w_pool, s_pool, w_out, w_up, w_down, cos, sin):
    B, S, _ = x.shape
    f32 = jnp.float32
    mod = jax.nn.silu(c) @ w_ada + b_ada
    sh1, sc1, gt1, sh2, sc2, gt2 = jnp.split(mod[:, None, :], N_MOD, axis=-1)

    h = rms_norm(x, g_mix) * (1.0 + sc1) + sh1
    z = h @ w_in
    sizes = [RET_WIDTH] * 4 + [POOL_WIDTH, GLA_KEY_WIDTH, GLA_KEY_WIDTH,
                               GLA_WIDTH, GLA_WIDTH, GLA_GATE_RANK]
    offsets = []
    acc = 0
    for sz in sizes[:-1]:
        acc += sz
        offsets.append(acc)
    (rq, rk, rv, rg, pu, gq, gk, gv, gr, gz) = jnp.split(z.astype(f32), offsets, axis=-1)

    rq = rotary(rq.reshape(B, S, RET_HEADS, RET_HEAD_DIM), cos, sin)
    rk = rotary(rk.reshape(B, S, RET_HEADS, RET_HEAD_DIM), cos, sin)
    ro = retention(rq, rk, rv.reshape(B, S, RET_HEADS, RET_HEAD_DIM))
    ret_out = head_layer_norm(ro).reshape(B, S, RET_WIDTH) * jax.nn.silu(rg)

    pool_out = multiscale_pool(pu, w_pool, s_pool)

    log_a = jax.nn.log_sigmoid(gz @ w_gate_up.astype(f32) + b_gate.astype(f32)) / GLA_GATE_TAU
    go = gated_linear_attention(gq.reshape(B, S, GLA_HEADS, GLA_K_DIM),
                                gk.reshape(B, S, GLA_HEADS, GLA_K_DIM),
                                gv.reshape(B, S, GLA_HEADS, GLA_V_DIM),
                                log_a.reshape(B, S, GLA_HEADS, GLA_K_DIM))
    gla_out = head_rms_norm(go).reshape(B, S, GLA_WIDTH) * jax.nn.silu(gr)

    mix = jnp.concatenate([ret_out, pool_out, gla_out], axis=-1).astype(x.dtype)
    x = x + gt1 * (mix @ w_out)

    h = rms_norm(x, g_mlp) * (1.0 + sc2) + sh2
    x = x + gt2 * (jnp.square(jax.nn.relu(h @ w_up)) @ w_down)
    return x


def setup_inputs(seed: int = 0) -> dict:
    key = jax.random.key(seed)
    ks = jax.random.split(key, 16)
    f32 = jnp.float32
    nrm = lambda k, shape, scale: jax.random.normal(k, shape, f32) * scale
    x = nrm(ks[0], (BATCH, SEQ, D_MODEL), 1.0)
    c = nrm(ks[1], (BATCH, D_MODEL), 1.0)
    w_ada = nrm(ks[2], (DEPTH, D_MODEL, N_MOD * D_MODEL), 0.5 * D_MODEL ** -0.5)
    b_ada = nrm(ks[3], (DEPTH, N_MOD * D_MODEL), 0.01)
    g_mix = 1.0 + nrm(ks[4], (DEPTH, D_MODEL), 0.05)
    g_mlp = 1.0 + nrm(ks[5], (DEPTH, D_MODEL), 0.05)
    w_in = nrm(ks[6], (DEPTH, D_MODEL, IN_WIDTH), D_MODEL ** -0.5)
    w_gate_up = nrm(ks[7], (DEPTH, GLA_GATE_RANK, GLA_KEY_WIDTH), GLA_GATE_RANK ** -0.5)
    b_gate = nrm(ks[8], (DEPTH, GLA_KEY_WIDTH), 0.1)
    w_pool = nrm(ks[9], (DEPTH, POOL_GROUPS, POOL_GROUP_DIM, POOL_GROUP_DIM), POOL_GROUP_DIM ** -0.5)
    s_pool = 1.0 + nrm(ks[10], (DEPTH, POOL_WIDTH), 0.1)
    w_out = nrm(ks[11], (DEPTH, D_MIX, D_MODEL), D_MIX ** -0.5)
    w_up = nrm(ks[12], (DEPTH, D_MODEL, D_FF), D_MODEL ** -0.5)
    w_down = nrm(ks[13], (DEPTH, D_FF, D_MODEL), D_FF ** -0.5)
    g_final = 1.0 + nrm(ks[14], (D_MODEL,), 0.05)
    return {"x": x, "c": c, "w_ada": w_ada, "b_ada": b_ada, "g_mix": g_mix,
            "g_mlp": g_mlp, "w_in": w_in, "w_gate_up": w_gate_up, "b_gate": b_gate,
            "w_pool": w_pool, "s_pool": s_pool, "w_out": w_out, "w_up": w_up,
            "w_down": w_down, "g_final": g_final}


def reference(x, c, w_ada, b_ada, g_mix, g_mlp, w_in, w_gate_up, b_gate,
              w_pool, s_pool, w_out, w_up, w_down, g_final):
    S = x.shape[1]
    pos = jnp.arange(S, dtype=jnp.float32)
    inv_freq = ROPE_BASE ** (-jnp.arange(0, RET_HEAD_DIM, 2, dtype=jnp.float32) / RET_HEAD_DIM)
    ang = pos[:, None] * inv_freq[None, :]
    cos, sin = jnp.cos(ang), jnp.sin(ang)
    for l in range(DEPTH):
        x = hybrid_layer(x, c, w_ada[l], b_ada[l], g_mix[l], g_mlp[l], w_in[l],
                         w_gate_up[l], b_gate[l], w_pool[l], s_pool[l], w_out[l],
                         w_up[l], w_down[l], cos, sin)
    return rms_norm(x, g_final)
```

```python
import math
import numpy as np
import concourse.bass as bass
import concourse.mybir as mybir
from concourse.bass_utils import run_bass_kernel_spmd

F32 = mybir.dt.float32
BF16 = mybir.dt.bfloat16
AF = mybir.ActivationFunctionType
ALU = mybir.AluOpType

D = 2048
KC = 16
MT = 1024
TT = 512
NTT = MT // TT
CH = 128
NCH = MT // CH
RH = 6
GH = 4
DK = 96
DV = 192
NKO = 18
DFF = 8192
EPS = 1e-6
IN_WIDTH = 5904
SLOTW = 256


class Eng:
    def __init__(self, fw, name, eng):
        self.fw = fw
        self.name = name
        self.eng = eng
        self.sem = fw.nc.alloc_semaphore("sem_" + name)
        self.count = 0
        self.waited = {}

    def wait(self, toks):
        best = {}
        for t in toks:
            if t is None:
                continue
            key, sem, val = t
            if val <= self.waited.get(key, 0):
                continue
            if key not in best or best[key][1] < val:
                best[key] = (sem, val)
        for key, (sem, val) in best.items():
            self.eng.wait_ge(sem, val)
            self.waited[key] = val

    def mark(self, inst):
        inst.then_inc(self.sem, 1)
        self.count += 1
        return (self.name, self.sem, self.count)

    def now(self):
        if self.count == 0:
            return None
        return (self.name, self.sem, self.count)


class Buf:
    def __init__(self, name="", seed=None):
        self.name = name
        self.w = None
        self.r = list(seed) if seed else []

    def rdeps(self):
        return [self.w]

    def wdeps(self):
        return [self.w] + self.r

    def wrote(self, tok):
        self.w = tok
        self.r = []

    def read(self, tok):
        self.r.append(tok)
        if len(self.r) > 16:
            best = {}
            for t in self.r:
                if t is None:
                    continue
                if t[0] not in best or best[t[0]][2] < t[2]:
                    best[t[0]] = t
            self.r = list(best.values())

    def all_tokens(self):
        return [t for t in ([self.w] + self.r) if t is not None]


class PBuf(Buf):
    def rdeps(self):
        return self.wdeps()

    def read(self, tok):
        self.wrote(tok)


class FW:
    def __init__(self):
        self.nc = bass.Bass("TRN2", target_bir_lowering=False)
        nc = self.nc
        self.pe = Eng(self, "pe", nc.tensor)
        self.act = Eng(self, "act", nc.scalar)
        self.dve = Eng(self, "dve", nc.vector)
        self.pool = Eng(self, "pool", nc.gpsimd)
        self.sp = Eng(self, "sp", nc.sync)
        self.engs = [self.pe, self.act, self.dve, self.pool, self.sp]
        self.dsems = []

    def sb(self, name, shape, dt):
        return self.nc.alloc_sbuf_tensor(name, list(shape), dt)

    def op(self, E, fn, reads=(), writes=(), mark=True):
        deps = []
        for b in reads:
            deps += b.rdeps()
        for b in writes:
            deps += b.wdeps()
        E.wait(deps)
        inst = fn()
        if not mark:
            return None
        tok = E.mark(inst)
        for b in reads:
            b.read(tok)
        for b in writes:
            b.wrote(tok)
        return tok

    def dsem(self, name):
        s = [self.nc.alloc_semaphore(name), 0, "d_" + name]
        self.dsems.append(s)
        return s

    def dma(self, E, out_ap, in_ap, reads=(), writes=(), sem=None):
        deps = []
        for b in reads:
            deps += b.rdeps()
        for b in writes:
            deps += b.wdeps()
        E.wait(deps)
        inst = E.eng.dma_start(out=out_ap, in_=in_ap)
        sem[1] += 16
        inst.then_inc(sem[0], 16)
        tok = (sem[2], sem[0], sem[1])
        for b in reads:
            b.read(tok)
        for b in writes:
            b.wrote(tok)
        return tok

    def fence_tokens(self):
        toks = [e.now() for e in self.engs]
        toks += [(s[2], s[0], s[1]) for s in self.dsems if s[1] > 0]
        return [t for t in toks if t is not None]


class Rot:
    def __init__(self, fw, name, n, shape, dt, space="sb"):
        self.items = []
        for i in range(n):
            t = fw.sb(f"{name}{i}", shape, dt)
            self.items.append((t, Buf(f"{name}{i}")))
        self.i = 0

    def next(self):
        it = self.items[self.i % len(self.items)]
        self.i += 1
        return it


RET_W = 768
OFF_RQ, OFF_RK, OFF_RV, OFF_RG = 0, 768, 1536, 2304
OFF_PU = 3072
OFF_GQ, OFF_GK, OFF_GV, OFF_GR, OFF_GZ = 3584, 3968, 4352, 5120, 5888


def in_col_perm():
    cols = []
    for h in range(RH):
        cols += list(range(OFF_RQ + h * 128, OFF_RQ + (h + 1) * 128))
        cols += list(range(OFF_RK + h * 128, OFF_RK + (h + 1) * 128))
    for h in range(RH):
        cols += list(range(OFF_RG + h * 128, OFF_RG + (h + 1) * 128))
        cols += list(range(OFF_RV + h * 128, OFF_RV + (h + 1) * 128))
    cols += list(range(OFF_PU, OFF_PU + 512))
    for h in range(GH):
        cols += list(range(OFF_GQ + h * DK, OFF_GQ + (h + 1) * DK))
        cols += list(range(OFF_GK + h * DK, OFF_GK + (h + 1) * DK))
        cols += list(range(OFF_GV + h * DV, OFF_GV + (h + 1) * DV))
        cols += list(range(OFF_GR + h * DV, OFF_GR + (h + 1) * DV))
    cols += list(range(OFF_GZ, OFF_GZ + 16))
    assert len(cols) == IN_WIDTH and len(set(cols)) == IN_WIDTH
    return np.array(cols)


PC_RQK = lambda h: h * 256
PC_RGV = lambda h: 1536 + h * 256
PC_POOL = lambda s: 3072 + s * 256
PC_GQK = lambda h: 3584 + h * 576
PC_GV = lambda h: 3584 + h * 576 + 192
PC_GR = lambda h: 3584 + h * 576 + 384
PC_GZ = 5888


def out_row_map():
    rows = []
    for h in range(RH):
        rows += list(range(h * 128, (h + 1) * 128))
    for g in range(4):
        rows += list(range(768 + g * 128, 768 + (g + 1) * 128))
    for h in range(GH):
        base = 1280 + h * DV
        rows += list(range(base, base + 128))
        rows += list(range(base + 128, base + 192)) + [-1] * 64
    assert len(rows) == NKO * 128
    return np.array(rows)


def host_tables(pos0, ntok, seq_start):
    t = {}
    inv_freq = (10000.0 ** (-np.arange(0, 128, 2, dtype=np.float32) / np.float32(128))).astype(np.float32)
    pos = (pos0 + np.arange(ntok)).astype(np.float32)
    ang = (pos[None, :] * inv_freq[:, None]).astype(np.float32)
    cos = np.cos(ang.astype(np.float64)).astype(np.float32)
    sin = np.sin(ang.astype(np.float64)).astype(np.float32)
    t["cosT"] = np.ascontiguousarray(np.concatenate([cos, cos], 0))
    t["sinT"] = np.ascontiguousarray(np.concatenate([-sin, sin], 0))
    hh = np.arange(RH, dtype=np.float64)
    log_g = np.log1p(-np.exp2(-5.0 - hh))
    i = np.arange(128, dtype=np.float64)
    xi = np.exp((i[None, :] + 1.0) * log_g[:, None]) * (128.0 ** -0.5)
    t["xi"] = np.ascontiguousarray(np.broadcast_to(xi[None], (128, RH, 128))).astype(np.float32)
    jj = i[:, None, None]
    ii = i[None, None, :]
    d2 = np.where(jj <= ii, np.exp(-(jj + 1.0) * log_g[None, :, None]), 0.0)
    t["d2"] = np.ascontiguousarray(d2).astype(np.float32)
    zeta = np.exp((127.0 - i)[:, None] * log_g[None, :])
    t["zeta"] = np.ascontiguousarray(zeta).astype(np.float32)
    t["causal"] = (i[:, None] <= i[None, :]).astype(np.float32)
    t["ident"] = np.eye(128, dtype=np.float32)
    sw = np.zeros((128, 128), np.float32)
    for m in range(128):
        sw[(m + 64) % 128, m] = 1.0
    t["pswap"] = sw
    sm = np.ones((128, MT), np.float32)
    sm[:, ::CH] = 0.0
    t["scanmask"] = sm
    ic = np.zeros((128, 4, 16), np.float32)
    for g, w in enumerate((2, 4, 8, 16)):
        for tt in range(16):
            ic[:, g, tt] = 1.0 / (min(tt + 1, w) if seq_start else w)
    t["invcnt"] = ic
    return t


G128 = [float(np.exp(128.0 * np.log1p(-np.exp2(-5.0 - h)))) for h in range(RH)]


def build_program(NL, NMT, final_norm=True, stop=None):
    fw = FW()
    nc = fw.nc
    pe, act, dve, pool, sp = fw.pe, fw.act, fw.dve, fw.pool, fw.sp
    NTOK = NMT * MT

    def din(name, shape, dt=F32):
        return nc.dram_tensor(name, list(shape), dt, kind="ExternalInput").ap()

    xT_d = din("xT", [D, NTOK])
    c_d = din("c_fm", [128, KC])
    wada_d = din("w_ada", [NL, D, 6 * D])
    bada_d = din("b_ada_fm", [NL, 128, 96])
    gmix_d = din("g_mix_fm", [NL, 128, KC])
    gmlp_d = din("g_mlp_fm", [NL, 128, KC])
    win_d = din("w_in_p", [NL, D, IN_WIDTH])
    wg_d = din("w_gate_up", [NL, 16, 384])
    bg_d = din("b_gate_fm", [NL, 128, GH])
    wpool_d = din("w_pool", [NL, 4, 128, 128])
    spool_d = din("s_pool_fm", [NL, 128, 4])
    wout_d = din("w_out_p", [NL, NKO * 128, D])
    wup_d = din("w_up", [NL, D, DFF])
    wdn_d = din("w_down", [NL, DFF, D])
    gfin_d = din("g_final_fm", [128, KC])
    cos_d = din("cosT", [128, NTOK])
    sin_d = din("sinT", [128, NTOK])
    xi_d = din("xi", [128, RH, 128])
    d2_d = din("d2", [128, RH, 128])
    zeta_d = din("zeta", [128, RH])
    causal_d = din("causal", [128, 128])
    ident_d = din("ident", [128, 128])
    pswap_d = din("pswap", [128, 128])
    smask_d = din("scanmask", [128, MT])
    invcnt_d = din("invcnt", [128, 4, 16])
    yT_d = nc.dram_tensor("yT", [D, NTOK], F32, kind="ExternalOutput").ap()
    xs_d = nc.dram_tensor("xs", [D, NTOK], F32).ap()

    arena = fw.sb("arena", [128, KC * MT], F32)
    x_mt = arena[:, :].rearrange("p (k t) -> p k t", k=KC)
    B_x = Buf("x_mt")
    hT = fw.sb("hT", [128, KC, MT], BF16)
    B_h = [Buf(f"hT{t}") for t in range(NTT)]
    mixb = fw.sb("mix", [128, NKO, MT], BF16)
    B_mix = [Buf(f"mix{k}") for k in range(NKO)]
    NSLOT = 3
    wslot = [fw.sb(f"wslot{i}", [128, NKO, SLOTW], BF16) for i in range(NSLOT)]
    B_slot = [Buf(f"wslot{i}") for i in range(NSLOT)]
    S_slot = [fw.dsem(f"wsl{i}") for i in range(NSLOT)]
    slot_i = [0]

    cos_sb = fw.sb("cos_sb", [128, MT], F32)
    sin_sb = fw.sb("sin_sb", [128, MT], F32)
    B_cs = Buf("cossin")
    xi_sb = fw.sb("xi_sb", [128, RH, 128], F32)
    d2_sb = fw.sb("d2_sb", [128, RH, 128], F32)
    zeta_sb = fw.sb("zeta_sb", [128, RH], F32)
    causal_sb = fw.sb("causal_sb", [128, 128], F32)
    ident_sb = fw.sb("ident_sb", [128, 128], BF16)
    pswap_sb = fw.sb("pswap_sb", [128, 128], BF16)
    ones_sb = fw.sb("ones_sb", [128, 128], BF16)
    smask_sb = fw.sb("smask_sb", [128, MT], F32)
    invcnt_sb = fw.sb("invcnt_sb", [128, 4, 16], F32)
    eps_sb = fw.sb("eps_sb", [128, 1], F32)
    B_const = Buf("const")

    c_sb = fw.sb("c_sb", [128, KC], F32)
    sc_bf = fw.sb("sc_bf", [128, KC], BF16)
    gfin_sb = fw.sb("gfin_sb", [128, KC], F32)
    B_c = Buf("c")

    bada_sb = fw.sb("bada_sb", [128, 96], F32)
    gmix_sb = fw.sb("gmix_sb", [128, KC], F32)
    gmlp_sb = fw.sb("gmlp_sb", [128, KC], F32)
    wg_sb = fw.sb("wg_sb", [16, 384], BF16)
    nbg_sb = fw.sb("nbg_sb", [128, GH], F32)
    bg_sb = fw.sb("bg_sb", [128, GH], F32)
    wpool_sb = fw.sb("wpool_sb", [128, 4, 128], BF16)
    spool_sb = fw.sb("spool_sb", [128, 4], F32)
    wgz_sb = fw.sb("wgz_sb", [128, KC, 16], BF16)
    mod_sb = fw.sb("mod_sb", [128, 96], F32)
    modn_sb = fw.sb("modn_sb", [128, 96], F32)
    B_modn = Buf("modn")
    gmod1 = fw.sb("gmod1", [128, KC], F32)
    gmod2 = fw.sb("gmod2", [128, KC], F32)
    B_lp = Buf("layerparams")
    B_mod = Buf("mod")

    Sret = fw.sb("Sret", [128, RH, 128], F32)
    Sret_bf = fw.sb("Sret_bf", [128, RH, 128], BF16)
    Sgla = fw.sb("Sgla", [128, GH, DV], F32)
    Sgla_bf = fw.sb("Sgla_bf", [128, GH, DV], BF16)
    halo = fw.sb("halo", [128, 4, 16], F32)
    B_Sret = [Buf(f"Sret{h}") for h in range(RH)]
    B_Sretbf = [Buf(f"Sretbf{h}") for h in range(RH)]
    B_Sgla = [Buf(f"Sgla{h}") for h in range(GH)]
    B_Sglabf = [Buf(f"Sglabf{h}") for h in range(GH)]
    B_halo = [Buf(f"halo{g}") for g in range(4)]

    tmpf = Rot(fw, "tmpf", 3, [128, TT], F32)
    tmpb = Rot(fw, "tmpb", 2, [128, TT], BF16)
    rstd_t = Rot(fw, "rstd", 2, [128, TT], F32)

    pbank = [nc.alloc_psum_tensor(f"pb{i}", [128, 512], F32) for i in range(8)]
    B_pb = [PBuf(f"pb{i}") for i in range(8)]
    PZ = [0, 1]
    PROT, PSC, PROA, PROB, PSTAT, PU = 2, 3, 4, 5, 6, 7
    pz_i = [0]

    def next_pz():
        i = PZ[pz_i[0] % len(PZ)]
        pz_i[0] += 1
        return pbank[i], B_pb[i]

    B_sc = [B_pb[PSC]] * 4
    sc_i = [0]
    B_u = [B_pb[PU]] * 2
    u_i = [0]
    tr_ps_ret = pbank[PROB][:, 0:128].bitcast(BF16)
    tr_ps_gla = pbank[PROT][:, 0:128].bitcast(BF16)
    tr_i = [0]

    ld_sem = fw.dsem("ld")
    xs_sem = fw.dsem("xs")
    B_xs = [Buf(f"xs{m}") for m in range(NMT)]

    def mm_group(out_ap, pairs, reads, writes, flags=None):
        n = len(pairs)
        for i, (l, r) in enumerate(pairs):
            last = i == n - 1
            st = (i == 0) if flags is None else flags[0]
            sp_ = last if flags is None else (flags[1] and last)
            fw.op(pe, lambda l=l, r=r, st=st, sp_=sp_: nc.tensor.matmul(out_ap, l, r, start=st, stop=sp_),
                  reads=reads, writes=writes, mark=last)

    def load_slot(src_ap, nk, width):
        i = slot_i[0] % NSLOT
        slot_i[0] += 1
        fw.dma(pool, wslot[i][:, 0:nk, 0:width], src_ap.rearrange("(k p) c -> p k c", p=128),
               writes=[B_slot[i]], sem=S_slot[i])
        return wslot[i], B_slot[i]

    lp_sem = fw.dsem("lp")
    cs_sem = fw.dsem("cs")

    def ld(dst, src, B, eng=None, sem=None):
        fw.dma(eng or sp, dst, src, writes=[B], sem=sem or ld_sem)

    for dst, src in ((xi_sb[:], xi_d[:, :, :]), (d2_sb[:], d2_d[:, :, :]), (zeta_sb[:], zeta_d[:, :]),
                     (causal_sb[:], causal_d[:, :]), (smask_sb[:], smask_d[:, :]),
                     (invcnt_sb[:], invcnt_d[:, :, :]), (c_sb[:], c_d[:, :]), (gfin_sb[:], gfin_d[:, :])):
        ld(dst, src, B_const)
    ld(ident_sb[:], ident_d[:, :], B_const, eng=pool)
    ld(pswap_sb[:], pswap_d[:, :], B_const, eng=pool)
    fw.op(dve, lambda: nc.vector.memset(ones_sb[:], 1.0), writes=[B_const])
    fw.op(dve, lambda: nc.vector.memset(eps_sb[:], EPS), writes=[B_const])
    const_tok = (ld_sem[2], ld_sem[0], ld_sem[1])
    for e in (pe, act, dve):
        e.wait([const_tok, dve.now()])

    class _Ready(Buf):
        def rdeps(self):
            return []

        def read(self, tok):
            pass
    CONST = _Ready("CONST")
    fw.op(act, lambda: nc.scalar.activation(out=sc_bf[:], in_=c_sb[:], func=AF.Silu), reads=[CONST], writes=[B_c])
    fw.op(dve, lambda: nc.vector.memset(mixb[:], 0.0), writes=B_mix)

    N_ADA_SLOTS = 6 * D // SLOTW

    def ada_slot(l, s):
        pm, Bpm = pbank[PROT], B_pb[PROT]
        wsl, Bs = load_slot(wada_d[l, :, s * SLOTW:(s + 1) * SLOTW], KC, SLOTW)
        nb = SLOTW // 128
        for cb in range(nb):
            mm_group(pm[:, cb:cb + 1],
                     [(wsl[:, kc, cb * 128:(cb + 1) * 128], sc_bf[:, kc:kc + 1]) for kc in range(KC)],
                     reads=[Bs, B_c], writes=[Bpm])
        fw.op(act, lambda: nc.scalar.copy(out=modn_sb[:, s * nb:(s + 1) * nb], in_=pm[:, 0:nb]),
              reads=[Bpm], writes=[B_modn])

    def layer_prologue(l):
        ld(bada_sb[:], bada_d[l, :, :], B_lp, sem=lp_sem)
        ld(gmix_sb[:], gmix_d[l, :, :], B_lp, sem=lp_sem)
        ld(gmlp_sb[:], gmlp_d[l, :, :], B_lp, sem=lp_sem)
        ld(bg_sb[:], bg_d[l, :, :], B_lp, sem=lp_sem)
        ld(spool_sb[:], spool_d[l, :, :], B_lp, sem=lp_sem)
        ld(wg_sb[:], wg_d[l, :, :], B_lp, eng=pool, sem=lp_sem)
        ld(wpool_sb[:], wpool_d[l, :, :, :].rearrange("g c d -> c g d"), B_lp, eng=pool, sem=lp_sem)
        ld(wgz_sb[:], win_d[l, :, PC_GZ:PC_GZ + 16].rearrange("(k p) c -> p k c", p=128), B_lp, eng=pool, sem=lp_sem)
        B_lp.w = (lp_sem[2], lp_sem[0], lp_sem[1])
        fw.op(dve, lambda: nc.vector.tensor_scalar(nbg_sb[:], bg_sb[:], -1.0, None, ALU.mult), reads=[B_lp], writes=[B_lp])
        if l == 0:
            for s in range(N_ADA_SLOTS):
                ada_slot(0, s)
        fw.op(dve, lambda: nc.vector.tensor_tensor(mod_sb[:], modn_sb[:], bada_sb[:], ALU.add),
              reads=[B_modn, B_lp], writes=[B_mod])
        fw.op(dve, lambda: nc.vector.scalar_tensor_tensor(gmod1[:], mod_sb[:, 16:32], 1.0, gmix_sb[:], ALU.add, ALU.mult),
              reads=[B_mod, B_lp], writes=[B_mod])
        fw.op(dve, lambda: nc.vector.scalar_tensor_tensor(gmod2[:], mod_sb[:, 64:80], 1.0, gmlp_sb[:], ALU.add, ALU.mult),
              reads=[B_mod, B_lp], writes=[B_mod])
        for h in range(RH):
            fw.op(dve, lambda h=h: nc.vector.memset(Sret[:, h, :], 0.0), writes=[B_Sret[h]])
            fw.op(dve, lambda h=h: nc.vector.memset(Sret_bf[:, h, :], 0.0), writes=[B_Sretbf[h]])
        for h in range(GH):
            fw.op(dve, lambda h=h: nc.vector.memset(Sgla[:, h, :], 0.0), writes=[B_Sgla[h]])
            fw.op(dve, lambda h=h: nc.vector.memset(Sgla_bf[:, h, :], 0.0), writes=[B_Sglabf[h]])
        for g in range(4):
            fw.op(dve, lambda g=g: nc.vector.memset(halo[:, g, :], 0.0), writes=[B_halo[g]])

    SH1, SC1, GT1, SH2, SC2, GT2 = 0, 16, 32, 48, 64, 80

    def rms_to_h(gmod, shift_off):
        for tt in range(NTT):
            ts = slice(tt * TT, (tt + 1) * TT)
            ss, Bss = pbank[PSTAT], B_pb[PSTAT]
            for kc in range(KC):
                sq, Bsq = tmpb.next()
                fw.op(act, lambda kc=kc, sq=sq: nc.scalar.activation(out=sq[:], in_=x_mt[:, kc, ts], func=AF.Square),
                      reads=[B_x], writes=[Bsq])
                fw.op(pe, lambda kc=kc, sq=sq: nc.tensor.matmul(ss[:, :], ones_sb[:, :], sq[:], start=(kc == 0), stop=(kc == KC - 1)),
                      reads=[Bsq, CONST], writes=[Bss], mark=True)
            rs, Brs = rstd_t.next()
            fw.op(act, lambda: nc.scalar.activation(out=rs[:], in_=ss[:, :], func=AF.Ln, bias=eps_sb[:, 0:1], scale=1.0 / D),
                  reads=[Bss, CONST], writes=[Brs])
            fw.op(act, lambda: nc.scalar.activation(out=rs[:], in_=rs[:], func=AF.Exp, scale=-0.5),
                  reads=[Brs], writes=[Brs])
            for kc in range(KC):
                t1, Bt1 = tmpf.next()
                fw.op(dve, lambda kc=kc, t1=t1: nc.vector.scalar_tensor_tensor(t1[:], x_mt[:, kc, ts], gmod[:, kc:kc + 1], rs[:], ALU.mult, ALU.mult),
                      reads=[B_x, B_mod, Brs], writes=[Bt1])
                if shift_off is None:
                    pass
                else:
                    fw.op(act, lambda kc=kc, t1=t1: nc.scalar.activation(out=hT[:, kc, ts], in_=t1[:], func=AF.Identity,
                                                                         bias=mod_sb[:, shift_off + kc:shift_off + kc + 1], scale=1.0),
                          reads=[Bt1, B_mod], writes=[B_h[tt]])

    def proj_F(wsl, Bs, c0, M, tt, nk=KC, src=None, Bsrc=None):
        src = hT if src is None else src
        Bsrc = B_h[tt] if Bsrc is None else Bsrc
        pz, Bpz = next_pz()
        ts = slice(tt * TT, (tt + 1) * TT)
        mm_group(pz[0:M, :], [(wsl[:, kc, c0:c0 + M], src[:, kc, ts]) for kc in range(nk)],
                 reads=[Bs, Bsrc], writes=[Bpz])
        return pz, Bpz

    def proj_T(wsl, Bs, c0, N, ch_list, out_view_fn, Bout, evac_eng="act"):
        per_bank = max(1, 512 // N)
        for g0 in range(0, len(ch_list), per_bank):
            grp = ch_list[g0:g0 + per_bank]
            pz, Bpz = next_pz()
            for gi, ch in enumerate(grp):
                mm_group(pz[:, gi * N:(gi + 1) * N],
                         [(hT[:, kc, ch * CH:(ch + 1) * CH], wsl[:, kc, c0:c0 + N]) for kc in range(KC)],
                         reads=[Bs, B_h[(ch * CH) // TT]], writes=[Bpz])
            dst = out_view_fn(grp)
            srcv = pz[:, 0:len(grp) * N].rearrange("p (c n) -> p c n", n=N)
            fw.op(act, lambda dst=dst, srcv=srcv: nc.scalar.copy(out=dst, in_=srcv), reads=[Bpz], writes=[Bout])
            yield

    arena_bf = arena[:, :].bitcast(BF16)
    SET_BF = 8192

    def set_view(s, off, n):
        return arena_bf[:, s * SET_BF + off: s * SET_BF + off + n]

    def f32_view(off, n):
        return arena[:, 8192 + off: 8192 + off + n]

    def run_mt(l, m):
        tok0 = m * MT
        last_layer = (l == NL - 1)
        if stop == "prologue":
            return
        src = xT_d if l == 0 else xs_d
        fence = fw.fence_tokens()
        B_x.r += fence
        for half in range(2):
            ks = slice(half * 8, (half + 1) * 8)
            fw.dma(sp, x_mt[:, ks, :], src[:, tok0:tok0 + MT].rearrange("(k p) t -> p k t", p=128)[:, ks, :],
                   reads=[B_xs[m]], writes=[B_x], sem=xs_sem)
        B_x.w = (xs_sem[2], xs_sem[0], xs_sem[1])
        fw.dma(sp, cos_sb[:], cos_d[:, tok0:tok0 + MT], writes=[B_cs], sem=cs_sem)
        fw.dma(sp, sin_sb[:], sin_d[:, tok0:tok0 + MT], writes=[B_cs], sem=cs_sem)
        B_cs.w = (cs_sem[2], cs_sem[0], cs_sem[1])

        rms_to_h(gmod1, SH1)

        if stop == "norm1":
            return
        seed = B_x.all_tokens()
        set_prev = [list(seed), list(seed)]

        units = []
        kz_r = [(f32_view(0, 64).bitcast(BF16), Buf("kz0", seed)), (f32_view(64, 64).bitcast(BF16), Buf("kz1", seed))]
        sT_r = [(f32_view(128, 64).bitcast(BF16), Buf("sT0", seed)), (f32_view(192, 64).bitcast(BF16), Buf("sT1", seed))]
        kst_r = [(f32_view(256, 64).bitcast(BF16), Buf("kst0", seed)), (f32_view(320, 64).bitcast(BF16), Buf("kst1", seed))]
        gsT_r = [(f32_view(384, 64).bitcast(BF16), Buf("gsT0", seed)), (f32_view(448, 64).bitcast(BF16), Buf("gsT1", seed))]

        def make_ret(h, s):
            st = {}

            def A():
                sd = set_prev[s]
                qp = set_view(s, 0, MT); kp = set_view(s, MT, MT); sg = set_view(s, 2 * MT, MT)
                vtm = set_view(s, 3 * MT, MT).rearrange("p (c e) -> p c e", e=128)
                Bq, Bk, Bg, Bv = Buf("qp", sd), Buf("kp", sd), Buf("sg", sd), Buf("vtm", sd)
                st.update(qp=qp, kp=kp, sg=sg, vtm=vtm, Bq=Bq, Bk=Bk, Bg=Bg, Bv=Bv)
                wsl, Bs = load_slot(win_d[l, :, PC_RQK(h):PC_RQK(h) + 256], KC, 256)
                for tt in range(NTT):
                    ts = slice(tt * TT, (tt + 1) * TT)
                    for which in range(2):
                        pz, Bpz = proj_F(wsl, Bs, which * 128, 128, tt)
                        raw, Braw = tmpb.next()
                        fw.op(act, lambda raw=raw, pz=pz: nc.scalar.copy(out=raw[:], in_=pz[:, :]), reads=[Bpz], writes=[Braw])
                        pr, Bpr = pbank[PROT], B_pb[PROT]
                        fw.op(pe, lambda raw=raw, pr=pr: nc.tensor.matmul(pr[:, :], pswap_sb[:, :], raw[:], start=True, stop=True),
                              reads=[Braw, CONST], writes=[Bpr])
                        t1, Bt1 = tmpf.next()
                        t2, Bt2 = tmpf.next()
                        fw.op(dve, lambda t1=t1, pz=pz: nc.vector.tensor_tensor(t1[:], pz[:, :], cos_sb[:, ts], ALU.mult),
                              reads=[Bpz, B_cs], writes=[Bt1])
                        fw.op(dve, lambda t2=t2, pr=pr: nc.vector.tensor_tensor(t2[:], pr[:, :], sin_sb[:, ts], ALU.mult),
                              reads=[Bpr, B_cs], writes=[Bt2])
                        if which == 0:
                            fw.op(dve, lambda t1=t1, t2=t2: nc.vector.tensor_tensor(t1[:], t1[:], t2[:], ALU.add),
                                  reads=[Bt2], writes=[Bt1])
                            fw.op(dve, lambda t1=t1: nc.vector.tensor_tensor(
                                qp[:, ts].rearrange("p (c i) -> p c i", i=128),
                                t1[:].rearrange("p (c i) -> p c i", i=128),
                                xi_sb[:, h, :].unsqueeze(1).to_broadcast([128, TT // 128, 128]), ALU.mult),
                                reads=[Bt1, CONST], writes=[Bq])
                        else:
                            fw.op(dve, lambda t1=t1, t2=t2: nc.vector.tensor_tensor(kp[:, ts], t1[:], t2[:], ALU.add),
                                  reads=[Bt1, Bt2], writes=[Bk])
                        yield
                wsl, Bs = load_slot(win_d[l, :, PC_RGV(h):PC_RGV(h) + 256], KC, 256)
                for tt in range(NTT):
                    ts = slice(tt * TT, (tt + 1) * TT)
                    pz, Bpz = proj_F(wsl, Bs, 0, 128, tt)
                    fw.op(act, lambda pz=pz: nc.scalar.activation(out=sg[:, ts], in_=pz[:, :], func=AF.Silu), reads=[Bpz], writes=[Bg])
                    yield
                yield from proj_T(wsl, Bs, 128, 128, list(range(NCH)), lambda grp: vtm[:, grp[0]:grp[0] + len(grp), :], Bv)

            def B():
                qp, kp, sg, vtm = st["qp"], st["kp"], st["sg"], st["vtm"]
                Bq, Bk, Bg, Bv = st["Bq"], st["Bk"], st["Bg"], st["Bv"]
                pre = {}

                def emit_i(ch):
                    cs_ = slice(ch * CH, (ch + 1) * CH)
                    ti = tr_i[0] % 2; tr_i[0] += 1
                    trp = tr_ps_ret[:, ti * 128:(ti + 1) * 128]
                    Btr = B_pb[PROB]
                    fw.op(pe, lambda: nc.tensor.transpose(trp, kp[:, cs_], ident_sb[:, :]),
                          reads=[Bk, CONST], writes=[Btr])
                    kz, Bkz = kz_r[ch % 2]
                    fw.op(act, lambda: nc.scalar.mul(kz, trp, zeta_sb[:, h:h + 1]),
                          reads=[Btr, CONST], writes=[Bkz])
                    si = sc_i[0] % 4; sc_i[0] += 1
                    scp = pbank[PSC][:, si * 128:(si + 1) * 128]
                    fw.op(pe, lambda: nc.tensor.matmul(scp, kp[:, cs_], qp[:, cs_], start=True, stop=True),
                          reads=[Bk, Bq], writes=[B_sc[si]])
                    sT, BsT = sT_r[ch % 2]
                    fw.op(dve, lambda: nc.vector.tensor_tensor(sT, scp, d2_sb[:, h, :], ALU.mult),
                          reads=[B_sc[si], CONST], writes=[BsT])
                    pre[ch] = (kz, Bkz, sT, BsT)

                emit_i(0)
                yield
                for grp in range(NCH // 4):
                    ro, Bro = pbank[PROA], B_pb[PROA]
                    for ci in range(4):
                        ch = grp * 4 + ci
                        cs_ = slice(ch * CH, (ch + 1) * CH)
                        if ch + 1 < NCH:
                            emit_i(ch + 1)
                        kz, Bkz, sT, BsT = pre.pop(ch)
                        rov = ro[:, ci * 128:(ci + 1) * 128]
                        fw.op(pe, lambda rov=rov, sT=sT: nc.tensor.matmul(rov, vtm[:, ch, :], sT, start=True, stop=False),
                              reads=[Bv, BsT], writes=[Bro], mark=False)
                        fw.op(pe, lambda rov=rov: nc.tensor.matmul(rov, Sret_bf[:, h, :], qp[:, cs_], start=False, stop=True),
                              reads=[Bv, BsT, B_Sretbf[h], Bq], writes=[Bro])
                        ui = u_i[0] % 2; u_i[0] += 1
                        up = pbank[PU][:, ui * 192:ui * 192 + 128]
                        fw.op(pe, lambda up=up, kz=kz: nc.tensor.matmul(up, kz, vtm[:, ch, :], start=True, stop=True),
                              reads=[Bkz, Bv], writes=[B_u[ui]])
                        fw.op(dve, lambda up=up: nc.vector.scalar_tensor_tensor(Sret[:, h, :], Sret[:, h, :], G128[h], up, ALU.mult, ALU.add),
                              reads=[B_u[ui]], writes=[B_Sret[h]])
                        fw.op(act, lambda: nc.scalar.copy(out=Sret_bf[:, h, :], in_=Sret[:, h, :]),
                              reads=[B_Sret[h]], writes=[B_Sretbf[h]])
                        yield
                    gs = slice(grp * TT, (grp + 1) * TT)
                    rob, Brob = tmpb.next()
                    fw.op(act, lambda rob=rob: nc.scalar.copy(out=rob[:], in_=ro[:, :]), reads=[Bro], writes=[Brob])
                    stp, Bst = pbank[PSTAT], B_pb[PSTAT]
                    fw.op(pe, lambda rob=rob: nc.tensor.matmul(stp[:, :], ones_sb[:, :], rob[:], start=True, stop=True),
                          reads=[Brob, CONST], writes=[Bst])
                    yield
                    mean, Bmean = tmpf.next()
                    fw.op(act, lambda mean=mean: nc.scalar.mul(mean[:], stp[:, :], 1.0 / 128), reads=[Bst], writes=[Bmean])
                    cen, Bcen = tmpf.next()
                    fw.op(dve, lambda cen=cen, mean=mean: nc.vector.tensor_tensor(cen[:], ro[:, :], mean[:], ALU.subtract),
                          reads=[Bro, Bmean], writes=[Bcen])
                    sq, Bsq = tmpb.next()
                    fw.op(act, lambda sq=sq, cen=cen: nc.scalar.activation(out=sq[:], in_=cen[:], func=AF.Square), reads=[Bcen], writes=[Bsq])
                    fw.op(pe, lambda sq=sq: nc.tensor.matmul(stp[:, :], ones_sb[:, :], sq[:], start=True, stop=True),
                          reads=[Bsq, CONST], writes=[Bst])
                    yield
                    rs, Brs = rstd_t.next()
                    fw.op(act, lambda rs=rs: nc.scalar.activation(out=rs[:], in_=stp[:, :], func=AF.Ln, bias=eps_sb[:, 0:1], scale=1.0 / 128),
                          reads=[Bst, CONST], writes=[Brs])
                    fw.op(act, lambda rs=rs: nc.scalar.activation(out=rs[:], in_=rs[:], func=AF.Exp, scale=-0.5), reads=[Brs], writes=[Brs])
                    fw.op(dve, lambda cen=cen, rs=rs: nc.vector.tensor_tensor(cen[:], cen[:], rs[:], ALU.mult), reads=[Brs], writes=[Bcen])
                    fw.op(dve, lambda cen=cen: nc.vector.tensor_tensor(mixb[:, h, gs], cen[:], sg[:, gs], ALU.mult),
                          reads=[Bcen, Bg], writes=[B_mix[h]])
                    yield
                toks = []
                for b in (Bq, Bk, Bg, Bv):
                    toks += b.all_tokens()
                set_prev[s] = toks
            return A, B

        def make_pool(s):
            st = {}

            def A():
                return
                yield

            def B():
                sd = set_prev[s]
                EXT = MT + 16
                bufA = set_view(s, 0, 2 * EXT).bitcast(F32)
                bufB = set_view(s, 2 * EXT, 2 * EXT).bitcast(F32)
                bufU = set_view(s, 4 * EXT, 2 * EXT).bitcast(F32)
                pooled = set_view(s, 6 * EXT, MT)
                BA, BB, BU, BP = Buf("pA", sd), Buf("pB", sd), Buf("pU", sd), Buf("pP", sd)
                for g in range(4):
                    if g % 2 == 0:
                        wsl, Bs = load_slot(win_d[l, :, PC_POOL(g // 2):PC_POOL(g // 2) + 256], KC, 256)
                        st["w"] = (wsl, Bs)
                    wsl, Bs = st["w"]
                    w = 2 << g
                    fw.op(act, lambda g=g: nc.scalar.copy(out=bufU[:, 0:16], in_=halo[:, g, :]), reads=[B_halo[g]], writes=[BU])
                    for tt in range(NTT):
                        pz, Bpz = proj_F(wsl, Bs, (g % 2) * 128, 128, tt)
                        fw.op(act, lambda pz=pz, tt=tt: nc.scalar.copy(out=bufU[:, 16 + tt * TT:16 + (tt + 1) * TT], in_=pz[:, :]),
                              reads=[Bpz], writes=[BU])
                        yield
                    fw.op(act, lambda g=g: nc.scalar.copy(out=halo[:, g, :], in_=bufU[:, MT:MT + 16]), reads=[BU], writes=[B_halo[g]])
                    cur, Bcur = bufU, BU
                    step = 1
                    pp = [(bufA, BA), (bufB, BB)]
                    k = 0
                    while step < w:
                        nxt, Bnxt = pp[k % 2]; k += 1
                        fw.op(dve, lambda cur=cur, nxt=nxt, step=step: nc.vector.tensor_tensor(nxt[:, step:EXT], cur[:, step:EXT], cur[:, 0:EXT - step], ALU.add),
                              reads=[Bcur], writes=[Bnxt])
                        cur, Bcur = nxt, Bnxt
                        step *= 2
                    fw.op(dve, lambda cur=cur, w=w: nc.vector.scalar_tensor_tensor(pooled[:, :], cur[:, 16:EXT], 1.0 / w, bufU[:, 16:EXT], ALU.mult, ALU.subtract),
                          reads=[Bcur, BU], writes=[BP])
                    if m == 0:
                        t1, Bt1 = tmpf.next()
                        fw.op(dve, lambda cur=cur, t1=t1, g=g: nc.vector.tensor_tensor(t1[:, 0:16], cur[:, 16:32], invcnt_sb[:, g, :], ALU.mult),
                              reads=[Bcur, CONST], writes=[Bt1])
                        fw.op(dve, lambda t1=t1: nc.vector.tensor_tensor(pooled[:, 0:16], t1[:, 0:16], bufU[:, 16:32], ALU.subtract),
                              reads=[Bt1, BU], writes=[BP])
                    for tt in range(NTT):
                        ts = slice(tt * TT, (tt + 1) * TT)
                        pz, Bpz = next_pz()
                        fw.op(pe, lambda pz=pz, g=g, ts=ts: nc.tensor.matmul(pz[:, :], wpool_sb[:, g, :], pooled[:, ts], start=True, stop=True),
                              reads=[BP, B_lp], writes=[Bpz])
                        fw.op(act, lambda pz=pz, g=g, ts=ts: nc.scalar.mul(mixb[:, RH + g, ts], pz[:, :], spool_sb[:, g:g + 1]),
                              reads=[Bpz, B_lp], writes=[B_mix[RH + g]])
                    yield
                toks = []
                for b in (BA, BB, BU, BP):
                    toks += b.all_tokens()
                set_prev[s] = toks
            return A, B

        def make_gla(h, s):
            st = {}

            def A():
                sd = set_prev[s]
                qd = set_view(s, 0, MT); ki = set_view(s, MT, MT); ks = set_view(s, 2 * MT, MT)
                sga = set_view(s, 3 * MT, MT); sgb = set_view(s, 4 * MT, MT)
                vtm = set_view(s, 5 * MT, NCH * DV).rearrange("p (c e) -> p c e", e=DV)
                gch = set_view(s, 5 * MT + NCH * DV, 2 * NCH).bitcast(F32)
                Bqd, Bki, Bks, Bsga, Bsgb, Bv, Bgch = (Buf(n, sd) for n in ("qd", "ki", "ks", "sga", "sgb", "gv", "gch"))
                st.update(qd=qd, ki=ki, ks=ks, sga=sga, sgb=sgb, vtm=vtm, gch=gch,
                          Bqd=Bqd, Bki=Bki, Bks=Bks, Bsga=Bsga, Bsgb=Bsgb, Bv=Bv, Bgch=Bgch)
                wsl, Bs = load_slot(win_d[l, :, PC_GQK(h):PC_GQK(h) + 192], KC, 192)
                for tt in range(NTT):
                    ts = slice(tt * TT, (tt + 1) * TT)
                    pg, Bpg = next_pz()
                    fw.op(pe, lambda pg=pg, ts=ts: nc.tensor.matmul(pg[0:DK, :], wg_sb[:, h * DK:(h + 1) * DK], st_gz[0][0:16, ts], start=True, stop=True),
                          reads=[B_lp, st_gz[1]], writes=[Bpg])
                    e1, Be1 = tmpf.next()
                    fw.op(act, lambda pg=pg, e1=e1: nc.scalar.activation(out=e1[0:DK, :], in_=pg[0:DK, :], func=AF.Exp, bias=nbg_sb[0:DK, h:h + 1], scale=-1.0),
                          reads=[Bpg, B_lp], writes=[Be1])
                    fw.op(act, lambda e1=e1: nc.scalar.activation(out=e1[0:DK, :], in_=e1[0:DK, :], func=AF.Ln, bias=1.0, scale=1.0),
                          reads=[Be1], writes=[Be1])
                    cs, Bcs = tmpf.next()
                    fw.op(dve, lambda e1=e1, cs=cs: nc.vector.tensor_tensor_scan(cs[0:DK, :], smask_sb[0:DK, 0:TT], e1[0:DK, :], 0.0, ALU.mult, ALU.add),
                          reads=[Be1, CONST], writes=[Bcs])
                    eb, Beb = tmpf.next()
                    fw.op(act, lambda eb=eb, cs=cs: nc.scalar.activation(out=eb[0:DK, :], in_=cs[0:DK, :], func=AF.Exp, scale=-1.0 / 16),
                          reads=[Bcs], writes=[Beb])
                    fw.op(act, lambda cs=cs: nc.scalar.activation(out=cs[0:DK, :], in_=cs[0:DK, :], func=AF.Exp, scale=1.0 / 16),
                          reads=[Beb], writes=[Bcs])
                    fw.op(act, lambda eb=eb, tt=tt: nc.scalar.copy(out=gch[0:DK, tt * 4:(tt + 1) * 4], in_=eb[0:DK, 127::128]),
                          reads=[Beb], writes=[Bgch])
                    yield
                    pq, Bpq = proj_F(wsl, Bs, 0, DK, tt)
                    fw.op(dve, lambda pq=pq, eb=eb, ts=ts: nc.vector.scalar_tensor_tensor(qd[0:DK, ts], pq[0:DK, :], float(DK) ** -0.5, eb[0:DK, :], ALU.mult, ALU.mult),
                          reads=[Bpq, Beb], writes=[Bqd])
                    yield
                    pk, Bpk = proj_F(wsl, Bs, DK, DK, tt)
                    fw.op(dve, lambda pk=pk, cs=cs, ts=ts: nc.vector.tensor_tensor(ki[0:DK, ts], pk[0:DK, :], cs[0:DK, :], ALU.mult),
                          reads=[Bpk, Bcs], writes=[Bki])
                    fw.op(dve, lambda eb=eb, ts=ts: nc.vector.tensor_tensor(
                        ks[0:DK, ts].rearrange("p (c i) -> p c i", i=128),
                        ki[0:DK, ts].rearrange("p (c i) -> p c i", i=128),
                        eb[0:DK, 127::128].unsqueeze(2).to_broadcast([DK, TT // 128, 128]), ALU.mult),
                        reads=[Bki, Beb], writes=[Bks])
                    yield
                wsl, Bs = load_slot(win_d[l, :, PC_GV(h):PC_GV(h) + 192], KC, 192)
                yield from proj_T(wsl, Bs, 0, DV, list(range(NCH)), lambda grp: vtm[:, grp[0]:grp[0] + len(grp), :], Bv)
                wsl, Bs = load_slot(win_d[l, :, PC_GR(h):PC_GR(h) + 192], KC, 192)
                for tt in range(NTT):
                    ts = slice(tt * TT, (tt + 1) * TT)
                    pz, Bpz = proj_F(wsl, Bs, 0, 128, tt)
                    fw.op(act, lambda pz=pz, ts=ts: nc.scalar.activation(out=sga[:, ts], in_=pz[:, :], func=AF.Silu), reads=[Bpz], writes=[Bsga])
                    yield
                    pz, Bpz = proj_F(wsl, Bs, 128, 64, tt)
                    fw.op(act, lambda pz=pz, ts=ts: nc.scalar.activation(out=sgb[0:64, ts], in_=pz[0:64, :], func=AF.Silu), reads=[Bpz], writes=[Bsgb])
                    yield

            def B():
                qd, ki, ks, sga, sgb, vtm, gch = (st[k] for k in ("qd", "ki", "ks", "sga", "sgb", "vtm", "gch"))
                Bqd, Bki, Bks, Bsga, Bsgb, Bv, Bgch = (st[k] for k in ("Bqd", "Bki", "Bks", "Bsga", "Bsgb", "Bv", "Bgch"))
                sT_r = gsT_r
                ka, kb = 10 + 2 * h, 11 + 2 * h
                pre = {}

                def emit_i(ch):
                    cs_ = slice(ch * CH, (ch + 1) * CH)
                    ti = tr_i[0] % 2; tr_i[0] += 1
                    trp = tr_ps_gla[:, ti * 128:ti * 128 + DK]
                    Btr = B_pb[PROT]
                    fw.op(pe, lambda: nc.tensor.transpose(trp, ks[0:DK, cs_], ident_sb[0:DK, 0:DK]),
                          reads=[Bks, CONST], writes=[Btr])
                    kst, Bkst = kst_r[ch % 2]
                    fw.op(act, lambda: nc.scalar.copy(out=kst[:, 0:DK], in_=trp), reads=[Btr], writes=[Bkst])
                    si = sc_i[0] % 4; sc_i[0] += 1
                    scp = pbank[PSC][:, si * 128:(si + 1) * 128]
                    fw.op(pe, lambda: nc.tensor.matmul(scp, ki[0:DK, cs_], qd[0:DK, cs_], start=True, stop=True),
                          reads=[Bki, Bqd], writes=[B_sc[si]])
                    sT, BsT = sT_r[ch % 2]
                    fw.op(dve, lambda: nc.vector.tensor_tensor(sT, scp, causal_sb[:, :], ALU.mult),
                          reads=[B_sc[si], CONST], writes=[BsT])
                    pre[ch] = (kst, Bkst, sT, BsT)

                emit_i(0)
                yield
                for grp in range(NCH // 4):
                    roA, BroA = pbank[PROA], B_pb[PROA]
                    roB, BroB = pbank[PROB], B_pb[PROB]
                    for ci in range(4):
                        ch = grp * 4 + ci
                        cs_ = slice(ch * CH, (ch + 1) * CH)
                        if ch + 1 < NCH:
                            emit_i(ch + 1)
                        kst, Bkst, sT, BsT = pre.pop(ch)
                        ra = roA[:, ci * 128:(ci + 1) * 128]
                        rb = roB[0:64, ci * 128:(ci + 1) * 128]
                        fw.op(pe, lambda ra=ra, sT=sT: nc.tensor.matmul(ra, vtm[:, ch, 0:128], sT, start=True, stop=False),
                              reads=[Bv, BsT], writes=[BroA], mark=False)
                        fw.op(pe, lambda ra=ra: nc.tensor.matmul(ra, Sgla_bf[0:DK, h, 0:128], qd[0:DK, cs_], start=False, stop=True),
                              reads=[Bv, BsT, B_Sglabf[h], Bqd], writes=[BroA])
                        fw.op(pe, lambda rb=rb, sT=sT: nc.tensor.matmul(rb, vtm[:, ch, 128:192], sT, start=True, stop=False),
                              reads=[Bv, BsT], writes=[BroB], mark=False)
                        fw.op(pe, lambda rb=rb: nc.tensor.matmul(rb, Sgla_bf[0:DK, h, 128:192], qd[0:DK, cs_], start=False, stop=True),
                              reads=[Bv, BsT, B_Sglabf[h], Bqd], writes=[BroB])
                        ui = u_i[0] % 2; u_i[0] += 1
                        up = pbank[PU][0:DK, ui * 192:(ui + 1) * 192]
                        fw.op(pe, lambda up=up, kst=kst: nc.tensor.matmul(up, kst[:, 0:DK], vtm[:, ch, :], start=True, stop=True),
                              reads=[Bkst, Bv], writes=[B_u[ui]])
                        fw.op(dve, lambda up=up, ch=ch: nc.vector.scalar_tensor_tensor(Sgla[0:DK, h, :], Sgla[0:DK, h, :], gch[0:DK, ch:ch + 1], up, ALU.mult, ALU.add),
                              reads=[B_u[ui], Bgch], writes=[B_Sgla[h]])
                        fw.op(act, lambda: nc.scalar.copy(out=Sgla_bf[0:DK, h, :], in_=Sgla[0:DK, h, :]),
                              reads=[B_Sgla[h]], writes=[B_Sglabf[h]])
                        yield
                    gs = slice(grp * TT, (grp + 1) * TT)
                    sqa, Bsqa = tmpb.next()
                    sqb, Bsqb = tmpb.next()
                    fw.op(act, lambda sqa=sqa: nc.scalar.activation(out=sqa[:], in_=roA[:, :], func=AF.Square), reads=[BroA], writes=[Bsqa])
                    fw.op(act, lambda sqb=sqb: nc.scalar.activation(out=sqb[0:64, :], in_=roB[0:64, :], func=AF.Square), reads=[BroB], writes=[Bsqb])
                    stp, Bst = pbank[PSTAT], B_pb[PSTAT]
                    fw.op(pe, lambda sqa=sqa: nc.tensor.matmul(stp[:, :], ones_sb[:, :], sqa[:], start=True, stop=False),
                          reads=[Bsqa, CONST], writes=[Bst], mark=False)
                    fw.op(pe, lambda sqb=sqb: nc.tensor.matmul(stp[:, :], ones_sb[0:64, :], sqb[0:64, :], start=False, stop=True),
                          reads=[Bsqa, Bsqb, CONST], writes=[Bst])
                    yield
                    rs, Brs = rstd_t.next()
                    fw.op(act, lambda rs=rs: nc.scalar.activation(out=rs[:], in_=stp[:, :], func=AF.Ln, bias=eps_sb[:, 0:1], scale=1.0 / DV),
                          reads=[Bst, CONST], writes=[Brs])
                    fw.op(act, lambda rs=rs: nc.scalar.activation(out=rs[:], in_=rs[:], func=AF.Exp, scale=-0.5), reads=[Brs], writes=[Brs])
                    t1, Bt1 = tmpf.next()
                    fw.op(dve, lambda t1=t1, rs=rs: nc.vector.tensor_tensor(t1[:], roA[:, :], rs[:], ALU.mult), reads=[BroA, Brs], writes=[Bt1])
                    fw.op(dve, lambda t1=t1: nc.vector.tensor_tensor(mixb[:, ka, gs], t1[:], sga[:, gs], ALU.mult), reads=[Bt1, Bsga], writes=[B_mix[ka]])
                    t2, Bt2 = tmpf.next()
                    fw.op(dve, lambda t2=t2, rs=rs: nc.vector.tensor_tensor(t2[0:64, :], roB[0:64, :], rs[0:64, :], ALU.mult), reads=[BroB, Brs], writes=[Bt2])
                    fw.op(dve, lambda t2=t2: nc.vector.tensor_tensor(mixb[0:64, kb, gs], t2[0:64, :], sgb[0:64, gs], ALU.mult), reads=[Bt2, Bsgb], writes=[B_mix[kb]])
                    yield
                toks = []
                for b in (Bqd, Bki, Bks, Bsga, Bsgb, Bv, Bgch):
                    toks += b.all_tokens()
                set_prev[s] = toks
            return A, B

        gzT = f32_view(512, MT // 2).bitcast(BF16)
        B_gz = Buf("gzT", seed)
        st_gz = (gzT, B_gz)

        def gz_A():
            for tt in range(NTT):
                ts = slice(tt * TT, (tt + 1) * TT)
                pz, Bpz = next_pz()
                mm_group(pz[0:16, :], [(wgz_sb[:, kc, :], hT[:, kc, ts]) for kc in range(KC)], reads=[B_lp, B_h[tt]], writes=[Bpz])
                fw.op(act, lambda pz=pz, ts=ts: nc.scalar.copy(out=gzT[0:16, ts], in_=pz[0:16, :]), reads=[Bpz], writes=[B_gz])
                yield

        ui_ = 0
        kinds = []
        for h in range(RH):
            units.append(make_ret(h, ui_ % 2)); ui_ += 1; kinds.append("ret")
        units.append(make_pool(ui_ % 2)); ui_ += 1; kinds.append("pool")
        gla_first = len(units)
        for h in range(GH):
            units.append(make_gla(h, ui_ % 2)); ui_ += 1; kinds.append("gla")

        if stop and stop.startswith("units"):
            units = units[:int(stop[5:].rstrip("A"))]
        NA = {"ret": 8, "pool": 0, "gla": 14}
        NB = {"ret": 15, "pool": 12, "gla": 13}

        def chain(*gens):
            for g in gens:
                yield from g

        for _ in units[0][0]():
            pass
        if stop and stop.endswith("A"):
            units = []
        for ui2 in range(len(units)):
            genB = units[ui2][1]()
            if ui2 + 1 < len(units):
                na = NA[kinds[ui2 + 1]]
                if ui2 + 1 == gla_first:
                    genA = chain(gz_A(), units[ui2 + 1][0]())
                    na += 2
                else:
                    genA = units[ui2 + 1][0]()
            else:
                genA = iter(())
                na = 0
            ratio = na / NB[kinds[ui2]]
            acc = 0.0
            for _ in genB:
                acc += ratio
                while acc >= 1.0:
                    acc -= 1.0
                    next(genA, None)
            for _ in genA:
                pass

        fence = fw.fence_tokens()
        B_x.r = list(fence)
        B_x.w = None
        for half in range(2):
            ks_ = slice(half * 8, (half + 1) * 8)
            fw.dma(sp, x_mt[:, ks_, :], src[:, tok0:tok0 + MT].rearrange("(k p) t -> p k t", p=128)[:, ks_, :],
                   reads=[B_xs[m]], writes=[B_x], sem=xs_sem)
        B_x.w = (xs_sem[2], xs_sem[0], xs_sem[1])

        if stop and (stop.startswith("units") or stop == "mixers"):
            for half in range(2):
                ks_ = slice(half * 8, (half + 1) * 8)
                fw.dma(sp, yT_d[:, tok0:tok0 + MT].rearrange("(k p) t -> p k t", p=128)[:, ks_, :], x_mt[:, ks_, :],
                       reads=[B_x], writes=[B_xs[m]], sem=xs_sem)
            return
        for sl in range(D // SLOTW):
            wsl, Bs = load_slot(wout_d[l, :, sl * SLOTW:(sl + 1) * SLOTW], NKO, SLOTW)
            for cb in range(SLOTW // 128):
                rb = sl * (SLOTW // 128) + cb
                for tt in range(NTT):
                    ts = slice(tt * TT, (tt + 1) * TT)
                    pz, Bpz = next_pz()
                    mm_group(pz[:, :], [(wsl[:, k, cb * 128:(cb + 1) * 128], mixb[:, k, ts]) for k in range(NKO)],
                             reads=[Bs] + B_mix, writes=[Bpz])
                    fw.op(dve, lambda pz=pz, rb=rb, ts=ts: nc.vector.scalar_tensor_tensor(
                        x_mt[:, rb, ts], pz[:, :], mod_sb[:, GT1 + rb:GT1 + rb + 1], x_mt[:, rb, ts], ALU.mult, ALU.add),
                        reads=[Bpz, B_mod], writes=[B_x])

        if stop == "outproj":
            for half in range(2):
                ks_ = slice(half * 8, (half + 1) * 8)
                fw.dma(sp, yT_d[:, tok0:tok0 + MT].rearrange("(k p) t -> p k t", p=128)[:, ks_, :], x_mt[:, ks_, :],
                       reads=[B_x], writes=[B_xs[m]], sem=xs_sem)
            return
        rms_to_h(gmod2, SH2)

        hid = mixb
        B_hid = B_mix
        ada_todo = []
        if l + 1 < NL:
            per = (N_ADA_SLOTS + NMT - 1) // NMT
            ada_todo = list(range(m * per, min(N_ADA_SLOTS, (m + 1) * per)))
        n_iter = 2 * (DFF // D) * (D // SLOTW)
        ada_every = max(1, n_iter // max(1, len(ada_todo))) if ada_todo else 0
        it_ctr = [0]

        def ada_tick():
            it_ctr[0] += 1
            if ada_todo and it_ctr[0] % ada_every == 0:
                ada_slot(l + 1, ada_todo.pop(0))

        for hb in range(DFF // D):
            for sl in range(D // SLOTW):
                c0 = hb * D + sl * SLOTW
                wsl, Bs = load_slot(wup_d[l, :, c0:c0 + SLOTW], KC, SLOTW)
                for cb in range(SLOTW // 128):
                    kk = sl * (SLOTW // 128) + cb
                    for tt in range(NTT):
                        ts = slice(tt * TT, (tt + 1) * TT)
                        pz, Bpz = proj_F(wsl, Bs, cb * 128, 128, tt)
                        r, Br = tmpf.next()
                        fw.op(act, lambda pz=pz, r=r: nc.scalar.activation(out=r[:], in_=pz[:, :], func=AF.Relu), reads=[Bpz], writes=[Br])
                        fw.op(dve, lambda r=r, kk=kk, ts=ts: nc.vector.tensor_tensor(hid[:, kk, ts], r[:], r[:], ALU.mult),
                              reads=[Br], writes=[B_hid[kk]])
                ada_tick()
            for sl in range(D // SLOTW):
                wsl, Bs = load_slot(wdn_d[l, hb * D:(hb + 1) * D, sl * SLOTW:(sl + 1) * SLOTW], KC, SLOTW)
                for cb in range(SLOTW // 128):
                    rb = sl * (SLOTW // 128) + cb
                    for tt in range(NTT):
                        ts = slice(tt * TT, (tt + 1) * TT)
                        pz, Bpz = next_pz()
                        mm_group(pz[:, :], [(wsl[:, k, cb * 128:(cb + 1) * 128], hid[:, k, ts]) for k in range(KC)],
                                 reads=[Bs] + B_hid[0:KC], writes=[Bpz])
                        fw.op(dve, lambda pz=pz, rb=rb, ts=ts: nc.vector.scalar_tensor_tensor(
                            x_mt[:, rb, ts], pz[:, :], mod_sb[:, GT2 + rb:GT2 + rb + 1], x_mt[:, rb, ts], ALU.mult, ALU.add),
                            reads=[Bpz, B_mod], writes=[B_x])
                ada_tick()
        while ada_todo:
            ada_slot(l + 1, ada_todo.pop(0))

        if last_layer and final_norm:
            for tt in range(NTT):
                ts = slice(tt * TT, (tt + 1) * TT)
                ss, Bss = pbank[PSTAT], B_pb[PSTAT]
                for kc in range(KC):
                    sq, Bsq = tmpb.next()
                    fw.op(act, lambda kc=kc, sq=sq: nc.scalar.activation(out=sq[:], in_=x_mt[:, kc, ts], func=AF.Square),
                          reads=[B_x], writes=[Bsq])
                    fw.op(pe, lambda kc=kc, sq=sq: nc.tensor.matmul(ss[:, :], ones_sb[:, :], sq[:], start=(kc == 0), stop=(kc == KC - 1)),
                          reads=[Bsq, CONST], writes=[Bss], mark=True)
                rs, Brs = rstd_t.next()
                fw.op(act, lambda rs=rs: nc.scalar.activation(out=rs[:], in_=ss[:, :], func=AF.Ln, bias=eps_sb[:, 0:1], scale=1.0 / D),
                      reads=[Bss, CONST], writes=[Brs])
                fw.op(act, lambda rs=rs: nc.scalar.activation(out=rs[:], in_=rs[:], func=AF.Exp, scale=-0.5), reads=[Brs], writes=[Brs])
                for kc in range(KC):
                    fw.op(dve, lambda kc=kc, rs=rs: nc.vector.scalar_tensor_tensor(x_mt[:, kc, ts], x_mt[:, kc, ts], gfin_sb[:, kc:kc + 1], rs[:], ALU.mult, ALU.mult),
                          reads=[Brs, CONST], writes=[B_x])
            dst = yT_d
        else:
            dst = yT_d if last_layer else xs_d
        for half in range(2):
            ks_ = slice(half * 8, (half + 1) * 8)
            fw.dma(sp, dst[:, tok0:tok0 + MT].rearrange("(k p) t -> p k t", p=128)[:, ks_, :], x_mt[:, ks_, :],
                   reads=[B_x], writes=[B_xs[m]], sem=xs_sem)
        B_xs[m].w = (xs_sem[2], xs_sem[0], xs_sem[1])

    for l in range(NL):
        layer_prologue(l)
        for m in range(NMT):
            run_mt(l, m)
    sp.wait(fw.fence_tokens())
    return nc


def prep_shared(inputs, NL):
    f = lambda a: np.ascontiguousarray(np.asarray(a, dtype=np.float32))
    perm = in_col_perm()
    rows = out_row_map()
    w_out = np.asarray(inputs["w_out"], dtype=np.float32)[:NL]
    w_out_p = np.zeros((NL, NKO * 128, D), np.float32)
    valid = rows >= 0
    w_out_p[:, valid, :] = w_out[:, rows[valid], :]
    sh = {
        "w_ada": f(np.asarray(inputs["w_ada"])[:NL]),
        "b_ada_fm": f(np.asarray(inputs["b_ada"])[:NL].reshape(NL, 96, 128).transpose(0, 2, 1)),
        "g_mix_fm": f(np.asarray(inputs["g_mix"])[:NL].reshape(NL, KC, 128).transpose(0, 2, 1)),
        "g_mlp_fm": f(np.asarray(inputs["g_mlp"])[:NL].reshape(NL, KC, 128).transpose(0, 2, 1)),
        "w_in_p": f(np.asarray(inputs["w_in"])[:NL][:, :, perm]),
        "w_gate_up": f(np.asarray(inputs["w_gate_up"])[:NL]),
        "w_pool": f(np.asarray(inputs["w_pool"])[:NL]),
        "s_pool_fm": f(np.asarray(inputs["s_pool"])[:NL].reshape(NL, 4, 128).transpose(0, 2, 1)),
        "w_out_p": w_out_p,
        "w_up": f(np.asarray(inputs["w_up"])[:NL]),
        "w_down": f(np.asarray(inputs["w_down"])[:NL]),
        "g_final_fm": f(np.asarray(inputs["g_final"]).reshape(KC, 128).T),
    }
    bg = np.zeros((NL, 128, GH), np.float32)
    bg[:, :DK, :] = np.asarray(inputs["b_gate"], dtype=np.float32)[:NL].reshape(NL, GH, DK).transpose(0, 2, 1)
    sh["b_gate_fm"] = bg
    return sh


def run_cores(nc, shared, x_list, c_list, pos0_list, seqstart_list):
    in_maps = []
    for x, c, p0, ss in zip(x_list, c_list, pos0_list, seqstart_list):
        m = dict(shared)
        m["xT"] = np.ascontiguousarray(np.asarray(x, dtype=np.float32).T)
        m["c_fm"] = np.ascontiguousarray(np.asarray(c, dtype=np.float32).reshape(KC, 128).T)
        m.update(host_tables(p0, x.shape[0], ss))
        in_maps.append(m)
    res = run_bass_kernel_spmd(nc, in_maps, core_ids=list(range(len(in_maps))))
    return [np.ascontiguousarray(r["yT"].T) for r in res.results]


_CACHE = {}


def kernel(x, c, w_ada, b_ada, g_mix, g_mlp, w_in, w_gate_up, b_gate, w_pool, s_pool,
           w_out, w_up, w_down, g_final):
    inputs = dict(w_ada=w_ada, b_ada=b_ada, g_mix=g_mix, g_mlp=g_mlp, w_in=w_in, w_gate_up=w_gate_up,
                  b_gate=b_gate, w_pool=w_pool, s_pool=s_pool, w_out=w_out, w_up=w_up, w_down=w_down,
                  g_final=g_final)
    x = np.asarray(x, dtype=np.float32)
    c = np.asarray(c, dtype=np.float32)
    B, S, _ = x.shape
    NL = 4
    NMT = S // MT
    key = (NL, NMT)
    if key not in _CACHE:
        _CACHE[key] = build_program(NL, NMT)
    nc = _CACHE[key]
    shared = prep_shared(inputs, NL)
    xs, cs, p0, ss = [], [], [], []
    for core in range(8):
        b = core % B
        xs.append(x[b]); cs.append(c[b]); p0.append(0); ss.append(True)
    outs = run_cores(nc, shared, xs, cs, p0, ss)
    return np.stack([outs[b] for b in range(B)], axis=0).astype(np.float32)
```

```python
import math
import numpy as np
import concourse.bass as bass
import concourse.mybir as mybir
from concourse.bass_utils import run_bass_kernel_spmd

F32 = mybir.dt.float32
BF16 = mybir.dt.bfloat16
AF = mybir.ActivationFunctionType
ALU = mybir.AluOpType

D = 2048
KC = 16
MT = 1024
TT = 512
NTT = MT // TT
CH = 128
NCH = MT // CH
RH = 6
GH = 4
DK = 96
DV = 192
NKO = 18
DFF = 8192
EPS = 1e-6
IN_WIDTH = 5904
SLOTW = 256


class Eng:
    def __init__(self, fw, name, eng):
        self.fw = fw
        self.name = name
        self.eng = eng
        self.sem = fw.nc.alloc_semaphore("sem_" + name)
        self.count = 0
        self.waited = {}

    def wait(self, toks):
        best = {}
        for t in toks:
            if t is None:
                continue
            key, sem, val = t
            if val <= self.waited.get(key, 0):
                continue
            if key not in best or best[key][1] < val:
                best[key] = (sem, val)
        for key, (sem, val) in best.items():
            self.eng.wait_ge(sem, val)
            self.waited[key] = val

    def mark(self, inst):
        inst.then_inc(self.sem, 1)
        self.count += 1
        return (self.name, self.sem, self.count)

    def now(self):
        if self.count == 0:
            return None
        return (self.name, self.sem, self.count)


class Buf:
    def __init__(self, name="", seed=None):
        self.name = name
        self.w = None
        self.r = list(seed) if seed else []

    def rdeps(self):
        return [self.w]

    def wdeps(self):
        return [self.w] + self.r

    def wrote(self, tok):
        self.w = tok
        self.r = []

    def read(self, tok):
        self.r.append(tok)
        if len(self.r) > 16:
            best = {}
            for t in self.r:
                if t is None:
                    continue
                if t[0] not in best or best[t[0]][2] < t[2]:
                    best[t[0]] = t
            self.r = list(best.values())

    def all_tokens(self):
        return [t for t in ([self.w] + self.r) if t is not None]


class PBuf(Buf):
    def rdeps(self):
        return self.wdeps()

    def read(self, tok):
        self.wrote(tok)


class FW:
    def __init__(self):
        self.nc = bass.Bass("TRN2", target_bir_lowering=False)
        nc = self.nc
        self.pe = Eng(self, "pe", nc.tensor)
        self.act = Eng(self, "act", nc.scalar)
        self.dve = Eng(self, "dve", nc.vector)
        self.pool = Eng(self, "pool", nc.gpsimd)
        self.sp = Eng(self, "sp", nc.sync)
        self.engs = [self.pe, self.act, self.dve, self.pool, self.sp]
        self.dsems = []

    def sb(self, name, shape, dt):
        return self.nc.alloc_sbuf_tensor(name, list(shape), dt)

    def op(self, E, fn, reads=(), writes=(), mark=True):
        deps = []
        for b in reads:
            deps += b.rdeps()
        for b in writes:
            deps += b.wdeps()
        E.wait(deps)
        inst = fn()
        if not mark:
            return None
        tok = E.mark(inst)
        for b in reads:
            b.read(tok)
        for b in writes:
            b.wrote(tok)
        return tok

    def dsem(self, name):
        s = [self.nc.alloc_semaphore(name), 0, "d_" + name]
        self.dsems.append(s)
        return s

    def dma(self, E, out_ap, in_ap, reads=(), writes=(), sem=None):
        deps = []
        for b in reads:
            deps += b.rdeps()
        for b in writes:
            deps += b.wdeps()
        E.wait(deps)
        inst = E.eng.dma_start(out=out_ap, in_=in_ap)
        sem[1] += 16
        inst.then_inc(sem[0], 16)
        tok = (sem[2], sem[0], sem[1])
        for b in reads:
            b.read(tok)
        for b in writes:
            b.wrote(tok)
        return tok

    def fence_tokens(self):
        toks = [e.now() for e in self.engs]
        toks += [(s[2], s[0], s[1]) for s in self.dsems if s[1] > 0]
        return [t for t in toks if t is not None]


class Rot:
    def __init__(self, fw, name, n, shape, dt, space="sb"):
        self.items = []
        for i in range(n):
            t = fw.sb(f"{name}{i}", shape, dt)
            self.items.append((t, Buf(f"{name}{i}")))
        self.i = 0

    def next(self):
        it = self.items[self.i % len(self.items)]
        self.i += 1
        return it


RET_W = 768
OFF_RQ, OFF_RK, OFF_RV, OFF_RG = 0, 768, 1536, 2304
OFF_PU = 3072
OFF_GQ, OFF_GK, OFF_GV, OFF_GR, OFF_GZ = 3584, 3968, 4352, 5120, 5888


def in_col_perm():
    cols = []
    for h in range(RH):
        cols += list(range(OFF_RQ + h * 128, OFF_RQ + (h + 1) * 128))
        cols += list(range(OFF_RK + h * 128, OFF_RK + (h + 1) * 128))
    for h in range(RH):
        cols += list(range(OFF_RG + h * 128, OFF_RG + (h + 1) * 128))
        cols += list(range(OFF_RV + h * 128, OFF_RV + (h + 1) * 128))
    cols += list(range(OFF_PU, OFF_PU + 512))
    for h in range(GH):
        cols += list(range(OFF_GQ + h * DK, OFF_GQ + (h + 1) * DK))
        cols += list(range(OFF_GK + h * DK, OFF_GK + (h + 1) * DK))
        cols += list(range(OFF_GV + h * DV, OFF_GV + (h + 1) * DV))
        cols += list(range(OFF_GR + h * DV, OFF_GR + (h + 1) * DV))
    cols += list(range(OFF_GZ, OFF_GZ + 16))
    assert len(cols) == IN_WIDTH and len(set(cols)) == IN_WIDTH
    return np.array(cols)


PC_RQK = lambda h: h * 256
PC_RGV = lambda h: 1536 + h * 256
PC_POOL = lambda s: 3072 + s * 256
PC_GQK = lambda h: 3584 + h * 576
PC_GV = lambda h: 3584 + h * 576 + 192
PC_GR = lambda h: 3584 + h * 576 + 384
PC_GZ = 5888


def out_row_map():
    rows = []
    for h in range(RH):
        rows += list(range(h * 128, (h + 1) * 128))
    for g in range(4):
        rows += list(range(768 + g * 128, 768 + (g + 1) * 128))
    for h in range(GH):
        base = 1280 + h * DV
        rows += list(range(base, base + 128))
        rows += list(range(base + 128, base + 192)) + [-1] * 64
    assert len(rows) == NKO * 128
    return np.array(rows)


def host_tables(pos0, ntok, seq_start):
    t = {}
    inv_freq = (10000.0 ** (-np.arange(0, 128, 2, dtype=np.float32) / np.float32(128))).astype(np.float32)
    pos = (pos0 + np.arange(ntok)).astype(np.float32)
    ang = (pos[None, :] * inv_freq[:, None]).astype(np.float32)
    cos = np.cos(ang.astype(np.float64)).astype(np.float32)
    sin = np.sin(ang.astype(np.float64)).astype(np.float32)
    t["cosT"] = np.ascontiguousarray(np.concatenate([cos, cos], 0))
    t["sinT"] = np.ascontiguousarray(np.concatenate([-sin, sin], 0))
    hh = np.arange(RH, dtype=np.float64)
    log_g = np.log1p(-np.exp2(-5.0 - hh))
    i = np.arange(128, dtype=np.float64)
    xi = np.exp((i[None, :] + 1.0) * log_g[:, None]) * (128.0 ** -0.5)
    t["xi"] = np.ascontiguousarray(np.broadcast_to(xi[None], (128, RH, 128))).astype(np.float32)
    jj = i[:, None, None]
    ii = i[None, None, :]
    d2 = np.where(jj <= ii, np.exp(-(jj + 1.0) * log_g[None, :, None]), 0.0)
    t["d2"] = np.ascontiguousarray(d2).astype(np.float32)
    zeta = np.exp((127.0 - i)[:, None] * log_g[None, :])
    t["zeta"] = np.ascontiguousarray(zeta).astype(np.float32)
    t["causal"] = (i[:, None] <= i[None, :]).astype(np.float32)
    t["ident"] = np.eye(128, dtype=np.float32)
    sw = np.zeros((128, 128), np.float32)
    for m in range(128):
        sw[(m + 64) % 128, m] = 1.0
    t["pswap"] = sw
    sm = np.ones((128, MT), np.float32)
    sm[:, ::CH] = 0.0
    t["scanmask"] = sm
    ic = np.zeros((128, 4, 16), np.float32)
    for g, w in enumerate((2, 4, 8, 16)):
        for tt in range(16):
            ic[:, g, tt] = 1.0 / (min(tt + 1, w) if seq_start else w)
    t["invcnt"] = ic
    return t


G128 = [float(np.exp(128.0 * np.log1p(-np.exp2(-5.0 - h)))) for h in range(RH)]


def build_program(NL, NMT, final_norm=True, stop=None):
    fw = FW()
    nc = fw.nc
    pe, act, dve, pool, sp = fw.pe, fw.act, fw.dve, fw.pool, fw.sp
    NTOK = NMT * MT

    def din(name, shape, dt=F32):
        return nc.dram_tensor(name, list(shape), dt, kind="ExternalInput").ap()

    xT_d = din("xT", [D, NTOK])
    c_d = din("c_fm", [128, KC])
    wada_d = din("w_ada", [NL, D, 6 * D])
    bada_d = din("b_ada_fm", [NL, 128, 96])
    gmix_d = din("g_mix_fm", [NL, 128, KC])
    gmlp_d = din("g_mlp_fm", [NL, 128, KC])
    win_d = din("w_in_p", [NL, D, IN_WIDTH])
    wg_d = din("w_gate_up", [NL, 16, 384])
    bg_d = din("b_gate_fm", [NL, 128, GH])
    wpool_d = din("w_pool", [NL, 4, 128, 128])
    spool_d = din("s_pool_fm", [NL, 128, 4])
    wout_d = din("w_out_p", [NL, NKO * 128, D])
    wup_d = din("w_up", [NL, D, DFF])
    wdn_d = din("w_down", [NL, DFF, D])
    gfin_d = din("g_final_fm", [128, KC])
    cos_d = din("cosT", [128, NTOK])
    sin_d = din("sinT", [128, NTOK])
    xi_d = din("xi", [128, RH, 128])
    d2_d = din("d2", [128, RH, 128])
    zeta_d = din("zeta", [128, RH])
    causal_d = din("causal", [128, 128])
    ident_d = din("ident", [128, 128])
    pswap_d = din("pswap", [128, 128])
    smask_d = din("scanmask", [128, MT])
    invcnt_d = din("invcnt", [128, 4, 16])
    yT_d = nc.dram_tensor("yT", [D, NTOK], F32, kind="ExternalOutput").ap()
    xs_d = nc.dram_tensor("xs", [D, NTOK], F32).ap()

    arena = fw.sb("arena", [128, KC * MT], F32)
    x_mt = arena[:, :].rearrange("p (k t) -> p k t", k=KC)
    B_x = Buf("x_mt")
    hT = fw.sb("hT", [128, KC, MT], BF16)
    B_h = [Buf(f"hT{t}") for t in range(NTT)]
    mixb = fw.sb("mix", [128, NKO, MT], BF16)
    B_mix = [Buf(f"mix{k}") for k in range(NKO)]
    NSLOT = 3
    wslot = [fw.sb(f"wslot{i}", [128, NKO, SLOTW], BF16) for i in range(NSLOT)]
    B_slot = [Buf(f"wslot{i}") for i in range(NSLOT)]
    S_slot = [fw.dsem(f"wsl{i}") for i in range(NSLOT)]
    slot_i = [0]

    cos_sb = fw.sb("cos_sb", [128, MT], F32)
    sin_sb = fw.sb("sin_sb", [128, MT], F32)
    B_cs = Buf("cossin")
    xi_sb = fw.sb("xi_sb", [128, RH, 128], F32)
    d2_sb = fw.sb("d2_sb", [128, RH, 128], F32)
    zeta_sb = fw.sb("zeta_sb", [128, RH], F32)
    causal_sb = fw.sb("causal_sb", [128, 128], F32)
    ident_sb = fw.sb("ident_sb", [128, 128], BF16)
    pswap_sb = fw.sb("pswap_sb", [128, 128], BF16)
    ones_sb = fw.sb("ones_sb", [128, 128], BF16)
    smask_sb = fw.sb("smask_sb", [128, MT], F32)
    invcnt_sb = fw.sb("invcnt_sb", [128, 4, 16], F32)
    eps_sb = fw.sb("eps_sb", [128, 1], F32)
    B_const = Buf("const")

    c_sb = fw.sb("c_sb", [128, KC], F32)
    sc_bf = fw.sb("sc_bf", [128, KC], BF16)
    gfin_sb = fw.sb("gfin_sb", [128, KC], F32)
    B_c = Buf("c")

    bada_sb = fw.sb("bada_sb", [128, 96], F32)
    gmix_sb = fw.sb("gmix_sb", [128, KC], F32)
    gmlp_sb = fw.sb("gmlp_sb", [128, KC], F32)
    wg_sb = fw.sb("wg_sb", [16, 384], BF16)
    nbg_sb = fw.sb("nbg_sb", [128, GH], F32)
    bg_sb = fw.sb("bg_sb", [128, GH], F32)
    wpool_sb = fw.sb("wpool_sb", [128, 4, 128], BF16)
    spool_sb = fw.sb("spool_sb", [128, 4], F32)
    wgz_sb = fw.sb("wgz_sb", [128, KC, 16], BF16)
    mod_sb = fw.sb("mod_sb", [128, 96], F32)
    modn_sb = fw.sb("modn_sb", [128, 96], F32)
    B_modn = Buf("modn")
    gmod1 = fw.sb("gmod1", [128, KC], F32)
    gmod2 = fw.sb("gmod2", [128, KC], F32)
    B_lp = Buf("layerparams")
    B_mod = Buf("mod")

    Sret = fw.sb("Sret", [128, RH, 128], F32)
    Sret_bf = fw.sb("Sret_bf", [128, RH, 128], BF16)
    Sgla = fw.sb("Sgla", [128, GH, DV], F32)
    Sgla_bf = fw.sb("Sgla_bf", [128, GH, DV], BF16)
    halo = fw.sb("halo", [128, 4, 16], F32)
    B_Sret = [Buf(f"Sret{h}") for h in range(RH)]
    B_Sretbf = [Buf(f"Sretbf{h}") for h in range(RH)]
    B_Sgla = [Buf(f"Sgla{h}") for h in range(GH)]
    B_Sglabf = [Buf(f"Sglabf{h}") for h in range(GH)]
    B_halo = [Buf(f"halo{g}") for g in range(4)]

    tmpf = Rot(fw, "tmpf", 3, [128, TT], F32)
    tmpb = Rot(fw, "tmpb", 2, [128, TT], BF16)
    rstd_t = Rot(fw, "rstd", 2, [128, TT], F32)

    pbank = [nc.alloc_psum_tensor(f"pb{i}", [128, 512], F32) for i in range(8)]
    B_pb = [PBuf(f"pb{i}") for i in range(8)]
    PZ = [0, 1]
    PROT, PSC, PROA, PROB, PSTAT, PU = 2, 3, 4, 5, 6, 7
    pz_i = [0]

    def next_pz():
        i = PZ[pz_i[0] % len(PZ)]
        pz_i[0] += 1
        return pbank[i], B_pb[i]

    B_sc = [B_pb[PSC]] * 4
    sc_i = [0]
    B_u = [B_pb[PU]] * 2
    u_i = [0]
    tr_ps_ret = pbank[PROB][:, 0:128].bitcast(BF16)
    tr_ps_gla = pbank[PROT][:, 0:128].bitcast(BF16)
    tr_i = [0]

    ld_sem = fw.dsem("ld")
    xs_sem = fw.dsem("xs")
    B_xs = [Buf(f"xs{m}") for m in range(NMT)]

    def mm_group(out_ap, pairs, reads, writes, flags=None):
        n = len(pairs)
        for i, (l, r) in enumerate(pairs):
            last = i == n - 1
            st = (i == 0) if flags is None else flags[0]
            sp_ = last if flags is None else (flags[1] and last)
            fw.op(pe, lambda l=l, r=r, st=st, sp_=sp_: nc.tensor.matmul(out_ap, l, r, start=st, stop=sp_),
                  reads=reads, writes=writes, mark=last)

    def load_slot(src_ap, nk, width):
        i = slot_i[0] % NSLOT
        slot_i[0] += 1
        fw.dma(pool, wslot[i][:, 0:nk, 0:width], src_ap.rearrange("(k p) c -> p k c", p=128),
               writes=[B_slot[i]], sem=S_slot[i])
        return wslot[i], B_slot[i]

    lp_sem = fw.dsem("lp")
    cs_sem = fw.dsem("cs")

    def ld(dst, src, B, eng=None, sem=None):
        fw.dma(eng or sp, dst, src, writes=[B], sem=sem or ld_sem)

    for dst, src in ((xi_sb[:], xi_d[:, :, :]), (d2_sb[:], d2_d[:, :, :]), (zeta_sb[:], zeta_d[:, :]),
                     (causal_sb[:], causal_d[:, :]), (smask_sb[:], smask_d[:, :]),
                     (invcnt_sb[:], invcnt_d[:, :, :]), (c_sb[:], c_d[:, :]), (gfin_sb[:], gfin_d[:, :])):
        ld(dst, src, B_const)
    ld(ident_sb[:], ident_d[:, :], B_const, eng=pool)
    ld(pswap_sb[:], pswap_d[:, :], B_const, eng=pool)
    fw.op(dve, lambda: nc.vector.memset(ones_sb[:], 1.0), writes=[B_const])
    fw.op(dve, lambda: nc.vector.memset(eps_sb[:], EPS), writes=[B_const])
    const_tok = (ld_sem[2], ld_sem[0], ld_sem[1])
    for e in (pe, act, dve):
        e.wait([const_tok, dve.now()])

    class _Ready(Buf):
        def rdeps(self):
            return []

        def read(self, tok):
            pass
    CONST = _Ready("CONST")
    fw.op(act, lambda: nc.scalar.activation(out=sc_bf[:], in_=c_sb[:], func=AF.Silu), reads=[CONST], writes=[B_c])
    fw.op(dve, lambda: nc.vector.memset(mixb[:], 0.0), writes=B_mix)

    N_ADA_SLOTS = 6 * D // SLOTW

    def ada_slot(l, s):
        pm, Bpm = pbank[PROT], B_pb[PROT]
        wsl, Bs = load_slot(wada_d[l, :, s * SLOTW:(s + 1) * SLOTW], KC, SLOTW)
        nb = SLOTW // 128
        for cb in range(nb):
            mm_group(pm[:, cb:cb + 1],
                     [(wsl[:, kc, cb * 128:(cb + 1) * 128], sc_bf[:, kc:kc + 1]) for kc in range(KC)],
                     reads=[Bs, B_c], writes=[Bpm])
        fw.op(act, lambda: nc.scalar.copy(out=modn_sb[:, s * nb:(s + 1) * nb], in_=pm[:, 0:nb]),
              reads=[Bpm], writes=[B_modn])

    def layer_prologue(l):
        ld(bada_sb[:], bada_d[l, :, :], B_lp, sem=lp_sem)
        ld(gmix_sb[:], gmix_d[l, :, :], B_lp, sem=lp_sem)
        ld(gmlp_sb[:], gmlp_d[l, :, :], B_lp, sem=lp_sem)
        ld(bg_sb[:], bg_d[l, :, :], B_lp, sem=lp_sem)
        ld(spool_sb[:], spool_d[l, :, :], B_lp, sem=lp_sem)
        ld(wg_sb[:], wg_d[l, :, :], B_lp, eng=pool, sem=lp_sem)
        ld(wpool_sb[:], wpool_d[l, :, :, :].rearrange("g c d -> c g d"), B_lp, eng=pool, sem=lp_sem)
        ld(wgz_sb[:], win_d[l, :, PC_GZ:PC_GZ + 16].rearrange("(k p) c -> p k c", p=128), B_lp, eng=pool, sem=lp_sem)
        B_lp.w = (lp_sem[2], lp_sem[0], lp_sem[1])
        fw.op(dve, lambda: nc.vector.tensor_scalar(nbg_sb[:], bg_sb[:], -1.0, None, ALU.mult), reads=[B_lp], writes=[B_lp])
        if l == 0:
            for s in range(N_ADA_SLOTS):
                ada_slot(0, s)
        fw.op(dve, lambda: nc.vector.tensor_tensor(mod_sb[:], modn_sb[:], bada_sb[:], ALU.add),
              reads=[B_modn, B_lp], writes=[B_mod])
        fw.op(dve, lambda: nc.vector.scalar_tensor_tensor(gmod1[:], mod_sb[:, 16:32], 1.0, gmix_sb[:], ALU.add, ALU.mult),
              reads=[B_mod, B_lp], writes=[B_mod])
        fw.op(dve, lambda: nc.vector.scalar_tensor_tensor(gmod2[:], mod_sb[:, 64:80], 1.0, gmlp_sb[:], ALU.add, ALU.mult),
              reads=[B_mod, B_lp], writes=[B_mod])
        for h in range(RH):
            fw.op(dve, lambda h=h: nc.vector.memset(Sret[:, h, :], 0.0), writes=[B_Sret[h]])
            fw.op(dve, lambda h=h: nc.vector.memset(Sret_bf[:, h, :], 0.0), writes=[B_Sretbf[h]])
        for h in range(GH):
            fw.op(dve, lambda h=h: nc.vector.memset(Sgla[:, h, :], 0.0), writes=[B_Sgla[h]])
            fw.op(dve, lambda h=h: nc.vector.memset(Sgla_bf[:, h, :], 0.0), writes=[B_Sglabf[h]])
        for g in range(4):
            fw.op(dve, lambda g=g: nc.vector.memset(halo[:, g, :], 0.0), writes=[B_halo[g]])

    SH1, SC1, GT1, SH2, SC2, GT2 = 0, 16, 32, 48, 64, 80

    def rms_to_h(gmod, shift_off):
        for tt in range(NTT):
            ts = slice(tt * TT, (tt + 1) * TT)
            ss, Bss = pbank[PSTAT], B_pb[PSTAT]
            for kc in range(KC):
                sq, Bsq = tmpb.next()
                fw.op(act, lambda kc=kc, sq=sq: nc.scalar.activation(out=sq[:], in_=x_mt[:, kc, ts], func=AF.Square),
                      reads=[B_x], writes=[Bsq])
                fw.op(pe, lambda kc=kc, sq=sq: nc.tensor.matmul(ss[:, :], ones_sb[:, :], sq[:], start=(kc == 0), stop=(kc == KC - 1)),
                      reads=[Bsq, CONST], writes=[Bss], mark=True)
            rs, Brs = rstd_t.next()
            fw.op(act, lambda: nc.scalar.activation(out=rs[:], in_=ss[:, :], func=AF.Ln, bias=eps_sb[:, 0:1], scale=1.0 / D),
                  reads=[Bss, CONST], writes=[Brs])
            fw.op(act, lambda: nc.scalar.activation(out=rs[:], in_=rs[:], func=AF.Exp, scale=-0.5),
                  reads=[Brs], writes=[Brs])
            for kc in range(KC):
                t1, Bt1 = tmpf.next()
                fw.op(dve, lambda kc=kc, t1=t1: nc.vector.scalar_tensor_tensor(t1[:], x_mt[:, kc, ts], gmod[:, kc:kc + 1], rs[:], ALU.mult, ALU.mult),
                      reads=[B_x, B_mod, Brs], writes=[Bt1])
                if shift_off is None:
                    pass
                else:
                    fw.op(act, lambda kc=kc, t1=t1: nc.scalar.activation(out=hT[:, kc, ts], in_=t1[:], func=AF.Identity,
                                                                         bias=mod_sb[:, shift_off + kc:shift_off + kc + 1], scale=1.0),
                          reads=[Bt1, B_mod], writes=[B_h[tt]])

    def proj_F(wsl, Bs, c0, M, tt, nk=KC, src=None, Bsrc=None):
        src = hT if src is None else src
        Bsrc = B_h[tt] if Bsrc is None else Bsrc
        pz, Bpz = next_pz()
        ts = slice(tt * TT, (tt + 1) * TT)
        mm_group(pz[0:M, :], [(wsl[:, kc, c0:c0 + M], src[:, kc, ts]) for kc in range(nk)],
                 reads=[Bs, Bsrc], writes=[Bpz])
        return pz, Bpz

    def proj_T(wsl, Bs, c0, N, ch_list, out_view_fn, Bout, evac_eng="act"):
        per_bank = max(1, 512 // N)
        for g0 in range(0, len(ch_list), per_bank):
            grp = ch_list[g0:g0 + per_bank]
            pz, Bpz = next_pz()
            for gi, ch in enumerate(grp):
                mm_group(pz[:, gi * N:(gi + 1) * N],
                         [(hT[:, kc, ch * CH:(ch + 1) * CH], wsl[:, kc, c0:c0 + N]) for kc in range(KC)],
                         reads=[Bs, B_h[(ch * CH) // TT]], writes=[Bpz])
            dst = out_view_fn(grp)
            srcv = pz[:, 0:len(grp) * N].rearrange("p (c n) -> p c n", n=N)
            fw.op(act, lambda dst=dst, srcv=srcv: nc.scalar.copy(out=dst, in_=srcv), reads=[Bpz], writes=[Bout])
            yield

    arena_bf = arena[:, :].bitcast(BF16)
    SET_BF = 8192

    def set_view(s, off, n):
        return arena_bf[:, s * SET_BF + off: s * SET_BF + off + n]

    def f32_view(off, n):
        return arena[:, 8192 + off: 8192 + off + n]

    def run_mt(l, m):
        tok0 = m * MT
        last_layer = (l == NL - 1)
        if stop == "prologue":
            return
        src = xT_d if l == 0 else xs_d
        fence = fw.fence_tokens()
        B_x.r += fence
        for half in range(2):
            ks = slice(half * 8, (half + 1) * 8)
            fw.dma(sp, x_mt[:, ks, :], src[:, tok0:tok0 + MT].rearrange("(k p) t -> p k t", p=128)[:, ks, :],
                   reads=[B_xs[m]], writes=[B_x], sem=xs_sem)
        B_x.w = (xs_sem[2], xs_sem[0], xs_sem[1])
        fw.dma(sp, cos_sb[:], cos_d[:, tok0:tok0 + MT], writes=[B_cs], sem=cs_sem)
        fw.dma(sp, sin_sb[:], sin_d[:, tok0:tok0 + MT], writes=[B_cs], sem=cs_sem)
        B_cs.w = (cs_sem[2], cs_sem[0], cs_sem[1])

        rms_to_h(gmod1, SH1)

        if stop == "norm1":
            return
        seed = B_x.all_tokens()
        set_prev = [list(seed), list(seed)]

        units = []
        kz_r = [(f32_view(0, 64).bitcast(BF16), Buf("kz0", seed)), (f32_view(64, 64).bitcast(BF16), Buf("kz1", seed))]
        sT_r = [(f32_view(128, 64).bitcast(BF16), Buf("sT0", seed)), (f32_view(192, 64).bitcast(BF16), Buf("sT1", seed))]
        kst_r = [(f32_view(256, 64).bitcast(BF16), Buf("kst0", seed)), (f32_view(320, 64).bitcast(BF16), Buf("kst1", seed))]
        gsT_r = [(f32_view(384, 64).bitcast(BF16), Buf("gsT0", seed)), (f32_view(448, 64).bitcast(BF16), Buf("gsT1", seed))]

        def make_ret(h, s):
            st = {}

            def A():
                sd = set_prev[s]
                qp = set_view(s, 0, MT); kp = set_view(s, MT, MT); sg = set_view(s, 2 * MT, MT)
                vtm = set_view(s, 3 * MT, MT).rearrange("p (c e) -> p c e", e=128)
                Bq, Bk, Bg, Bv = Buf("qp", sd), Buf("kp", sd), Buf("sg", sd), Buf("vtm", sd)
                st.update(qp=qp, kp=kp, sg=sg, vtm=vtm, Bq=Bq, Bk=Bk, Bg=Bg, Bv=Bv)
                wsl, Bs = load_slot(win_d[l, :, PC_RQK(h):PC_RQK(h) + 256], KC, 256)
                for tt in range(NTT):
                    ts = slice(tt * TT, (tt + 1) * TT)
                    for which in range(2):
                        pz, Bpz = proj_F(wsl, Bs, which * 128, 128, tt)
                        raw, Braw = tmpb.next()
                        fw.op(act, lambda raw=raw, pz=pz: nc.scalar.copy(out=raw[:], in_=pz[:, :]), reads=[Bpz], writes=[Braw])
                        pr, Bpr = pbank[PROT], B_pb[PROT]
                        fw.op(pe, lambda raw=raw, pr=pr: nc.tensor.matmul(pr[:, :], pswap_sb[:, :], raw[:], start=True, stop=True),
                              reads=[Braw, CONST], writes=[Bpr])
                        t1, Bt1 = tmpf.next()
                        t2, Bt2 = tmpf.next()
                        fw.op(dve, lambda t1=t1, pz=pz: nc.vector.tensor_tensor(t1[:], pz[:, :], cos_sb[:, ts], ALU.mult),
                              reads=[Bpz, B_cs], writes=[Bt1])
                        fw.op(dve, lambda t2=t2, pr=pr: nc.vector.tensor_tensor(t2[:], pr[:, :], sin_sb[:, ts], ALU.mult),
                              reads=[Bpr, B_cs], writes=[Bt2])
                        if which == 0:
                            fw.op(dve, lambda t1=t1, t2=t2: nc.vector.tensor_tensor(t1[:], t1[:], t2[:], ALU.add),
                                  reads=[Bt2], writes=[Bt1])
                            fw.op(dve, lambda t1=t1: nc.vector.tensor_tensor(
                                qp[:, ts].rearrange("p (c i) -> p c i", i=128),
                                t1[:].rearrange("p (c i) -> p c i", i=128),
                                xi_sb[:, h, :].unsqueeze(1).to_broadcast([128, TT // 128, 128]), ALU.mult),
                                reads=[Bt1, CONST], writes=[Bq])
                        else:
                            fw.op(dve, lambda t1=t1, t2=t2: nc.vector.tensor_tensor(kp[:, ts], t1[:], t2[:], ALU.add),
                                  reads=[Bt1, Bt2], writes=[Bk])
                        yield
                wsl, Bs = load_slot(win_d[l, :, PC_RGV(h):PC_RGV(h) + 256], KC, 256)
                for tt in range(NTT):
                    ts = slice(tt * TT, (tt + 1) * TT)
                    pz, Bpz = proj_F(wsl, Bs, 0, 128, tt)
                    fw.op(act, lambda pz=pz: nc.scalar.activation(out=sg[:, ts], in_=pz[:, :], func=AF.Silu), reads=[Bpz], writes=[Bg])
                    yield
                yield from proj_T(wsl, Bs, 128, 128, list(range(NCH)), lambda grp: vtm[:, grp[0]:grp[0] + len(grp), :], Bv)

            def B():
                qp, kp, sg, vtm = st["qp"], st["kp"], st["sg"], st["vtm"]
                Bq, Bk, Bg, Bv = st["Bq"], st["Bk"], st["Bg"], st["Bv"]
                pre = {}

                def emit_i(ch):
                    cs_ = slice(ch * CH, (ch + 1) * CH)
                    ti = tr_i[0] % 2; tr_i[0] += 1
                    trp = tr_ps_ret[:, ti * 128:(ti + 1) * 128]
                    Btr = B_pb[PROB]
                    fw.op(pe, lambda: nc.tensor.transpose(trp, kp[:, cs_], ident_sb[:, :]),
                          reads=[Bk, CONST], writes=[Btr])
                    kz, Bkz = kz_r[ch % 2]
                    fw.op(act, lambda: nc.scalar.mul(kz, trp, zeta_sb[:, h:h + 1]),
                          reads=[Btr, CONST], writes=[Bkz])
                    si = sc_i[0] % 4; sc_i[0] += 1
                    scp = pbank[PSC][:, si * 128:(si + 1) * 128]
                    fw.op(pe, lambda: nc.tensor.matmul(scp, kp[:, cs_], qp[:, cs_], start=True, stop=True),
                          reads=[Bk, Bq], writes=[B_sc[si]])
                    sT, BsT = sT_r[ch % 2]
                    fw.op(dve, lambda: nc.vector.tensor_tensor(sT, scp, d2_sb[:, h, :], ALU.mult),
                          reads=[B_sc[si], CONST], writes=[BsT])
                    pre[ch] = (kz, Bkz, sT, BsT)

                emit_i(0)
                yield
                for grp in range(NCH // 4):
                    ro, Bro = pbank[PROA], B_pb[PROA]
                    for ci in range(4):
                        ch = grp * 4 + ci
                        cs_ = slice(ch * CH, (ch + 1) * CH)
                        if ch + 1 < NCH:
                            emit_i(ch + 1)
                        kz, Bkz, sT, BsT = pre.pop(ch)
                        rov = ro[:, ci * 128:(ci + 1) * 128]
                        fw.op(pe, lambda rov=rov, sT=sT: nc.tensor.matmul(rov, vtm[:, ch, :], sT, start=True, stop=False),
                              reads=[Bv, BsT], writes=[Bro], mark=False)
                        fw.op(pe, lambda rov=rov: nc.tensor.matmul(rov, Sret_bf[:, h, :], qp[:, cs_], start=False, stop=True),
                              reads=[Bv, BsT, B_Sretbf[h], Bq], writes=[Bro])
                        ui = u_i[0] % 2; u_i[0] += 1
                        up = pbank[PU][:, ui * 192:ui * 192 + 128]
                        fw.op(pe, lambda up=up, kz=kz: nc.tensor.matmul(up, kz, vtm[:, ch, :], start=True, stop=True),
                              reads=[Bkz, Bv], writes=[B_u[ui]])
                        fw.op(dve, lambda up=up: nc.vector.scalar_tensor_tensor(Sret[:, h, :], Sret[:, h, :], G128[h], up, ALU.mult, ALU.add),
                              reads=[B_u[ui]], writes=[B_Sret[h]])
                        fw.op(act, lambda: nc.scalar.copy(out=Sret_bf[:, h, :], in_=Sret[:, h, :]),
                              reads=[B_Sret[h]], writes=[B_Sretbf[h]])
                        yield
                    gs = slice(grp * TT, (grp + 1) * TT)
                    rob, Brob = tmpb.next()
                    fw.op(act, lambda rob=rob: nc.scalar.copy(out=rob[:], in_=ro[:, :]), reads=[Bro], writes=[Brob])
                    stp, Bst = pbank[PSTAT], B_pb[PSTAT]
                    fw.op(pe, lambda rob=rob: nc.tensor.matmul(stp[:, :], ones_sb[:, :], rob[:], start=True, stop=True),
                          reads=[Brob, CONST], writes=[Bst])
                    yield
                    mean, Bmean = tmpf.next()
                    fw.op(act, lambda mean=mean: nc.scalar.mul(mean[:], stp[:, :], 1.0 / 128), reads=[Bst], writes=[Bmean])
                    cen, Bcen = tmpf.next()
                    fw.op(dve, lambda cen=cen, mean=mean: nc.vector.tensor_tensor(cen[:], ro[:, :], mean[:], ALU.subtract),
                          reads=[Bro, Bmean], writes=[Bcen])
                    sq, Bsq = tmpb.next()
                    fw.op(act, lambda sq=sq, cen=cen: nc.scalar.activation(out=sq[:], in_=cen[:], func=AF.Square), reads=[Bcen], writes=[Bsq])
                    fw.op(pe, lambda sq=sq: nc.tensor.matmul(stp[:, :], ones_sb[:, :], sq[:], start=True, stop=True),
                          reads=[Bsq, CONST], writes=[Bst])
                    yield
                    rs, Brs = rstd_t.next()
                    fw.op(act, lambda rs=rs: nc.scalar.activation(out=rs[:], in_=stp[:, :], func=AF.Ln, bias=eps_sb[:, 0:1], scale=1.0 / 128),
                          reads=[Bst, CONST], writes=[Brs])
                    fw.op(act, lambda rs=rs: nc.scalar.activation(out=rs[:], in_=rs[:], func=AF.Exp, scale=-0.5), reads=[Brs], writes=[Brs])
                    fw.op(dve, lambda cen=cen, rs=rs: nc.vector.tensor_tensor(cen[:], cen[:], rs[:], ALU.mult), reads=[Brs], writes=[Bcen])
                    fw.op(dve, lambda cen=cen: nc.vector.tensor_tensor(mixb[:, h, gs], cen[:], sg[:, gs], ALU.mult),
                          reads=[Bcen, Bg], writes=[B_mix[h]])
                    yield
                toks = []
                for b in (Bq, Bk, Bg, Bv):
                    toks += b.all_tokens()
                set_prev[s] = toks
            return A, B

        def make_pool(s):
            st = {}

            def A():
                return
                yield

            def B():
                sd = set_prev[s]
                EXT = MT + 16
                bufA = set_view(s, 0, 2 * EXT).bitcast(F32)
                bufB = set_view(s, 2 * EXT, 2 * EXT).bitcast(F32)
                bufU = set_view(s, 4 * EXT, 2 * EXT).bitcast(F32)
                pooled = set_view(s, 6 * EXT, MT)
                BA, BB, BU, BP = Buf("pA", sd), Buf("pB", sd), Buf("pU", sd), Buf("pP", sd)
                for g in range(4):
                    if g % 2 == 0:
                        wsl, Bs = load_slot(win_d[l, :, PC_POOL(g // 2):PC_POOL(g // 2) + 256], KC, 256)
                        st["w"] = (wsl, Bs)
                    wsl, Bs = st["w"]
                    w = 2 << g
                    fw.op(act, lambda g=g: nc.scalar.copy(out=bufU[:, 0:16], in_=halo[:, g, :]), reads=[B_halo[g]], writes=[BU])
                    for tt in range(NTT):
                        pz, Bpz = proj_F(wsl, Bs, (g % 2) * 128, 128, tt)
                        fw.op(act, lambda pz=pz, tt=tt: nc.scalar.copy(out=bufU[:, 16 + tt * TT:16 + (tt + 1) * TT], in_=pz[:, :]),
                              reads=[Bpz], writes=[BU])
                        yield
                    fw.op(act, lambda g=g: nc.scalar.copy(out=halo[:, g, :], in_=bufU[:, MT:MT + 16]), reads=[BU], writes=[B_halo[g]])
                    cur, Bcur = bufU, BU
                    step = 1
                    pp = [(bufA, BA), (bufB, BB)]
                    k = 0
                    while step < w:
                        nxt, Bnxt = pp[k % 2]; k += 1
                        fw.op(dve, lambda cur=cur, nxt=nxt, step=step: nc.vector.tensor_tensor(nxt[:, step:EXT], cur[:, step:EXT], cur[:, 0:EXT - step], ALU.add),
                              reads=[Bcur], writes=[Bnxt])
                        cur, Bcur = nxt, Bnxt
                        step *= 2
                    fw.op(dve, lambda cur=cur, w=w: nc.vector.scalar_tensor_tensor(pooled[:, :], cur[:, 16:EXT], 1.0 / w, bufU[:, 16:EXT], ALU.mult, ALU.subtract),
                          reads=[Bcur, BU], writes=[BP])
                    if m == 0:
                        t1, Bt1 = tmpf.next()
                        fw.op(dve, lambda cur=cur, t1=t1, g=g: nc.vector.tensor_tensor(t1[:, 0:16], cur[:, 16:32], invcnt_sb[:, g, :], ALU.mult),
                              reads=[Bcur, CONST], writes=[Bt1])
                        fw.op(dve, lambda t1=t1: nc.vector.tensor_tensor(pooled[:, 0:16], t1[:, 0:16], bufU[:, 16:32], ALU.subtract),
                              reads=[Bt1, BU], writes=[BP])
                    for tt in range(NTT):
                        ts = slice(tt * TT, (tt + 1) * TT)
                        pz, Bpz = next_pz()
                        fw.op(pe, lambda pz=pz, g=g, ts=ts: nc.tensor.matmul(pz[:, :], wpool_sb[:, g, :], pooled[:, ts], start=True, stop=True),
                              reads=[BP, B_lp], writes=[Bpz])
                        fw.op(act, lambda pz=pz, g=g, ts=ts: nc.scalar.mul(mixb[:, RH + g, ts], pz[:, :], spool_sb[:, g:g + 1]),
                              reads=[Bpz, B_lp], writes=[B_mix[RH + g]])
                    yield
                toks = []
                for b in (BA, BB, BU, BP):
                    toks += b.all_tokens()
                set_prev[s] = toks
            return A, B

        def make_gla(h, s):
            st = {}

            def A():
                sd = set_prev[s]
                qd = set_view(s, 0, MT); ki = set_view(s, MT, MT); ks = set_view(s, 2 * MT, MT)
                sga = set_view(s, 3 * MT, MT); sgb = set_view(s, 4 * MT, MT)
                vtm = set_view(s, 5 * MT, NCH * DV).rearrange("p (c e) -> p c e", e=DV)
                gch = set_view(s, 5 * MT + NCH * DV, 2 * NCH).bitcast(F32)
                Bqd, Bki, Bks, Bsga, Bsgb, Bv, Bgch = (Buf(n, sd) for n in ("qd", "ki", "ks", "sga", "sgb", "gv", "gch"))
                st.update(qd=qd, ki=ki, ks=ks, sga=sga, sgb=sgb, vtm=vtm, gch=gch,
                          Bqd=Bqd, Bki=Bki, Bks=Bks, Bsga=Bsga, Bsgb=Bsgb, Bv=Bv, Bgch=Bgch)
                wsl, Bs = load_slot(win_d[l, :, PC_GQK(h):PC_GQK(h) + 192], KC, 192)
                for tt in range(NTT):
                    ts = slice(tt * TT, (tt + 1) * TT)
                    pg, Bpg = next_pz()
                    fw.op(pe, lambda pg=pg, ts=ts: nc.tensor.matmul(pg[0:DK, :], wg_sb[:, h * DK:(h + 1) * DK], st_gz[0][0:16, ts], start=True, stop=True),
                          reads=[B_lp, st_gz[1]], writes=[Bpg])
                    e1, Be1 = tmpf.next()
                    fw.op(act, lambda pg=pg, e1=e1: nc.scalar.activation(out=e1[0:DK, :], in_=pg[0:DK, :], func=AF.Exp, bias=nbg_sb[0:DK, h:h + 1], scale=-1.0),
                          reads=[Bpg, B_lp], writes=[Be1])
                    fw.op(act, lambda e1=e1: nc.scalar.activation(out=e1[0:DK, :], in_=e1[0:DK, :], func=AF.Ln, bias=1.0, scale=1.0),
                          reads=[Be1], writes=[Be1])
                    cs, Bcs = tmpf.next()
                    fw.op(dve, lambda e1=e1, cs=cs: nc.vector.tensor_tensor_scan(cs[0:DK, :], smask_sb[0:DK, 0:TT], e1[0:DK, :], 0.0, ALU.mult, ALU.add),
                          reads=[Be1, CONST], writes=[Bcs])
                    eb, Beb = tmpf.next()
                    fw.op(act, lambda eb=eb, cs=cs: nc.scalar.activation(out=eb[0:DK, :], in_=cs[0:DK, :], func=AF.Exp, scale=-1.0 / 16),
                          reads=[Bcs], writes=[Beb])
                    fw.op(act, lambda cs=cs: nc.scalar.activation(out=cs[0:DK, :], in_=cs[0:DK, :], func=AF.Exp, scale=1.0 / 16),
                          reads=[Beb], writes=[Bcs])
                    fw.op(act, lambda eb=eb, tt=tt: nc.scalar.copy(out=gch[0:DK, tt * 4:(tt + 1) * 4], in_=eb[0:DK, 127::128]),
                          reads=[Beb], writes=[Bgch])
                    yield
                    pq, Bpq = proj_F(wsl, Bs, 0, DK, tt)
                    fw.op(dve, lambda pq=pq, eb=eb, ts=ts: nc.vector.scalar_tensor_tensor(qd[0:DK, ts], pq[0:DK, :], float(DK) ** -0.5, eb[0:DK, :], ALU.mult, ALU.mult),
                          reads=[Bpq, Beb], writes=[Bqd])
                    yield
                    pk, Bpk = proj_F(wsl, Bs, DK, DK, tt)
                    fw.op(dve, lambda pk=pk, cs=cs, ts=ts: nc.vector.tensor_tensor(ki[0:DK, ts], pk[0:DK, :], cs[0:DK, :], ALU.mult),
                          reads=[Bpk, Bcs], writes=[Bki])
                    fw.op(dve, lambda eb=eb, ts=ts: nc.vector.tensor_tensor(
                        ks[0:DK, ts].rearrange("p (c i) -> p c i", i=128),
                        ki[0:DK, ts].rearrange("p (c i) -> p c i", i=128),
                        eb[0:DK, 127::128].unsqueeze(2).to_broadcast([DK, TT // 128, 128]), ALU.mult),
                        reads=[Bki, Beb], writes=[Bks])
                    yield
                wsl, Bs = load_slot(win_d[l, :, PC_GV(h):PC_GV(h) + 192], KC, 192)
                yield from proj_T(wsl, Bs, 0, DV, list(range(NCH)), lambda grp: vtm[:, grp[0]:grp[0] + len(grp), :], Bv)
                wsl, Bs = load_slot(win_d[l, :, PC_GR(h):PC_GR(h) + 192], KC, 192)
                for tt in range(NTT):
                    ts = slice(tt * TT, (tt + 1) * TT)
                    pz, Bpz = proj_F(wsl, Bs, 0, 128, tt)
                    fw.op(act, lambda pz=pz, ts=ts: nc.scalar.activation(out=sga[:, ts], in_=pz[:, :], func=AF.Silu), reads=[Bpz], writes=[Bsga])
                    yield
                    pz, Bpz = proj_F(wsl, Bs, 128, 64, tt)
                    fw.op(act, lambda pz=pz, ts=ts: nc.scalar.activation(out=sgb[0:64, ts], in_=pz[0:64, :], func=AF.Silu), reads=[Bpz], writes=[Bsgb])
                    yield

            def B():
                qd, ki, ks, sga, sgb, vtm, gch = (st[k] for k in ("qd", "ki", "ks", "sga", "sgb", "vtm", "gch"))
                Bqd, Bki, Bks, Bsga, Bsgb, Bv, Bgch = (st[k] for k in ("Bqd", "Bki", "Bks", "Bsga", "Bsgb", "Bv", "Bgch"))
                sT_r = gsT_r
                ka, kb = 10 + 2 * h, 11 + 2 * h
                pre = {}

                def emit_i(ch):
                    cs_ = slice(ch * CH, (ch + 1) * CH)
                    ti = tr_i[0] % 2; tr_i[0] += 1
                    trp = tr_ps_gla[:, ti * 128:ti * 128 + DK]
                    Btr = B_pb[PROT]
                    fw.op(pe, lambda: nc.tensor.transpose(trp, ks[0:DK, cs_], ident_sb[0:DK, 0:DK]),
                          reads=[Bks, CONST], writes=[Btr])
                    kst, Bkst = kst_r[ch % 2]
                    fw.op(act, lambda: nc.scalar.copy(out=kst[:, 0:DK], in_=trp), reads=[Btr], writes=[Bkst])
                    si = sc_i[0] % 4; sc_i[0] += 1
                    scp = pbank[PSC][:, si * 128:(si + 1) * 128]
                    fw.op(pe, lambda: nc.tensor.matmul(scp, ki[0:DK, cs_], qd[0:DK, cs_], start=True, stop=True),
                          reads=[Bki, Bqd], writes=[B_sc[si]])
                    sT, BsT = sT_r[ch % 2]
                    fw.op(dve, lambda: nc.vector.tensor_tensor(sT, scp, causal_sb[:, :], ALU.mult),
                          reads=[B_sc[si], CONST], writes=[BsT])
                    pre[ch] = (kst, Bkst, sT, BsT)

                emit_i(0)
                yield
                for grp in range(NCH // 4):
                    roA, BroA = pbank[PROA], B_pb[PROA]
                    roB, BroB = pbank[PROB], B_pb[PROB]
                    for ci in range(4):
                        ch = grp * 4 + ci
                        cs_ = slice(ch * CH, (ch + 1) * CH)
                        if ch + 1 < NCH:
                            emit_i(ch + 1)
                        kst, Bkst, sT, BsT = pre.pop(ch)
                        ra = roA[:, ci * 128:(ci + 1) * 128]
                        rb = roB[0:64, ci * 128:(ci + 1) * 128]
                        fw.op(pe, lambda ra=ra, sT=sT: nc.tensor.matmul(ra, vtm[:, ch, 0:128], sT, start=True, stop=False),
                              reads=[Bv, BsT], writes=[BroA], mark=False)
                        fw.op(pe, lambda ra=ra: nc.tensor.matmul(ra, Sgla_bf[0:DK, h, 0:128], qd[0:DK, cs_], start=False, stop=True),
                              reads=[Bv, BsT, B_Sglabf[h], Bqd], writes=[BroA])
                        fw.op(pe, lambda rb=rb, sT=sT: nc.tensor.matmul(rb, vtm[:, ch, 128:192], sT, start=True, stop=False),
                              reads=[Bv, BsT], writes=[BroB], mark=False)
                        fw.op(pe, lambda rb=rb: nc.tensor.matmul(rb, Sgla_bf[0:DK, h, 128:192], qd[0:DK, cs_], start=False, stop=True),
                              reads=[Bv, BsT, B_Sglabf[h], Bqd], writes=[BroB])
                        ui = u_i[0] % 2; u_i[0] += 1
                        up = pbank[PU][0:DK, ui * 192:(ui + 1) * 192]
                        fw.op(pe, lambda up=up, kst=kst: nc.tensor.matmul(up, kst[:, 0:DK], vtm[:, ch, :], start=True, stop=True),
                              reads=[Bkst, Bv], writes=[B_u[ui]])
                        fw.op(dve, lambda up=up, ch=ch: nc.vector.scalar_tensor_tensor(Sgla[0:DK, h, :], Sgla[0:DK, h, :], gch[0:DK, ch:ch + 1], up, ALU.mult, ALU.add),
                              reads=[B_u[ui], Bgch], writes=[B_Sgla[h]])
                        fw.op(act, lambda: nc.scalar.copy(out=Sgla_bf[0:DK, h, :], in_=Sgla[0:DK, h, :]),
                              reads=[B_Sgla[h]], writes=[B_Sglabf[h]])
                        yield
                    gs = slice(grp * TT, (grp + 1) * TT)
                    sqa, Bsqa = tmpb.next()
                    sqb, Bsqb = tmpb.next()
                    fw.op(act, lambda sqa=sqa: nc.scalar.activation(out=sqa[:], in_=roA[:, :], func=AF.Square), reads=[BroA], writes=[Bsqa])
                    fw.op(act, lambda sqb=sqb: nc.scalar.activation(out=sqb[0:64, :], in_=roB[0:64, :], func=AF.Square), reads=[BroB], writes=[Bsqb])
                    stp, Bst = pbank[PSTAT], B_pb[PSTAT]
                    fw.op(pe, lambda sqa=sqa: nc.tensor.matmul(stp[:, :], ones_sb[:, :], sqa[:], start=True, stop=False),
                          reads=[Bsqa, CONST], writes=[Bst], mark=False)
                    fw.op(pe, lambda sqb=sqb: nc.tensor.matmul(stp[:, :], ones_sb[0:64, :], sqb[0:64, :], start=False, stop=True),
                          reads=[Bsqa, Bsqb, CONST], writes=[Bst])
                    yield
                    rs, Brs = rstd_t.next()
                    fw.op(act, lambda rs=rs: nc.scalar.activation(out=rs[:], in_=stp[:, :], func=AF.Ln, bias=eps_sb[:, 0:1], scale=1.0 / DV),
                          reads=[Bst, CONST], writes=[Brs])
                    fw.op(act, lambda rs=rs: nc.scalar.activation(out=rs[:], in_=rs[:], func=AF.Exp, scale=-0.5), reads=[Brs], writes=[Brs])
                    t1, Bt1 = tmpf.next()
                    fw.op(dve, lambda t1=t1, rs=rs: nc.vector.tensor_tensor(t1[:], roA[:, :], rs[:], ALU.mult), reads=[BroA, Brs], writes=[Bt1])
                    fw.op(dve, lambda t1=t1: nc.vector.tensor_tensor(mixb[:, ka, gs], t1[:], sga[:, gs], ALU.mult), reads=[Bt1, Bsga], writes=[B_mix[ka]])
                    t2, Bt2 = tmpf.next()
                    fw.op(dve, lambda t2=t2, rs=rs: nc.vector.tensor_tensor(t2[0:64, :], roB[0:64, :], rs[0:64, :], ALU.mult), reads=[BroB, Brs], writes=[Bt2])
                    fw.op(dve, lambda t2=t2: nc.vector.tensor_tensor(mixb[0:64, kb, gs], t2[0:64, :], sgb[0:64, gs], ALU.mult), reads=[Bt2, Bsgb], writes=[B_mix[kb]])
                    yield
                toks = []
                for b in (Bqd, Bki, Bks, Bsga, Bsgb, Bv, Bgch):
                    toks += b.all_tokens()
                set_prev[s] = toks
            return A, B

        gzT = f32_view(512, MT // 2).bitcast(BF16)
        B_gz = Buf("gzT", seed)
        st_gz = (gzT, B_gz)

        def gz_A():
            for tt in range(NTT):
                ts = slice(tt * TT, (tt + 1) * TT)
                pz, Bpz = next_pz()
                mm_group(pz[0:16, :], [(wgz_sb[:, kc, :], hT[:, kc, ts]) for kc in range(KC)], reads=[B_lp, B_h[tt]], writes=[Bpz])
                fw.op(act, lambda pz=pz, ts=ts: nc.scalar.copy(out=gzT[0:16, ts], in_=pz[0:16, :]), reads=[Bpz], writes=[B_gz])
                yield

        ui_ = 0
        kinds = []
        for h in range(RH):
            units.append(make_ret(h, ui_ % 2)); ui_ += 1; kinds.append("ret")
        units.append(make_pool(ui_ % 2)); ui_ += 1; kinds.append("pool")
        gla_first = len(units)
        for h in range(GH):
            units.append(make_gla(h, ui_ % 2)); ui_ += 1; kinds.append("gla")

        if stop and stop.startswith("units"):
            units = units[:int(stop[5:].rstrip("A"))]
        NA = {"ret": 8, "pool": 0, "gla": 14}
        NB = {"ret": 15, "pool": 12, "gla": 13}

        def chain(*gens):
            for g in gens:
                yield from g

        for _ in units[0][0]():
            pass
        if stop and stop.endswith("A"):
            units = []
        for ui2 in range(len(units)):
            genB = units[ui2][1]()
            if ui2 + 1 < len(units):
                na = NA[kinds[ui2 + 1]]
                if ui2 + 1 == gla_first:
                    genA = chain(gz_A(), units[ui2 + 1][0]())
                    na += 2
                else:
                    genA = units[ui2 + 1][0]()
            else:
                genA = iter(())
                na = 0
            ratio = na / NB[kinds[ui2]]
            acc = 0.0
            for _ in genB:
                acc += ratio
                while acc >= 1.0:
                    acc -= 1.0
                    next(genA, None)
            for _ in genA:
                pass

        fence = fw.fence_tokens()
        B_x.r = list(fence)
        B_x.w = None
        for half in range(2):
            ks_ = slice(half * 8, (half + 1) * 8)
            fw.dma(sp, x_mt[:, ks_, :], src[:, tok0:tok0 + MT].rearrange("(k p) t -> p k t", p=128)[:, ks_, :],
                   reads=[B_xs[m]], writes=[B_x], sem=xs_sem)
        B_x.w = (xs_sem[2], xs_sem[0], xs_sem[1])

        if stop and (stop.startswith("units") or stop == "mixers"):
            for half in range(2):
                ks_ = slice(half * 8, (half + 1) * 8)
                fw.dma(sp, yT_d[:, tok0:tok0 + MT].rearrange("(k p) t -> p k t", p=128)[:, ks_, :], x_mt[:, ks_, :],
                       reads=[B_x], writes=[B_xs[m]], sem=xs_sem)
            return
        for sl in range(D // SLOTW):
            wsl, Bs = load_slot(wout_d[l, :, sl * SLOTW:(sl + 1) * SLOTW], NKO, SLOTW)
            for cb in range(SLOTW // 128):
                rb = sl * (SLOTW // 128) + cb
                for tt in range(NTT):
                    ts = slice(tt * TT, (tt + 1) * TT)
                    pz, Bpz = next_pz()
                    mm_group(pz[:, :], [(wsl[:, k, cb * 128:(cb + 1) * 128], mixb[:, k, ts]) for k in range(NKO)],
                             reads=[Bs] + B_mix, writes=[Bpz])
                    fw.op(dve, lambda pz=pz, rb=rb, ts=ts: nc.vector.scalar_tensor_tensor(
                        x_mt[:, rb, ts], pz[:, :], mod_sb[:, GT1 + rb:GT1 + rb + 1], x_mt[:, rb, ts], ALU.mult, ALU.add),
                        reads=[Bpz, B_mod], writes=[B_x])

        if stop == "outproj":
            for half in range(2):
                ks_ = slice(half * 8, (half + 1) * 8)
                fw.dma(sp, yT_d[:, tok0:tok0 + MT].rearrange("(k p) t -> p k t", p=128)[:, ks_, :], x_mt[:, ks_, :],
                       reads=[B_x], writes=[B_xs[m]], sem=xs_sem)
            return
        rms_to_h(gmod2, SH2)

        hid = mixb
        B_hid = B_mix
        ada_todo = []
        if l + 1 < NL:
            per = (N_ADA_SLOTS + NMT - 1) // NMT
            ada_todo = list(range(m * per, min(N_ADA_SLOTS, (m + 1) * per)))
        n_iter = 2 * (DFF // D) * (D // SLOTW)
        ada_every = max(1, n_iter // max(1, len(ada_todo))) if ada_todo else 0
        it_ctr = [0]

        def ada_tick():
            it_ctr[0] += 1
            if ada_todo and it_ctr[0] % ada_every == 0:
                ada_slot(l + 1, ada_todo.pop(0))

        for hb in range(DFF // D):
            for sl in range(D // SLOTW):
                c0 = hb * D + sl * SLOTW
                wsl, Bs = load_slot(wup_d[l, :, c0:c0 + SLOTW], KC, SLOTW)
                for cb in range(SLOTW // 128):
                    kk = sl * (SLOTW // 128) + cb
                    for tt in range(NTT):
                        ts = slice(tt * TT, (tt + 1) * TT)
                        pz, Bpz = proj_F(wsl, Bs, cb * 128, 128, tt)
                        r, Br = tmpf.next()
                        fw.op(act, lambda pz=pz, r=r: nc.scalar.activation(out=r[:], in_=pz[:, :], func=AF.Relu), reads=[Bpz], writes=[Br])
                        fw.op(dve, lambda r=r, kk=kk, ts=ts: nc.vector.tensor_tensor(hid[:, kk, ts], r[:], r[:], ALU.mult),
                              reads=[Br], writes=[B_hid[kk]])
                ada_tick()
            for sl in range(D // SLOTW):
                wsl, Bs = load_slot(wdn_d[l, hb * D:(hb + 1) * D, sl * SLOTW:(sl + 1) * SLOTW], KC, SLOTW)
                for cb in range(SLOTW // 128):
                    rb = sl * (SLOTW // 128) + cb
                    for tt in range(NTT):
                        ts = slice(tt * TT, (tt + 1) * TT)
                        pz, Bpz = next_pz()
                        mm_group(pz[:, :], [(wsl[:, k, cb * 128:(cb + 1) * 128], hid[:, k, ts]) for k in range(KC)],
                                 reads=[Bs] + B_hid[0:KC], writes=[Bpz])
                        fw.op(dve, lambda pz=pz, rb=rb, ts=ts: nc.vector.scalar_tensor_tensor(
                            x_mt[:, rb, ts], pz[:, :], mod_sb[:, GT2 + rb:GT2 + rb + 1], x_mt[:, rb, ts], ALU.mult, ALU.add),
                            reads=[Bpz, B_mod], writes=[B_x])
                ada_tick()
        while ada_todo:
            ada_slot(l + 1, ada_todo.pop(0))

        if last_layer and final_norm:
            for tt in range(NTT):
                ts = slice(tt * TT, (tt + 1) * TT)
                ss, Bss = pbank[PSTAT], B_pb[PSTAT]
                for kc in range(KC):
                    sq, Bsq = tmpb.next()
                    fw.op(act, lambda kc=kc, sq=sq: nc.scalar.activation(out=sq[:], in_=x_mt[:, kc, ts], func=AF.Square),
                          reads=[B_x], writes=[Bsq])
                    fw.op(pe, lambda kc=kc, sq=sq: nc.tensor.matmul(ss[:, :], ones_sb[:, :], sq[:], start=(kc == 0), stop=(kc == KC - 1)),
                          reads=[Bsq, CONST], writes=[Bss], mark=True)
                rs, Brs = rstd_t.next()
                fw.op(act, lambda rs=rs: nc.scalar.activation(out=rs[:], in_=ss[:, :], func=AF.Ln, bias=eps_sb[:, 0:1], scale=1.0 / D),
                      reads=[Bss, CONST], writes=[Brs])
                fw.op(act, lambda rs=rs: nc.scalar.activation(out=rs[:], in_=rs[:], func=AF.Exp, scale=-0.5), reads=[Brs], writes=[Brs])
                for kc in range(KC):
                    fw.op(dve, lambda kc=kc, rs=rs: nc.vector.scalar_tensor_tensor(x_mt[:, kc, ts], x_mt[:, kc, ts], gfin_sb[:, kc:kc + 1], rs[:], ALU.mult, ALU.mult),
                          reads=[Brs, CONST], writes=[B_x])
            dst = yT_d
        else:
            dst = yT_d if last_layer else xs_d
        for half in range(2):
            ks_ = slice(half * 8, (half + 1) * 8)
            fw.dma(sp, dst[:, tok0:tok0 + MT].rearrange("(k p) t -> p k t", p=128)[:, ks_, :], x_mt[:, ks_, :],
                   reads=[B_x], writes=[B_xs[m]], sem=xs_sem)
        B_xs[m].w = (xs_sem[2], xs_sem[0], xs_sem[1])

    for l in range(NL):
        layer_prologue(l)
        for m in range(NMT):
            run_mt(l, m)
    sp.wait(fw.fence_tokens())
    return nc


def prep_shared(inputs, NL):
    f = lambda a: np.ascontiguousarray(np.asarray(a, dtype=np.float32))
    perm = in_col_perm()
    rows = out_row_map()
    w_out = np.asarray(inputs["w_out"], dtype=np.float32)[:NL]
    w_out_p = np.zeros((NL, NKO * 128, D), np.float32)
    valid = rows >= 0
    w_out_p[:, valid, :] = w_out[:, rows[valid], :]
    sh = {
        "w_ada": f(np.asarray(inputs["w_ada"])[:NL]),
        "b_ada_fm": f(np.asarray(inputs["b_ada"])[:NL].reshape(NL, 96, 128).transpose(0, 2, 1)),
        "g_mix_fm": f(np.asarray(inputs["g_mix"])[:NL].reshape(NL, KC, 128).transpose(0, 2, 1)),
        "g_mlp_fm": f(np.asarray(inputs["g_mlp"])[:NL].reshape(NL, KC, 128).transpose(0, 2, 1)),
        "w_in_p": f(np.asarray(inputs["w_in"])[:NL][:, :, perm]),
        "w_gate_up": f(np.asarray(inputs["w_gate_up"])[:NL]),
        "w_pool": f(np.asarray(inputs["w_pool"])[:NL]),
        "s_pool_fm": f(np.asarray(inputs["s_pool"])[:NL].reshape(NL, 4, 128).transpose(0, 2, 1)),
        "w_out_p": w_out_p,
        "w_up": f(np.asarray(inputs["w_up"])[:NL]),
        "w_down": f(np.asarray(inputs["w_down"])[:NL]),
        "g_final_fm": f(np.asarray(inputs["g_final"]).reshape(KC, 128).T),
    }
    bg = np.zeros((NL, 128, GH), np.float32)
    bg[:, :DK, :] = np.asarray(inputs["b_gate"], dtype=np.float32)[:NL].reshape(NL, GH, DK).transpose(0, 2, 1)
    sh["b_gate_fm"] = bg
    return sh


def run_cores(nc, shared, x_list, c_list, pos0_list, seqstart_list):
    in_maps = []
    for x, c, p0, ss in zip(x_list, c_list, pos0_list, seqstart_list):
        m = dict(shared)
        m["xT"] = np.ascontiguousarray(np.asarray(x, dtype=np.float32).T)
        m["c_fm"] = np.ascontiguousarray(np.asarray(c, dtype=np.float32).reshape(KC, 128).T)
        m.update(host_tables(p0, x.shape[0], ss))
        in_maps.append(m)
    res = run_bass_kernel_spmd(nc, in_maps, core_ids=list(range(len(in_maps))))
    return [np.ascontiguousarray(r["yT"].T) for r in res.results]


_CACHE = {}


def kernel(x, c, w_ada, b_ada, g_mix, g_mlp, w_in, w_gate_up, b_gate, w_pool, s_pool,
           w_out, w_up, w_down, g_final):
    inputs = dict(w_ada=w_ada, b_ada=b_ada, g_mix=g_mix, g_mlp=g_mlp, w_in=w_in, w_gate_up=w_gate_up,
                  b_gate=b_gate, w_pool=w_pool, s_pool=s_pool, w_out=w_out, w_up=w_up, w_down=w_down,
                  g_final=g_final)
    x = np.asarray(x, dtype=np.float32)
    c = np.asarray(c, dtype=np.float32)
    B, S, _ = x.shape
    NL = 4
    NMT = S // MT
    key = (NL, NMT)
    if key not in _CACHE:
        _CACHE[key] = build_program(NL, NMT)
    nc = _CACHE[key]
    shared = prep_shared(inputs, NL)
    real = {0: 0, 2: 1, 4: 2, 6: 3}
    tabs = host_tables(0, S, True)
    zero_map = None
    in_maps = []
    for core in range(8):
        if core in real:
            b = real[core]
            mm = dict(shared)
            mm["xT"] = np.ascontiguousarray(x[b].T)
            mm["c_fm"] = np.ascontiguousarray(c[b].reshape(KC, 128).T)
            mm.update(tabs)
            in_maps.append(mm)
        else:
            if zero_map is None:
                ref = in_maps[0]
                zero_map = {k: np.zeros_like(v) for k, v in ref.items()}
            in_maps.append(zero_map)
    res = run_bass_kernel_spmd(nc, in_maps, core_ids=list(range(8)))
    outs = {b: np.ascontiguousarray(res.results[core]["yT"].T) for core, b in real.items()}
    return np.stack([outs[b] for b in range(B)], axis=0).astype(np.float32)
```

```python
import math
import numpy as np
import concourse.bass as bass
import concourse.mybir as mybir
from concourse.bass_utils import run_bass_kernel_spmd

F32 = mybir.dt.float32
BF16 = mybir.dt.bfloat16
AF = mybir.ActivationFunctionType
ALU = mybir.AluOpType

D = 2048
KC = 16
MT = 1024
TT = 512
NTT = MT // TT
CH = 128
NCH = MT // CH
RH = 6
GH = 4
DK = 96
DV = 192
NKO = 18
DFF = 8192
EPS = 1e-6
IN_WIDTH = 5904
SLOTW = 256


class Eng:
    def __init__(self, fw, name, eng):
        self.fw = fw
        self.name = name
        self.eng = eng
        self.sem = fw.nc.alloc_semaphore("sem_" + name)
        self.count = 0
        self.waited = {}

    def wait(self, toks):
        best = {}
        for t in toks:
            if t is None:
                continue
            key, sem, val = t
            if val <= self.waited.get(key, 0):
                continue
            if key not in best or best[key][1] < val:
                best[key] = (sem, val)
        for key, (sem, val) in best.items():
            self.eng.wait_ge(sem, val)
            self.waited[key] = val

    def mark(self, inst):
        inst.then_inc(self.sem, 1)
        self.count += 1
        return (self.name, self.sem, self.count)

    def now(self):
        if self.count == 0:
            return None
        return (self.name, self.sem, self.count)


class Buf:
    def __init__(self, name="", seed=None):
        self.name = name
        self.w = None
        self.r = list(seed) if seed else []

    def rdeps(self):
        return [self.w]

    def wdeps(self):
        return [self.w] + self.r

    def wrote(self, tok):
        self.w = tok
        self.r = []

    def read(self, tok):
        self.r.append(tok)
        if len(self.r) > 16:
            best = {}
            for t in self.r:
                if t is None:
                    continue
                if t[0] not in best or best[t[0]][2] < t[2]:
                    best[t[0]] = t
            self.r = list(best.values())

    def all_tokens(self):
        return [t for t in ([self.w] + self.r) if t is not None]


class PBuf(Buf):
    def rdeps(self):
        return self.wdeps()

    def read(self, tok):
        self.wrote(tok)


class FW:
    def __init__(self):
        self.nc = bass.Bass("TRN2", target_bir_lowering=False)
        nc = self.nc
        self.pe = Eng(self, "pe", nc.tensor)
        self.act = Eng(self, "act", nc.scalar)
        self.dve = Eng(self, "dve", nc.vector)
        self.pool = Eng(self, "pool", nc.gpsimd)
        self.sp = Eng(self, "sp", nc.sync)
        self.engs = [self.pe, self.act, self.dve, self.pool, self.sp]
        self.dsems = []

    def sb(self, name, shape, dt):
        return self.nc.alloc_sbuf_tensor(name, list(shape), dt)

    def op(self, E, fn, reads=(), writes=(), mark=True):
        deps = []
        for b in reads:
            deps += b.rdeps()
        for b in writes:
            deps += b.wdeps()
        E.wait(deps)
        inst = fn()
        if not mark:
            return None
        tok = E.mark(inst)
        for b in reads:
            b.read(tok)
        for b in writes:
            b.wrote(tok)
        return tok

    def dsem(self, name):
        s = [self.nc.alloc_semaphore(name), 0, "d_" + name]
        self.dsems.append(s)
        return s

    def dma(self, E, out_ap, in_ap, reads=(), writes=(), sem=None):
        deps = []
        for b in reads:
            deps += b.rdeps()
        for b in writes:
            deps += b.wdeps()
        E.wait(deps)
        inst = E.eng.dma_start(out=out_ap, in_=in_ap)
        sem[1] += 16
        inst.then_inc(sem[0], 16)
        tok = (sem[2], sem[0], sem[1])
        for b in reads:
            b.read(tok)
        for b in writes:
            b.wrote(tok)
        return tok

    def fence_tokens(self):
        toks = [e.now() for e in self.engs]
        toks += [(s[2], s[0], s[1]) for s in self.dsems if s[1] > 0]
        return [t for t in toks if t is not None]


class Rot:
    def __init__(self, fw, name, n, shape, dt, space="sb"):
        self.items = []
        for i in range(n):
            t = fw.sb(f"{name}{i}", shape, dt)
            self.items.append((t, Buf(f"{name}{i}")))
        self.i = 0

    def next(self):
        it = self.items[self.i % len(self.items)]
        self.i += 1
        return it


RET_W = 768
OFF_RQ, OFF_RK, OFF_RV, OFF_RG = 0, 768, 1536, 2304
OFF_PU = 3072
OFF_GQ, OFF_GK, OFF_GV, OFF_GR, OFF_GZ = 3584, 3968, 4352, 5120, 5888


def in_col_perm():
    cols = []
    for h in range(RH):
        cols += list(range(OFF_RQ + h * 128, OFF_RQ + (h + 1) * 128))
        cols += list(range(OFF_RK + h * 128, OFF_RK + (h + 1) * 128))
    for h in range(RH):
        cols += list(range(OFF_RG + h * 128, OFF_RG + (h + 1) * 128))
        cols += list(range(OFF_RV + h * 128, OFF_RV + (h + 1) * 128))
    cols += list(range(OFF_PU, OFF_PU + 512))
    for h in range(GH):
        cols += list(range(OFF_GQ + h * DK, OFF_GQ + (h + 1) * DK))
        cols += list(range(OFF_GK + h * DK, OFF_GK + (h + 1) * DK))
        cols += list(range(OFF_GV + h * DV, OFF_GV + (h + 1) * DV))
        cols += list(range(OFF_GR + h * DV, OFF_GR + (h + 1) * DV))
    cols += list(range(OFF_GZ, OFF_GZ + 16))
    assert len(cols) == IN_WIDTH and len(set(cols)) == IN_WIDTH
    return np.array(cols)


PC_RQK = lambda h: h * 256
PC_RGV = lambda h: 1536 + h * 256
PC_POOL = lambda s: 3072 + s * 256
PC_GQK = lambda h: 3584 + h * 576
PC_GV = lambda h: 3584 + h * 576 + 192
PC_GR = lambda h: 3584 + h * 576 + 384
PC_GZ = 5888


def out_row_map():
    rows = []
    for h in range(RH):
        rows += list(range(h * 128, (h + 1) * 128))
    for g in range(4):
        rows += list(range(768 + g * 128, 768 + (g + 1) * 128))
    for h in range(GH):
        base = 1280 + h * DV
        rows += list(range(base, base + 128))
        rows += list(range(base + 128, base + 192)) + [-1] * 64
    assert len(rows) == NKO * 128
    return np.array(rows)


def host_tables(pos0, ntok, seq_start):
    t = {}
    inv_freq = (10000.0 ** (-np.arange(0, 128, 2, dtype=np.float32) / np.float32(128))).astype(np.float32)
    pos = (pos0 + np.arange(ntok)).astype(np.float32)
    ang = (pos[None, :] * inv_freq[:, None]).astype(np.float32)
    cos = np.cos(ang.astype(np.float64)).astype(np.float32)
    sin = np.sin(ang.astype(np.float64)).astype(np.float32)
    t["cosT"] = np.ascontiguousarray(np.concatenate([cos, cos], 0))
    t["sinT"] = np.ascontiguousarray(np.concatenate([-sin, sin], 0))
    hh = np.arange(RH, dtype=np.float64)
    log_g = np.log1p(-np.exp2(-5.0 - hh))
    i = np.arange(128, dtype=np.float64)
    xi = np.exp((i[None, :] + 1.0) * log_g[:, None]) * (128.0 ** -0.5)
    t["xi"] = np.ascontiguousarray(np.broadcast_to(xi[None], (128, RH, 128))).astype(np.float32)
    jj = i[:, None, None]
    ii = i[None, None, :]
    d2 = np.where(jj <= ii, np.exp(-(jj + 1.0) * log_g[None, :, None]), 0.0)
    t["d2"] = np.ascontiguousarray(d2).astype(np.float32)
    zeta = np.exp((127.0 - i)[:, None] * log_g[None, :])
    t["zeta"] = np.ascontiguousarray(zeta).astype(np.float32)
    t["causal"] = (i[:, None] <= i[None, :]).astype(np.float32)
    t["ident"] = np.eye(128, dtype=np.float32)
    sw = np.zeros((128, 128), np.float32)
    for m in range(128):
        sw[(m + 64) % 128, m] = 1.0
    t["pswap"] = sw
    sm = np.ones((128, MT), np.float32)
    sm[:, ::CH] = 0.0
    t["scanmask"] = sm
    ic = np.zeros((128, 4, 16), np.float32)
    for g, w in enumerate((2, 4, 8, 16)):
        for tt in range(16):
            ic[:, g, tt] = 1.0 / (min(tt + 1, w) if seq_start else w)
    t["invcnt"] = ic
    return t


G128 = [float(np.exp(128.0 * np.log1p(-np.exp2(-5.0 - h)))) for h in range(RH)]


def build_program(NL, NMT, final_norm=True, stop=None):
    fw = FW()
    nc = fw.nc
    pe, act, dve, pool, sp = fw.pe, fw.act, fw.dve, fw.pool, fw.sp
    NTOK = NMT * MT

    def din(name, shape, dt=F32):
        return nc.dram_tensor(name, list(shape), dt, kind="ExternalInput").ap()

    xT_d = din("xT", [D, NTOK])
    c_d = din("c_fm", [128, KC])
    wada_d = din("w_ada", [NL, D, 6 * D])
    bada_d = din("b_ada_fm", [NL, 128, 96])
    gmix_d = din("g_mix_fm", [NL, 128, KC])
    gmlp_d = din("g_mlp_fm", [NL, 128, KC])
    win_d = din("w_in_p", [NL, D, IN_WIDTH])
    wg_d = din("w_gate_up", [NL, 16, 384])
    bg_d = din("b_gate_fm", [NL, 128, GH])
    wpool_d = din("w_pool", [NL, 4, 128, 128])
    spool_d = din("s_pool_fm", [NL, 128, 4])
    wout_d = din("w_out_p", [NL, NKO * 128, D])
    wup_d = din("w_up", [NL, D, DFF])
    wdn_d = din("w_down", [NL, DFF, D])
    gfin_d = din("g_final_fm", [128, KC])
    cos_d = din("cosT", [128, NTOK])
    sin_d = din("sinT", [128, NTOK])
    xi_d = din("xi", [128, RH, 128])
    d2_d = din("d2", [128, RH, 128])
    zeta_d = din("zeta", [128, RH])
    causal_d = din("causal", [128, 128])
    ident_d = din("ident", [128, 128])
    pswap_d = din("pswap", [128, 128])
    smask_d = din("scanmask", [128, MT])
    invcnt_d = din("invcnt", [128, 4, 16])
    yT_d = nc.dram_tensor("yT", [D, NTOK], F32, kind="ExternalOutput").ap()
    xs_d = nc.dram_tensor("xs", [D, NTOK], F32).ap()

    arena = fw.sb("arena", [128, KC * MT], F32)
    x_mt = arena[:, :].rearrange("p (k t) -> p k t", k=KC)
    B_x = Buf("x_mt")
    hT = fw.sb("hT", [128, KC, MT], BF16)
    B_h = [Buf(f"hT{t}") for t in range(NTT)]
    mixb = fw.sb("mix", [128, NKO, MT], BF16)
    B_mix = [Buf(f"mix{k}") for k in range(NKO)]
    NSLOT = 3
    wslot = [fw.sb(f"wslot{i}", [128, NKO, SLOTW], BF16) for i in range(NSLOT)]
    B_slot = [Buf(f"wslot{i}") for i in range(NSLOT)]
    S_slot = [fw.dsem(f"wsl{i}") for i in range(NSLOT)]
    slot_i = [0]

    cos_sb = fw.sb("cos_sb", [128, MT], F32)
    sin_sb = fw.sb("sin_sb", [128, MT], F32)
    B_cs = Buf("cossin")
    xi_sb = fw.sb("xi_sb", [128, RH, 128], F32)
    d2_sb = fw.sb("d2_sb", [128, RH, 128], F32)
    zeta_sb = fw.sb("zeta_sb", [128, RH], F32)
    causal_sb = fw.sb("causal_sb", [128, 128], F32)
    ident_sb = fw.sb("ident_sb", [128, 128], BF16)
    pswap_sb = fw.sb("pswap_sb", [128, 128], BF16)
    ones_sb = fw.sb("ones_sb", [128, 128], BF16)
    smask_sb = fw.sb("smask_sb", [128, MT], F32)
    invcnt_sb = fw.sb("invcnt_sb", [128, 4, 16], F32)
    eps_sb = fw.sb("eps_sb", [128, 1], F32)
    B_const = Buf("const")

    c_sb = fw.sb("c_sb", [128, KC], F32)
    sc_bf = fw.sb("sc_bf", [128, KC], BF16)
    gfin_sb = fw.sb("gfin_sb", [128, KC], F32)
    B_c = Buf("c")

    bada_sb = fw.sb("bada_sb", [128, 96], F32)
    gmix_sb = fw.sb("gmix_sb", [128, KC], F32)
    gmlp_sb = fw.sb("gmlp_sb", [128, KC], F32)
    wg_sb = fw.sb("wg_sb", [16, 384], BF16)
    nbg_sb = fw.sb("nbg_sb", [128, GH], F32)
    bg_sb = fw.sb("bg_sb", [128, GH], F32)
    wpool_sb = fw.sb("wpool_sb", [128, 4, 128], BF16)
    spool_sb = fw.sb("spool_sb", [128, 4], F32)
    wgz_sb = fw.sb("wgz_sb", [128, KC, 16], BF16)
    mod_sb = fw.sb("mod_sb", [128, 96], F32)
    modn_sb = fw.sb("modn_sb", [128, 96], F32)
    B_modn = Buf("modn")
    gmod1 = fw.sb("gmod1", [128, KC], F32)
    gmod2 = fw.sb("gmod2", [128, KC], F32)
    B_lp = Buf("layerparams")
    B_mod = Buf("mod")

    Sret = fw.sb("Sret", [128, RH, 128], F32)
    Sret_bf = fw.sb("Sret_bf", [128, RH, 128], BF16)
    Sgla = fw.sb("Sgla", [128, GH, DV], F32)
    Sgla_bf = fw.sb("Sgla_bf", [128, GH, DV], BF16)
    halo = fw.sb("halo", [128, 4, 16], F32)
    B_Sret = [Buf(f"Sret{h}") for h in range(RH)]
    B_Sretbf = [Buf(f"Sretbf{h}") for h in range(RH)]
    B_Sgla = [Buf(f"Sgla{h}") for h in range(GH)]
    B_Sglabf = [Buf(f"Sglabf{h}") for h in range(GH)]
    B_halo = [Buf(f"halo{g}") for g in range(4)]

    tmpf = Rot(fw, "tmpf", 3, [128, TT], F32)
    tmpb = Rot(fw, "tmpb", 2, [128, TT], BF16)
    rstd_t = Rot(fw, "rstd", 2, [128, TT], F32)

    pbank = [nc.alloc_psum_tensor(f"pb{i}", [128, 512], F32) for i in range(8)]
    B_pb = [PBuf(f"pb{i}") for i in range(8)]
    PZ = [0, 1]
    PROT, PSC, PROA, PROB, PSTAT, PU = 2, 3, 4, 5, 6, 7
    pz_i = [0]

    def next_pz():
        i = PZ[pz_i[0] % len(PZ)]
        pz_i[0] += 1
        return pbank[i], B_pb[i]

    B_sc = [B_pb[PSC]] * 4
    sc_i = [0]
    B_u = [B_pb[PU]] * 2
    u_i = [0]
    tr_ps_ret = pbank[PROB][:, 0:128].bitcast(BF16)
    tr_ps_gla = pbank[PROT][:, 0:128].bitcast(BF16)
    tr_i = [0]

    ld_sem = fw.dsem("ld")
    xs_sem = fw.dsem("xs")
    B_xs = [Buf(f"xs{m}") for m in range(NMT)]

    def mm_group(out_ap, pairs, reads, writes, flags=None):
        n = len(pairs)
        for i, (l, r) in enumerate(pairs):
            last = i == n - 1
            st = (i == 0) if flags is None else flags[0]
            sp_ = last if flags is None else (flags[1] and last)
            fw.op(pe, lambda l=l, r=r, st=st, sp_=sp_: nc.tensor.matmul(out_ap, l, r, start=st, stop=sp_),
                  reads=reads, writes=writes, mark=last)

    def load_slot(src_ap, nk, width):
        i = slot_i[0] % NSLOT
        slot_i[0] += 1
        fw.dma(pool, wslot[i][:, 0:nk, 0:width], src_ap.rearrange("(k p) c -> p k c", p=128),
               writes=[B_slot[i]], sem=S_slot[i])
        return wslot[i], B_slot[i]

    lp_sem = fw.dsem("lp")
    cs_sem = fw.dsem("cs")

    def ld(dst, src, B, eng=None, sem=None):
        fw.dma(eng or sp, dst, src, writes=[B], sem=sem or ld_sem)

    for dst, src in ((xi_sb[:], xi_d[:, :, :]), (d2_sb[:], d2_d[:, :, :]), (zeta_sb[:], zeta_d[:, :]),
                     (causal_sb[:], causal_d[:, :]), (smask_sb[:], smask_d[:, :]),
                     (invcnt_sb[:], invcnt_d[:, :, :]), (c_sb[:], c_d[:, :]), (gfin_sb[:], gfin_d[:, :])):
        ld(dst, src, B_const)
    ld(ident_sb[:], ident_d[:, :], B_const, eng=pool)
    ld(pswap_sb[:], pswap_d[:, :], B_const, eng=pool)
    fw.op(dve, lambda: nc.vector.memset(ones_sb[:], 1.0), writes=[B_const])
    fw.op(dve, lambda: nc.vector.memset(eps_sb[:], EPS), writes=[B_const])
    const_tok = (ld_sem[2], ld_sem[0], ld_sem[1])
    for e in (pe, act, dve):
        e.wait([const_tok, dve.now()])

    class _Ready(Buf):
        def rdeps(self):
            return []

        def read(self, tok):
            pass
    CONST = _Ready("CONST")
    fw.op(act, lambda: nc.scalar.activation(out=sc_bf[:], in_=c_sb[:], func=AF.Silu), reads=[CONST], writes=[B_c])
    fw.op(dve, lambda: nc.vector.memset(mixb[:], 0.0), writes=B_mix)

    N_ADA_SLOTS = 6 * D // SLOTW

    def ada_slot(l, s):
        pm, Bpm = pbank[PROT], B_pb[PROT]
        wsl, Bs = load_slot(wada_d[l, :, s * SLOTW:(s + 1) * SLOTW], KC, SLOTW)
        nb = SLOTW // 128
        for cb in range(nb):
            mm_group(pm[:, cb:cb + 1],
                     [(wsl[:, kc, cb * 128:(cb + 1) * 128], sc_bf[:, kc:kc + 1]) for kc in range(KC)],
                     reads=[Bs, B_c], writes=[Bpm])
        fw.op(act, lambda: nc.scalar.copy(out=modn_sb[:, s * nb:(s + 1) * nb], in_=pm[:, 0:nb]),
              reads=[Bpm], writes=[B_modn])

    def layer_prologue(l):
        ld(bada_sb[:], bada_d[l, :, :], B_lp, sem=lp_sem)
        ld(gmix_sb[:], gmix_d[l, :, :], B_lp, sem=lp_sem)
        ld(gmlp_sb[:], gmlp_d[l, :, :], B_lp, sem=lp_sem)
        ld(bg_sb[:], bg_d[l, :, :], B_lp, sem=lp_sem)
        ld(spool_sb[:], spool_d[l, :, :], B_lp, sem=lp_sem)
        ld(wg_sb[:], wg_d[l, :, :], B_lp, eng=pool, sem=lp_sem)
        ld(wpool_sb[:], wpool_d[l, :, :, :].rearrange("g c d -> c g d"), B_lp, eng=pool, sem=lp_sem)
        ld(wgz_sb[:], win_d[l, :, PC_GZ:PC_GZ + 16].rearrange("(k p) c -> p k c", p=128), B_lp, eng=pool, sem=lp_sem)
        B_lp.w = (lp_sem[2], lp_sem[0], lp_sem[1])
        fw.op(dve, lambda: nc.vector.tensor_scalar(nbg_sb[:], bg_sb[:], -1.0, None, ALU.mult), reads=[B_lp], writes=[B_lp])
        if l == 0:
            for s in range(N_ADA_SLOTS):
                ada_slot(0, s)
        fw.op(dve, lambda: nc.vector.tensor_tensor(mod_sb[:], modn_sb[:], bada_sb[:], ALU.add),
              reads=[B_modn, B_lp], writes=[B_mod])
        fw.op(dve, lambda: nc.vector.scalar_tensor_tensor(gmod1[:], mod_sb[:, 16:32], 1.0, gmix_sb[:], ALU.add, ALU.mult),
              reads=[B_mod, B_lp], writes=[B_mod])
        fw.op(dve, lambda: nc.vector.scalar_tensor_tensor(gmod2[:], mod_sb[:, 64:80], 1.0, gmlp_sb[:], ALU.add, ALU.mult),
              reads=[B_mod, B_lp], writes=[B_mod])
        for h in range(RH):
            fw.op(dve, lambda h=h: nc.vector.memset(Sret[:, h, :], 0.0), writes=[B_Sret[h]])
            fw.op(dve, lambda h=h: nc.vector.memset(Sret_bf[:, h, :], 0.0), writes=[B_Sretbf[h]])
        for h in range(GH):
            fw.op(dve, lambda h=h: nc.vector.memset(Sgla[:, h, :], 0.0), writes=[B_Sgla[h]])
            fw.op(dve, lambda h=h: nc.vector.memset(Sgla_bf[:, h, :], 0.0), writes=[B_Sglabf[h]])
        for g in range(4):
            fw.op(dve, lambda g=g: nc.vector.memset(halo[:, g, :], 0.0), writes=[B_halo[g]])

    SH1, SC1, GT1, SH2, SC2, GT2 = 0, 16, 32, 48, 64, 80

    def rms_to_h(gmod, shift_off):
        for tt in range(NTT):
            ts = slice(tt * TT, (tt + 1) * TT)
            ss, Bss = pbank[PSTAT], B_pb[PSTAT]
            for kc in range(KC):
                sq, Bsq = tmpb.next()
                fw.op(act, lambda kc=kc, sq=sq: nc.scalar.activation(out=sq[:], in_=x_mt[:, kc, ts], func=AF.Square),
                      reads=[B_x], writes=[Bsq])
                fw.op(pe, lambda kc=kc, sq=sq: nc.tensor.matmul(ss[:, :], ones_sb[:, :], sq[:], start=(kc == 0), stop=(kc == KC - 1)),
                      reads=[Bsq, CONST], writes=[Bss], mark=True)
            rs, Brs = rstd_t.next()
            fw.op(act, lambda: nc.scalar.activation(out=rs[:], in_=ss[:, :], func=AF.Ln, bias=eps_sb[:, 0:1], scale=1.0 / D),
                  reads=[Bss, CONST], writes=[Brs])
            fw.op(act, lambda: nc.scalar.activation(out=rs[:], in_=rs[:], func=AF.Exp, scale=-0.5),
                  reads=[Brs], writes=[Brs])
            for kc in range(KC):
                t1, Bt1 = tmpf.next()
                fw.op(dve, lambda kc=kc, t1=t1: nc.vector.scalar_tensor_tensor(t1[:], x_mt[:, kc, ts], gmod[:, kc:kc + 1], rs[:], ALU.mult, ALU.mult),
                      reads=[B_x, B_mod, Brs], writes=[Bt1])
                if shift_off is None:
                    pass
                else:
                    fw.op(act, lambda kc=kc, t1=t1: nc.scalar.activation(out=hT[:, kc, ts], in_=t1[:], func=AF.Identity,
                                                                         bias=mod_sb[:, shift_off + kc:shift_off + kc + 1], scale=1.0),
                          reads=[Bt1, B_mod], writes=[B_h[tt]])

    def proj_F(wsl, Bs, c0, M, tt, nk=KC, src=None, Bsrc=None):
        src = hT if src is None else src
        Bsrc = B_h[tt] if Bsrc is None else Bsrc
        pz, Bpz = next_pz()
        ts = slice(tt * TT, (tt + 1) * TT)
        mm_group(pz[0:M, :], [(wsl[:, kc, c0:c0 + M], src[:, kc, ts]) for kc in range(nk)],
                 reads=[Bs, Bsrc], writes=[Bpz])
        return pz, Bpz

    def proj_T(wsl, Bs, c0, N, ch_list, out_view_fn, Bout, evac_eng="act"):
        per_bank = max(1, 512 // N)
        for g0 in range(0, len(ch_list), per_bank):
            grp = ch_list[g0:g0 + per_bank]
            pz, Bpz = next_pz()
            for gi, ch in enumerate(grp):
                mm_group(pz[:, gi * N:(gi + 1) * N],
                         [(hT[:, kc, ch * CH:(ch + 1) * CH], wsl[:, kc, c0:c0 + N]) for kc in range(KC)],
                         reads=[Bs, B_h[(ch * CH) // TT]], writes=[Bpz])
            dst = out_view_fn(grp)
            srcv = pz[:, 0:len(grp) * N].rearrange("p (c n) -> p c n", n=N)
            fw.op(act, lambda dst=dst, srcv=srcv: nc.scalar.copy(out=dst, in_=srcv), reads=[Bpz], writes=[Bout])
            yield

    arena_bf = arena[:, :].bitcast(BF16)
    SET_BF = 8192

    def set_view(s, off, n):
        return arena_bf[:, s * SET_BF + off: s * SET_BF + off + n]

    def f32_view(off, n):
        return arena[:, 8192 + off: 8192 + off + n]

    def run_mt(l, m):
        tok0 = m * MT
        last_layer = (l == NL - 1)
        if stop == "prologue":
            return
        src = xT_d if l == 0 else xs_d
        fence = fw.fence_tokens()
        B_x.r += fence
        for half in range(2):
            ks = slice(half * 8, (half + 1) * 8)
            fw.dma(sp, x_mt[:, ks, :], src[:, tok0:tok0 + MT].rearrange("(k p) t -> p k t", p=128)[:, ks, :],
                   reads=[B_xs[m]], writes=[B_x], sem=xs_sem)
        B_x.w = (xs_sem[2], xs_sem[0], xs_sem[1])
        fw.dma(sp, cos_sb[:], cos_d[:, tok0:tok0 + MT], writes=[B_cs], sem=cs_sem)
        fw.dma(sp, sin_sb[:], sin_d[:, tok0:tok0 + MT], writes=[B_cs], sem=cs_sem)
        B_cs.w = (cs_sem[2], cs_sem[0], cs_sem[1])

        rms_to_h(gmod1, SH1)

        if stop == "norm1":
            return
        seed = B_x.all_tokens()
        set_prev = [list(seed), list(seed)]

        units = []
        LN_ROF = [(f32_view(1024, 512), Buf("rof0", seed)), (f32_view(1536, 512), Buf("rof1", seed))]
        LN_ROB = (f32_view(2048, 256).bitcast(BF16), Buf("rob", seed))
        LN_MEAN = (f32_view(2304, 512), Buf("lnmean", seed))
        LN_CEN = (f32_view(2816, 512), Buf("lncen", seed))
        LN_SQ = (f32_view(3328, 256).bitcast(BF16), Buf("lnsq", seed))
        LN_RS = (f32_view(3584, 512), Buf("lnrs", seed))
        kz_r = [(f32_view(0, 64).bitcast(BF16), Buf("kz0", seed)), (f32_view(64, 64).bitcast(BF16), Buf("kz1", seed))]
        sT_r = [(f32_view(128, 64).bitcast(BF16), Buf("sT0", seed)), (f32_view(192, 64).bitcast(BF16), Buf("sT1", seed))]
        kst_r = [(f32_view(256, 64).bitcast(BF16), Buf("kst0", seed)), (f32_view(320, 64).bitcast(BF16), Buf("kst1", seed))]
        gsT_r = [(f32_view(384, 64).bitcast(BF16), Buf("gsT0", seed)), (f32_view(448, 64).bitcast(BF16), Buf("gsT1", seed))]

        def make_ret(h, s):
            st = {}

            def A():
                sd = set_prev[s]
                qp = set_view(s, 0, MT); kp = set_view(s, MT, MT); sg = set_view(s, 2 * MT, MT)
                vtm = set_view(s, 3 * MT, MT).rearrange("p (c e) -> p c e", e=128)
                Bq, Bk, Bg, Bv = Buf("qp", sd), Buf("kp", sd), Buf("sg", sd), Buf("vtm", sd)
                st.update(qp=qp, kp=kp, sg=sg, vtm=vtm, Bq=Bq, Bk=Bk, Bg=Bg, Bv=Bv)
                wsl, Bs = load_slot(win_d[l, :, PC_RQK(h):PC_RQK(h) + 256], KC, 256)
                def finish(pz, Bpz, raw, Braw, ts, which):
                    pr, Bpr = pbank[PROT], B_pb[PROT]
                    fw.op(pe, lambda: nc.tensor.matmul(pr[:, :], pswap_sb[:, :], raw[:], start=True, stop=True),
                          reads=[Braw, CONST], writes=[Bpr])
                    t1, Bt1 = tmpf.next()
                    t2, Bt2 = tmpf.next()
                    fw.op(dve, lambda: nc.vector.tensor_tensor(t1[:], pz[:, :], cos_sb[:, ts], ALU.mult),
                          reads=[Bpz, B_cs], writes=[Bt1])
                    fw.op(dve, lambda: nc.vector.tensor_tensor(t2[:], pr[:, :], sin_sb[:, ts], ALU.mult),
                          reads=[Bpr, B_cs], writes=[Bt2])
                    if which == 0:
                        fw.op(dve, lambda: nc.vector.tensor_tensor(t1[:], t1[:], t2[:], ALU.add),
                              reads=[Bt2], writes=[Bt1])
                        fw.op(dve, lambda: nc.vector.tensor_tensor(
                            qp[:, ts].rearrange("p (c i) -> p c i", i=128),
                            t1[:].rearrange("p (c i) -> p c i", i=128),
                            xi_sb[:, h, :].unsqueeze(1).to_broadcast([128, TT // 128, 128]), ALU.mult),
                            reads=[Bt1, CONST], writes=[Bq])
                    else:
                        fw.op(dve, lambda: nc.vector.tensor_tensor(kp[:, ts], t1[:], t2[:], ALU.add),
                              reads=[Bt1, Bt2], writes=[Bk])

                pend = None
                for tt in range(NTT):
                    ts = slice(tt * TT, (tt + 1) * TT)
                    for which in range(2):
                        pz, Bpz = proj_F(wsl, Bs, which * 128, 128, tt)
                        raw, Braw = tmpb.next()
                        fw.op(act, lambda raw=raw, pz=pz: nc.scalar.copy(out=raw[:], in_=pz[:, :]), reads=[Bpz], writes=[Braw])
                        if pend is not None:
                            finish(*pend)
                        pend = (pz, Bpz, raw, Braw, ts, which)
                        yield
                wsl, Bs = load_slot(win_d[l, :, PC_RGV(h):PC_RGV(h) + 256], KC, 256)
                for tt in range(NTT):
                    ts = slice(tt * TT, (tt + 1) * TT)
                    pz, Bpz = proj_F(wsl, Bs, 0, 128, tt)
                    if pend is not None:
                        finish(*pend)
                        pend = None
                    fw.op(act, lambda pz=pz: nc.scalar.activation(out=sg[:, ts], in_=pz[:, :], func=AF.Silu), reads=[Bpz], writes=[Bg])
                    yield
                yield from proj_T(wsl, Bs, 128, 128, list(range(NCH)), lambda grp: vtm[:, grp[0]:grp[0] + len(grp), :], Bv)

            def B():
                qp, kp, sg, vtm = st["qp"], st["kp"], st["sg"], st["vtm"]
                Bq, Bk, Bg, Bv = st["Bq"], st["Bk"], st["Bg"], st["Bv"]
                pre = {}

                def emit_i(ch):
                    cs_ = slice(ch * CH, (ch + 1) * CH)
                    ti = tr_i[0] % 2; tr_i[0] += 1
                    trp = tr_ps_ret[:, ti * 128:(ti + 1) * 128]
                    Btr = B_pb[PROB]
                    fw.op(pe, lambda: nc.tensor.transpose(trp, kp[:, cs_], ident_sb[:, :]),
                          reads=[Bk, CONST], writes=[Btr])
                    kz, Bkz = kz_r[ch % 2]
                    fw.op(act, lambda: nc.scalar.mul(kz, trp, zeta_sb[:, h:h + 1]),
                          reads=[Btr, CONST], writes=[Bkz])
                    si = sc_i[0] % 4; sc_i[0] += 1
                    scp = pbank[PSC][:, si * 128:(si + 1) * 128]
                    fw.op(pe, lambda: nc.tensor.matmul(scp, kp[:, cs_], qp[:, cs_], start=True, stop=True),
                          reads=[Bk, Bq], writes=[B_sc[si]])
                    sT, BsT = sT_r[ch % 2]
                    fw.op(dve, lambda: nc.vector.tensor_tensor(sT, scp, d2_sb[:, h, :], ALU.mult),
                          reads=[B_sc[si], CONST], writes=[BsT])
                    pre[ch] = (kz, Bkz, sT, BsT)

                ro, Bro = pbank[PROA], B_pb[PROA]

                def ln_gen(grp):
                    gs = slice(grp * TT, (grp + 1) * TT)
                    rof, Brof = LN_ROF[grp % 2]
                    rob, Brob = LN_ROB
                    mean, Bmean = LN_MEAN
                    cen, Bcen = LN_CEN
                    sq, Bsq = LN_SQ
                    rs, Brs = LN_RS
                    stp, Bst = pbank[PSTAT], B_pb[PSTAT]
                    fw.op(act, lambda: nc.scalar.copy(out=rob, in_=ro[:, :]), reads=[Bro], writes=[Brob])
                    fw.op(act, lambda: nc.scalar.copy(out=rof, in_=ro[:, :]), reads=[Bro], writes=[Brof])
                    fw.op(pe, lambda: nc.tensor.matmul(stp[:, :], ones_sb[:, :], rob, start=True, stop=True),
                          reads=[Brob, CONST], writes=[Bst])
                    yield
                    fw.op(act, lambda: nc.scalar.mul(mean, stp[:, :], 1.0 / 128), reads=[Bst], writes=[Bmean])
                    fw.op(dve, lambda: nc.vector.tensor_tensor(cen, rof, mean, ALU.subtract),
                          reads=[Brof, Bmean], writes=[Bcen])
                    fw.op(act, lambda: nc.scalar.activation(out=sq, in_=cen, func=AF.Square), reads=[Bcen], writes=[Bsq])
                    fw.op(pe, lambda: nc.tensor.matmul(stp[:, :], ones_sb[:, :], sq, start=True, stop=True),
                          reads=[Bsq, CONST], writes=[Bst])
                    yield
                    fw.op(act, lambda: nc.scalar.activation(out=rs, in_=stp[:, :], func=AF.Ln, bias=eps_sb[:, 0:1], scale=1.0 / 128),
                          reads=[Bst, CONST], writes=[Brs])
                    fw.op(act, lambda: nc.scalar.activation(out=rs, in_=rs, func=AF.Exp, scale=-0.5), reads=[Brs], writes=[Brs])
                    fw.op(dve, lambda: nc.vector.tensor_tensor(cen, cen, rs, ALU.mult), reads=[Brs], writes=[Bcen])
                    fw.op(dve, lambda: nc.vector.tensor_tensor(mixb[:, h, gs], cen, sg[:, gs], ALU.mult),
                          reads=[Bcen, Bg], writes=[B_mix[h]])
                    yield

                pending_ln = iter(())
                emit_i(0)
                yield
                for grp in range(NCH // 4):
                    for ci in range(4):
                        ch = grp * 4 + ci
                        cs_ = slice(ch * CH, (ch + 1) * CH)
                        if ch + 1 < NCH:
                            emit_i(ch + 1)
                        kz, Bkz, sT, BsT = pre.pop(ch)
                        next(pending_ln, None)
                        rov = ro[:, ci * 128:(ci + 1) * 128]
                        fw.op(pe, lambda rov=rov, sT=sT: nc.tensor.matmul(rov, vtm[:, ch, :], sT, start=True, stop=False),
                              reads=[Bv, BsT], writes=[Bro], mark=False)
                        fw.op(pe, lambda rov=rov: nc.tensor.matmul(rov, Sret_bf[:, h, :], qp[:, cs_], start=False, stop=True),
                              reads=[Bv, BsT, B_Sretbf[h], Bq], writes=[Bro])
                        ui = u_i[0] % 2; u_i[0] += 1
                        up = pbank[PU][:, ui * 192:ui * 192 + 128]
                        fw.op(pe, lambda up=up, kz=kz: nc.tensor.matmul(up, kz, vtm[:, ch, :], start=True, stop=True),
                              reads=[Bkz, Bv], writes=[B_u[ui]])
                        fw.op(dve, lambda up=up: nc.vector.scalar_tensor_tensor(Sret[:, h, :], Sret[:, h, :], G128[h], up, ALU.mult, ALU.add),
                              reads=[B_u[ui]], writes=[B_Sret[h]])
                        fw.op(act, lambda: nc.scalar.copy(out=Sret_bf[:, h, :], in_=Sret[:, h, :]),
                              reads=[B_Sret[h]], writes=[B_Sretbf[h]])
                        yield
                    pending_ln = ln_gen(grp)
                    next(pending_ln)
                for _ in pending_ln:
                    yield
                toks = []
                for b in (Bq, Bk, Bg, Bv):
                    toks += b.all_tokens()
                set_prev[s] = toks
            return A, B

        def make_pool(s):
            st = {}

            def A():
                return
                yield

            def B():
                sd = set_prev[s]
                EXT = MT + 16
                bufA = set_view(s, 0, 2 * EXT).bitcast(F32)
                bufB = set_view(s, 2 * EXT, 2 * EXT).bitcast(F32)
                bufU = set_view(s, 4 * EXT, 2 * EXT).bitcast(F32)
                pooled = set_view(s, 6 * EXT, MT)
                BA, BB, BU, BP = Buf("pA", sd), Buf("pB", sd), Buf("pU", sd), Buf("pP", sd)
                for g in range(4):
                    if g % 2 == 0:
                        wsl, Bs = load_slot(win_d[l, :, PC_POOL(g // 2):PC_POOL(g // 2) + 256], KC, 256)
                        st["w"] = (wsl, Bs)
                    wsl, Bs = st["w"]
                    w = 2 << g
                    fw.op(act, lambda g=g: nc.scalar.copy(out=bufU[:, 0:16], in_=halo[:, g, :]), reads=[B_halo[g]], writes=[BU])
                    for tt in range(NTT):
                        pz, Bpz = proj_F(wsl, Bs, (g % 2) * 128, 128, tt)
                        fw.op(act, lambda pz=pz, tt=tt: nc.scalar.copy(out=bufU[:, 16 + tt * TT:16 + (tt + 1) * TT], in_=pz[:, :]),
                              reads=[Bpz], writes=[BU])
                        yield
                    fw.op(act, lambda g=g: nc.scalar.copy(out=halo[:, g, :], in_=bufU[:, MT:MT + 16]), reads=[BU], writes=[B_halo[g]])
                    cur, Bcur = bufU, BU
                    step = 1
                    pp = [(bufA, BA), (bufB, BB)]
                    k = 0
                    while step < w:
                        nxt, Bnxt = pp[k % 2]; k += 1
                        fw.op(dve, lambda cur=cur, nxt=nxt, step=step: nc.vector.tensor_tensor(nxt[:, step:EXT], cur[:, step:EXT], cur[:, 0:EXT - step], ALU.add),
                              reads=[Bcur], writes=[Bnxt])
                        cur, Bcur = nxt, Bnxt
                        step *= 2
                    fw.op(dve, lambda cur=cur, w=w: nc.vector.scalar_tensor_tensor(pooled[:, :], cur[:, 16:EXT], 1.0 / w, bufU[:, 16:EXT], ALU.mult, ALU.subtract),
                          reads=[Bcur, BU], writes=[BP])
                    if m == 0:
                        t1, Bt1 = tmpf.next()
                        fw.op(dve, lambda cur=cur, t1=t1, g=g: nc.vector.tensor_tensor(t1[:, 0:16], cur[:, 16:32], invcnt_sb[:, g, :], ALU.mult),
                              reads=[Bcur, CONST], writes=[Bt1])
                        fw.op(dve, lambda t1=t1: nc.vector.tensor_tensor(pooled[:, 0:16], t1[:, 0:16], bufU[:, 16:32], ALU.subtract),
                              reads=[Bt1, BU], writes=[BP])
                    for tt in range(NTT):
                        ts = slice(tt * TT, (tt + 1) * TT)
                        pz, Bpz = next_pz()
                        fw.op(pe, lambda pz=pz, g=g, ts=ts: nc.tensor.matmul(pz[:, :], wpool_sb[:, g, :], pooled[:, ts], start=True, stop=True),
                              reads=[BP, B_lp], writes=[Bpz])
                        fw.op(act, lambda pz=pz, g=g, ts=ts: nc.scalar.mul(mixb[:, RH + g, ts], pz[:, :], spool_sb[:, g:g + 1]),
                              reads=[Bpz, B_lp], writes=[B_mix[RH + g]])
                    yield
                toks = []
                for b in (BA, BB, BU, BP):
                    toks += b.all_tokens()
                set_prev[s] = toks
            return A, B

        def make_gla(h, s):
            st = {}

            def A():
                sd = set_prev[s]
                qd = set_view(s, 0, MT); ki = set_view(s, MT, MT); ks = set_view(s, 2 * MT, MT)
                sga = set_view(s, 3 * MT, MT); sgb = set_view(s, 4 * MT, MT)
                vtm = set_view(s, 5 * MT, NCH * DV).rearrange("p (c e) -> p c e", e=DV)
                gch = set_view(s, 5 * MT + NCH * DV, 2 * NCH).bitcast(F32)
                Bqd, Bki, Bks, Bsga, Bsgb, Bv, Bgch = (Buf(n, sd) for n in ("qd", "ki", "ks", "sga", "sgb", "gv", "gch"))
                st.update(qd=qd, ki=ki, ks=ks, sga=sga, sgb=sgb, vtm=vtm, gch=gch,
                          Bqd=Bqd, Bki=Bki, Bks=Bks, Bsga=Bsga, Bsgb=Bsgb, Bv=Bv, Bgch=Bgch)
                wsl, Bs = load_slot(win_d[l, :, PC_GQK(h):PC_GQK(h) + 192], KC, 192)
                for tt in range(NTT):
                    ts = slice(tt * TT, (tt + 1) * TT)
                    pg, Bpg = next_pz()
                    fw.op(pe, lambda pg=pg, ts=ts: nc.tensor.matmul(pg[0:DK, :], wg_sb[:, h * DK:(h + 1) * DK], st_gz[0][0:16, ts], start=True, stop=True),
                          reads=[B_lp, st_gz[1]], writes=[Bpg])
                    e1, Be1 = tmpf.next()
                    fw.op(act, lambda pg=pg, e1=e1: nc.scalar.activation(out=e1[0:DK, :], in_=pg[0:DK, :], func=AF.Exp, bias=nbg_sb[0:DK, h:h + 1], scale=-1.0),
                          reads=[Bpg, B_lp], writes=[Be1])
                    fw.op(act, lambda e1=e1: nc.scalar.activation(out=e1[0:DK, :], in_=e1[0:DK, :], func=AF.Ln, bias=1.0, scale=1.0),
                          reads=[Be1], writes=[Be1])
                    cs, Bcs = tmpf.next()
                    fw.op(dve, lambda e1=e1, cs=cs: nc.vector.tensor_tensor_scan(cs[0:DK, :], smask_sb[0:DK, 0:TT], e1[0:DK, :], 0.0, ALU.mult, ALU.add),
                          reads=[Be1, CONST], writes=[Bcs])
                    eb, Beb = tmpf.next()
                    fw.op(act, lambda eb=eb, cs=cs: nc.scalar.activation(out=eb[0:DK, :], in_=cs[0:DK, :], func=AF.Exp, scale=-1.0 / 16),
                          reads=[Bcs], writes=[Beb])
                    fw.op(act, lambda cs=cs: nc.scalar.activation(out=cs[0:DK, :], in_=cs[0:DK, :], func=AF.Exp, scale=1.0 / 16),
                          reads=[Beb], writes=[Bcs])
                    fw.op(act, lambda eb=eb, tt=tt: nc.scalar.copy(out=gch[0:DK, tt * 4:(tt + 1) * 4], in_=eb[0:DK, 127::128]),
                          reads=[Beb], writes=[Bgch])
                    yield
                    pq, Bpq = proj_F(wsl, Bs, 0, DK, tt)
                    fw.op(dve, lambda pq=pq, eb=eb, ts=ts: nc.vector.scalar_tensor_tensor(qd[0:DK, ts], pq[0:DK, :], float(DK) ** -0.5, eb[0:DK, :], ALU.mult, ALU.mult),
                          reads=[Bpq, Beb], writes=[Bqd])
                    yield
                    pk, Bpk = proj_F(wsl, Bs, DK, DK, tt)
                    fw.op(dve, lambda pk=pk, cs=cs, ts=ts: nc.vector.tensor_tensor(ki[0:DK, ts], pk[0:DK, :], cs[0:DK, :], ALU.mult),
                          reads=[Bpk, Bcs], writes=[Bki])
                    fw.op(dve, lambda eb=eb, ts=ts: nc.vector.tensor_tensor(
                        ks[0:DK, ts].rearrange("p (c i) -> p c i", i=128),
                        ki[0:DK, ts].rearrange("p (c i) -> p c i", i=128),
                        eb[0:DK, 127::128].unsqueeze(2).to_broadcast([DK, TT // 128, 128]), ALU.mult),
                        reads=[Bki, Beb], writes=[Bks])
                    yield
                wsl, Bs = load_slot(win_d[l, :, PC_GV(h):PC_GV(h) + 192], KC, 192)
                yield from proj_T(wsl, Bs, 0, DV, list(range(NCH)), lambda grp: vtm[:, grp[0]:grp[0] + len(grp), :], Bv)
                wsl, Bs = load_slot(win_d[l, :, PC_GR(h):PC_GR(h) + 192], KC, 192)
                for tt in range(NTT):
                    ts = slice(tt * TT, (tt + 1) * TT)
                    pz, Bpz = proj_F(wsl, Bs, 0, 128, tt)
                    fw.op(act, lambda pz=pz, ts=ts: nc.scalar.activation(out=sga[:, ts], in_=pz[:, :], func=AF.Silu), reads=[Bpz], writes=[Bsga])
                    yield
                    pz, Bpz = proj_F(wsl, Bs, 128, 64, tt)
                    fw.op(act, lambda pz=pz, ts=ts: nc.scalar.activation(out=sgb[0:64, ts], in_=pz[0:64, :], func=AF.Silu), reads=[Bpz], writes=[Bsgb])
                    yield

            def B():
                qd, ki, ks, sga, sgb, vtm, gch = (st[k] for k in ("qd", "ki", "ks", "sga", "sgb", "vtm", "gch"))
                Bqd, Bki, Bks, Bsga, Bsgb, Bv, Bgch = (st[k] for k in ("Bqd", "Bki", "Bks", "Bsga", "Bsgb", "Bv", "Bgch"))
                sT_r = gsT_r
                ka, kb = 10 + 2 * h, 11 + 2 * h
                pre = {}

                def emit_i(ch):
                    cs_ = slice(ch * CH, (ch + 1) * CH)
                    ti = tr_i[0] % 2; tr_i[0] += 1
                    trp = tr_ps_gla[:, ti * 128:ti * 128 + DK]
                    Btr = B_pb[PROT]
                    fw.op(pe, lambda: nc.tensor.transpose(trp, ks[0:DK, cs_], ident_sb[0:DK, 0:DK]),
                          reads=[Bks, CONST], writes=[Btr])
                    kst, Bkst = kst_r[ch % 2]
                    fw.op(act, lambda: nc.scalar.copy(out=kst[:, 0:DK], in_=trp), reads=[Btr], writes=[Bkst])
                    si = sc_i[0] % 4; sc_i[0] += 1
                    scp = pbank[PSC][:, si * 128:(si + 1) * 128]
                    fw.op(pe, lambda: nc.tensor.matmul(scp, ki[0:DK, cs_], qd[0:DK, cs_], start=True, stop=True),
                          reads=[Bki, Bqd], writes=[B_sc[si]])
                    sT, BsT = sT_r[ch % 2]
                    fw.op(dve, lambda: nc.vector.tensor_tensor(sT, scp, causal_sb[:, :], ALU.mult),
                          reads=[B_sc[si], CONST], writes=[BsT])
                    pre[ch] = (kst, Bkst, sT, BsT)

                emit_i(0)
                yield
                for grp in range(NCH // 4):
                    roA, BroA = pbank[PROA], B_pb[PROA]
                    roB, BroB = pbank[PROB], B_pb[PROB]
                    for ci in range(4):
                        ch = grp * 4 + ci
                        cs_ = slice(ch * CH, (ch + 1) * CH)
                        if ch + 1 < NCH:
                            emit_i(ch + 1)
                        kst, Bkst, sT, BsT = pre.pop(ch)
                        ra = roA[:, ci * 128:(ci + 1) * 128]
                        rb = roB[0:64, ci * 128:(ci + 1) * 128]
                        fw.op(pe, lambda ra=ra, sT=sT: nc.tensor.matmul(ra, vtm[:, ch, 0:128], sT, start=True, stop=False),
                              reads=[Bv, BsT], writes=[BroA], mark=False)
                        fw.op(pe, lambda ra=ra: nc.tensor.matmul(ra, Sgla_bf[0:DK, h, 0:128], qd[0:DK, cs_], start=False, stop=True),
                              reads=[Bv, BsT, B_Sglabf[h], Bqd], writes=[BroA])
                        fw.op(pe, lambda rb=rb, sT=sT: nc.tensor.matmul(rb, vtm[:, ch, 128:192], sT, start=True, stop=False),
                              reads=[Bv, BsT], writes=[BroB], mark=False)
                        fw.op(pe, lambda rb=rb: nc.tensor.matmul(rb, Sgla_bf[0:DK, h, 128:192], qd[0:DK, cs_], start=False, stop=True),
                              reads=[Bv, BsT, B_Sglabf[h], Bqd], writes=[BroB])
                        ui = u_i[0] % 2; u_i[0] += 1
                        up = pbank[PU][0:DK, ui * 192:(ui + 1) * 192]
                        fw.op(pe, lambda up=up, kst=kst: nc.tensor.matmul(up, kst[:, 0:DK], vtm[:, ch, :], start=True, stop=True),
                              reads=[Bkst, Bv], writes=[B_u[ui]])
                        fw.op(dve, lambda up=up, ch=ch: nc.vector.scalar_tensor_tensor(Sgla[0:DK, h, :], Sgla[0:DK, h, :], gch[0:DK, ch:ch + 1], up, ALU.mult, ALU.add),
                              reads=[B_u[ui], Bgch], writes=[B_Sgla[h]])
                        fw.op(act, lambda: nc.scalar.copy(out=Sgla_bf[0:DK, h, :], in_=Sgla[0:DK, h, :]),
                              reads=[B_Sgla[h]], writes=[B_Sglabf[h]])
                        yield
                    gs = slice(grp * TT, (grp + 1) * TT)
                    sqa, Bsqa = tmpb.next()
                    sqb, Bsqb = tmpb.next()
                    fw.op(act, lambda sqa=sqa: nc.scalar.activation(out=sqa[:], in_=roA[:, :], func=AF.Square), reads=[BroA], writes=[Bsqa])
                    fw.op(act, lambda sqb=sqb: nc.scalar.activation(out=sqb[0:64, :], in_=roB[0:64, :], func=AF.Square), reads=[BroB], writes=[Bsqb])
                    stp, Bst = pbank[PSTAT], B_pb[PSTAT]
                    fw.op(pe, lambda sqa=sqa: nc.tensor.matmul(stp[:, :], ones_sb[:, :], sqa[:], start=True, stop=False),
                          reads=[Bsqa, CONST], writes=[Bst], mark=False)
                    fw.op(pe, lambda sqb=sqb: nc.tensor.matmul(stp[:, :], ones_sb[0:64, :], sqb[0:64, :], start=False, stop=True),
                          reads=[Bsqa, Bsqb, CONST], writes=[Bst])
                    yield
                    rs, Brs = rstd_t.next()
                    fw.op(act, lambda rs=rs: nc.scalar.activation(out=rs[:], in_=stp[:, :], func=AF.Ln, bias=eps_sb[:, 0:1], scale=1.0 / DV),
                          reads=[Bst, CONST], writes=[Brs])
                    fw.op(act, lambda rs=rs: nc.scalar.activation(out=rs[:], in_=rs[:], func=AF.Exp, scale=-0.5), reads=[Brs], writes=[Brs])
                    t1, Bt1 = tmpf.next()
                    fw.op(dve, lambda t1=t1, rs=rs: nc.vector.tensor_tensor(t1[:], roA[:, :], rs[:], ALU.mult), reads=[BroA, Brs], writes=[Bt1])
                    fw.op(dve, lambda t1=t1: nc.vector.tensor_tensor(mixb[:, ka, gs], t1[:], sga[:, gs], ALU.mult), reads=[Bt1, Bsga], writes=[B_mix[ka]])
                    t2, Bt2 = tmpf.next()
                    fw.op(dve, lambda t2=t2, rs=rs: nc.vector.tensor_tensor(t2[0:64, :], roB[0:64, :], rs[0:64, :], ALU.mult), reads=[BroB, Brs], writes=[Bt2])
                    fw.op(dve, lambda t2=t2: nc.vector.tensor_tensor(mixb[0:64, kb, gs], t2[0:64, :], sgb[0:64, gs], ALU.mult), reads=[Bt2, Bsgb], writes=[B_mix[kb]])
                    yield
                toks = []
                for b in (Bqd, Bki, Bks, Bsga, Bsgb, Bv, Bgch):
                    toks += b.all_tokens()
                set_prev[s] = toks
            return A, B

        gzT = f32_view(512, MT // 2).bitcast(BF16)
        B_gz = Buf("gzT", seed)
        st_gz = (gzT, B_gz)

        def gz_A():
            for tt in range(NTT):
                ts = slice(tt * TT, (tt + 1) * TT)
                pz, Bpz = next_pz()
                mm_group(pz[0:16, :], [(wgz_sb[:, kc, :], hT[:, kc, ts]) for kc in range(KC)], reads=[B_lp, B_h[tt]], writes=[Bpz])
                fw.op(act, lambda pz=pz, ts=ts: nc.scalar.copy(out=gzT[0:16, ts], in_=pz[0:16, :]), reads=[Bpz], writes=[B_gz])
                yield

        ui_ = 0
        kinds = []
        for h in range(RH):
            units.append(make_ret(h, ui_ % 2)); ui_ += 1; kinds.append("ret")
        units.append(make_pool(ui_ % 2)); ui_ += 1; kinds.append("pool")
        gla_first = len(units)
        for h in range(GH):
            units.append(make_gla(h, ui_ % 2)); ui_ += 1; kinds.append("gla")

        if stop and stop.startswith("units"):
            units = units[:int(stop[5:].rstrip("A"))]
        NA = {"ret": 8, "pool": 0, "gla": 14}
        NB = {"ret": 11, "pool": 12, "gla": 13}

        def chain(*gens):
            for g in gens:
                yield from g

        for _ in units[0][0]():
            pass
        if stop and stop.endswith("A"):
            units = []
        for ui2 in range(len(units)):
            genB = units[ui2][1]()
            if ui2 + 1 < len(units):
                na = NA[kinds[ui2 + 1]]
                if ui2 + 1 == gla_first:
                    genA = chain(gz_A(), units[ui2 + 1][0]())
                    na += 2
                else:
                    genA = units[ui2 + 1][0]()
            else:
                genA = iter(())
                na = 0
            ratio = na / NB[kinds[ui2]]
            acc = 0.0
            for _ in genB:
                acc += ratio
                while acc >= 1.0:
                    acc -= 1.0
                    next(genA, None)
            for _ in genA:
                pass

        fence = fw.fence_tokens()
        B_x.r = list(fence)
        B_x.w = None
        for half in range(2):
            ks_ = slice(half * 8, (half + 1) * 8)
            fw.dma(sp, x_mt[:, ks_, :], src[:, tok0:tok0 + MT].rearrange("(k p) t -> p k t", p=128)[:, ks_, :],
                   reads=[B_xs[m]], writes=[B_x], sem=xs_sem)
        B_x.w = (xs_sem[2], xs_sem[0], xs_sem[1])

        if stop and (stop.startswith("units") or stop == "mixers"):
            for half in range(2):
                ks_ = slice(half * 8, (half + 1) * 8)
                fw.dma(sp, yT_d[:, tok0:tok0 + MT].rearrange("(k p) t -> p k t", p=128)[:, ks_, :], x_mt[:, ks_, :],
                       reads=[B_x], writes=[B_xs[m]], sem=xs_sem)
            return
        for sl in range(D // SLOTW):
            wsl, Bs = load_slot(wout_d[l, :, sl * SLOTW:(sl + 1) * SLOTW], NKO, SLOTW)
            for cb in range(SLOTW // 128):
                rb = sl * (SLOTW // 128) + cb
                for tt in range(NTT):
                    ts = slice(tt * TT, (tt + 1) * TT)
                    pz, Bpz = next_pz()
                    mm_group(pz[:, :], [(wsl[:, k, cb * 128:(cb + 1) * 128], mixb[:, k, ts]) for k in range(NKO)],
                             reads=[Bs] + B_mix, writes=[Bpz])
                    fw.op(dve, lambda pz=pz, rb=rb, ts=ts: nc.vector.scalar_tensor_tensor(
                        x_mt[:, rb, ts], pz[:, :], mod_sb[:, GT1 + rb:GT1 + rb + 1], x_mt[:, rb, ts], ALU.mult, ALU.add),
                        reads=[Bpz, B_mod], writes=[B_x])

        if stop == "outproj":
            for half in range(2):
                ks_ = slice(half * 8, (half + 1) * 8)
                fw.dma(sp, yT_d[:, tok0:tok0 + MT].rearrange("(k p) t -> p k t", p=128)[:, ks_, :], x_mt[:, ks_, :],
                       reads=[B_x], writes=[B_xs[m]], sem=xs_sem)
            return
        rms_to_h(gmod2, SH2)

        hid = mixb
        B_hid = B_mix
        ada_todo = []
        if l + 1 < NL:
            per = (N_ADA_SLOTS + NMT - 1) // NMT
            ada_todo = list(range(m * per, min(N_ADA_SLOTS, (m + 1) * per)))
        n_iter = 2 * (DFF // D) * (D // SLOTW)
        ada_every = max(1, n_iter // max(1, len(ada_todo))) if ada_todo else 0
        it_ctr = [0]

        def ada_tick():
            it_ctr[0] += 1
            if ada_todo and it_ctr[0] % ada_every == 0:
                ada_slot(l + 1, ada_todo.pop(0))

        for hb in range(DFF // D):
            for sl in range(D // SLOTW):
                c0 = hb * D + sl * SLOTW
                wsl, Bs = load_slot(wup_d[l, :, c0:c0 + SLOTW], KC, SLOTW)
                for cb in range(SLOTW // 128):
                    kk = sl * (SLOTW // 128) + cb
                    for tt in range(NTT):
                        ts = slice(tt * TT, (tt + 1) * TT)
                        pz, Bpz = proj_F(wsl, Bs, cb * 128, 128, tt)
                        r, Br = tmpf.next()
                        fw.op(act, lambda pz=pz, r=r: nc.scalar.activation(out=r[:], in_=pz[:, :], func=AF.Relu), reads=[Bpz], writes=[Br])
                        fw.op(dve, lambda r=r, kk=kk, ts=ts: nc.vector.tensor_tensor(hid[:, kk, ts], r[:], r[:], ALU.mult),
                              reads=[Br], writes=[B_hid[kk]])
                ada_tick()
            for sl in range(D // SLOTW):
                wsl, Bs = load_slot(wdn_d[l, hb * D:(hb + 1) * D, sl * SLOTW:(sl + 1) * SLOTW], KC, SLOTW)
                for cb in range(SLOTW // 128):
                    rb = sl * (SLOTW // 128) + cb
                    for tt in range(NTT):
                        ts = slice(tt * TT, (tt + 1) * TT)
                        pz, Bpz = next_pz()
                        mm_group(pz[:, :], [(wsl[:, k, cb * 128:(cb + 1) * 128], hid[:, k, ts]) for k in range(KC)],
                                 reads=[Bs] + B_hid[0:KC], writes=[Bpz])
                        fw.op(dve, lambda pz=pz, rb=rb, ts=ts: nc.vector.scalar_tensor_tensor(
                            x_mt[:, rb, ts], pz[:, :], mod_sb[:, GT2 + rb:GT2 + rb + 1], x_mt[:, rb, ts], ALU.mult, ALU.add),
                            reads=[Bpz, B_mod], writes=[B_x])
                ada_tick()
        while ada_todo:
            ada_slot(l + 1, ada_todo.pop(0))

        if last_layer and final_norm:
            for tt in range(NTT):
                ts = slice(tt * TT, (tt + 1) * TT)
                ss, Bss = pbank[PSTAT], B_pb[PSTAT]
                for kc in range(KC):
                    sq, Bsq = tmpb.next()
                    fw.op(act, lambda kc=kc, sq=sq: nc.scalar.activation(out=sq[:], in_=x_mt[:, kc, ts], func=AF.Square),
                          reads=[B_x], writes=[Bsq])
                    fw.op(pe, lambda kc=kc, sq=sq: nc.tensor.matmul(ss[:, :], ones_sb[:, :], sq[:], start=(kc == 0), stop=(kc == KC - 1)),
                          reads=[Bsq, CONST], writes=[Bss], mark=True)
                rs, Brs = rstd_t.next()
                fw.op(act, lambda rs=rs: nc.scalar.activation(out=rs[:], in_=ss[:, :], func=AF.Ln, bias=eps_sb[:, 0:1], scale=1.0 / D),
                      reads=[Bss, CONST], writes=[Brs])
                fw.op(act, lambda rs=rs: nc.scalar.activation(out=rs[:], in_=rs[:], func=AF.Exp, scale=-0.5), reads=[Brs], writes=[Brs])
                for kc in range(KC):
                    fw.op(dve, lambda kc=kc, rs=rs: nc.vector.scalar_tensor_tensor(x_mt[:, kc, ts], x_mt[:, kc, ts], gfin_sb[:, kc:kc + 1], rs[:], ALU.mult, ALU.mult),
                          reads=[Brs, CONST], writes=[B_x])
            dst = yT_d
        else:
            dst = yT_d if last_layer else xs_d
        for half in range(2):
            ks_ = slice(half * 8, (half + 1) * 8)
            fw.dma(sp, dst[:, tok0:tok0 + MT].rearrange("(k p) t -> p k t", p=128)[:, ks_, :], x_mt[:, ks_, :],
                   reads=[B_x], writes=[B_xs[m]], sem=xs_sem)
        B_xs[m].w = (xs_sem[2], xs_sem[0], xs_sem[1])

    for l in range(NL):
        layer_prologue(l)
        for m in range(NMT):
            run_mt(l, m)
    sp.wait(fw.fence_tokens())
    return nc


def prep_shared(inputs, NL):
    f = lambda a: np.ascontiguousarray(np.asarray(a, dtype=np.float32))
    perm = in_col_perm()
    rows = out_row_map()
    w_out = np.asarray(inputs["w_out"], dtype=np.float32)[:NL]
    w_out_p = np.zeros((NL, NKO * 128, D), np.float32)
    valid = rows >= 0
    w_out_p[:, valid, :] = w_out[:, rows[valid], :]
    sh = {
        "w_ada": f(np.asarray(inputs["w_ada"])[:NL]),
        "b_ada_fm": f(np.asarray(inputs["b_ada"])[:NL].reshape(NL, 96, 128).transpose(0, 2, 1)),
        "g_mix_fm": f(np.asarray(inputs["g_mix"])[:NL].reshape(NL, KC, 128).transpose(0, 2, 1)),
        "g_mlp_fm": f(np.asarray(inputs["g_mlp"])[:NL].reshape(NL, KC, 128).transpose(0, 2, 1)),
        "w_in_p": f(np.asarray(inputs["w_in"])[:NL][:, :, perm]),
        "w_gate_up": f(np.asarray(inputs["w_gate_up"])[:NL]),
        "w_pool": f(np.asarray(inputs["w_pool"])[:NL]),
        "s_pool_fm": f(np.asarray(inputs["s_pool"])[:NL].reshape(NL, 4, 128).transpose(0, 2, 1)),
        "w_out_p": w_out_p,
        "w_up": f(np.asarray(inputs["w_up"])[:NL]),
        "w_down": f(np.asarray(inputs["w_down"])[:NL]),
        "g_final_fm": f(np.asarray(inputs["g_final"]).reshape(KC, 128).T),
    }
    bg = np.zeros((NL, 128, GH), np.float32)
    bg[:, :DK, :] = np.asarray(inputs["b_gate"], dtype=np.float32)[:NL].reshape(NL, GH, DK).transpose(0, 2, 1)
    sh["b_gate_fm"] = bg
    return sh


def run_cores(nc, shared, x_list, c_list, pos0_list, seqstart_list):
    in_maps = []
    for x, c, p0, ss in zip(x_list, c_list, pos0_list, seqstart_list):
        m = dict(shared)
        m["xT"] = np.ascontiguousarray(np.asarray(x, dtype=np.float32).T)
        m["c_fm"] = np.ascontiguousarray(np.asarray(c, dtype=np.float32).reshape(KC, 128).T)
        m.update(host_tables(p0, x.shape[0], ss))
        in_maps.append(m)
    res = run_bass_kernel_spmd(nc, in_maps, core_ids=list(range(len(in_maps))))
    return [np.ascontiguousarray(r["yT"].T) for r in res.results]


_CACHE = {}


def kernel(x, c, w_ada, b_ada, g_mix, g_mlp, w_in, w_gate_up, b_gate, w_pool, s_pool,
           w_out, w_up, w_down, g_final):
    inputs = dict(w_ada=w_ada, b_ada=b_ada, g_mix=g_mix, g_mlp=g_mlp, w_in=w_in, w_gate_up=w_gate_up,
                  b_gate=b_gate, w_pool=w_pool, s_pool=s_pool, w_out=w_out, w_up=w_up, w_down=w_down,
                  g_final=g_final)
    x = np.asarray(x, dtype=np.float32)
    c = np.asarray(c, dtype=np.float32)
    B, S, _ = x.shape
    NL = 4
    NMT = S // MT
    key = (NL, NMT)
    if key not in _CACHE:
        _CACHE[key] = build_program(NL, NMT)
    nc = _CACHE[key]
    shared = prep_shared(inputs, NL)
    real = {0: 0, 2: 1, 4: 2, 6: 3}
    tabs = host_tables(0, S, True)
    zero_map = None
    in_maps = []
    for core in range(8):
        if core in real:
            b = real[core]
            mm = dict(shared)
            mm["xT"] = np.ascontiguousarray(x[b].T)
            mm["c_fm"] = np.ascontiguousarray(c[b].reshape(KC, 128).T)
            mm.update(tabs)
            in_maps.append(mm)
        else:
            if zero_map is None:
                ref = in_maps[0]
                zero_map = {k: np.zeros_like(v) for k, v in ref.items()}
            in_maps.append(zero_map)
    res = run_bass_kernel_spmd(nc, in_maps, core_ids=list(range(8)))
    outs = {b: np.ascontiguousarray(res.results[core]["yT"].T) for core, b in real.items()}
    return np.stack([outs[b] for b in range(B)], axis=0).astype(np.float32)
```

```python
import math
import numpy as np
import concourse.bass as bass
import concourse.mybir as mybir
from concourse.bass_utils import run_bass_kernel_spmd

F32 = mybir.dt.float32
BF16 = mybir.dt.bfloat16
AF = mybir.ActivationFunctionType
ALU = mybir.AluOpType

D = 2048
KC = 16
MT = 1024
TT = 512
NTT = MT // TT
CH = 128
NCH = MT // CH
RH = 6
GH = 4
DK = 96
DV = 192
NKO = 18
DFF = 8192
EPS = 1e-6
IN_WIDTH = 5904
SLOTW = 256


class Eng:
    def __init__(self, fw, name, eng):
        self.fw = fw
        self.name = name
        self.eng = eng
        self.sem = fw.nc.alloc_semaphore("sem_" + name)
        self.count = 0
        self.waited = {}

    def wait(self, toks):
        best = {}
        for t in toks:
            if t is None:
                continue
            key, sem, val = t
            if val <= self.waited.get(key, 0):
                continue
            if key == "pe" and self.name == "pe":
                continue
            if key not in best or best[key][1] < val:
                best[key] = (sem, val)
        for key, (sem, val) in best.items():
            self.eng.wait_ge(sem, val)
            self.waited[key] = val

    def mark(self, inst):
        inst.then_inc(self.sem, 1)
        self.count += 1
        return (self.name, self.sem, self.count)

    def now(self):
        if self.count == 0:
            return None
        return (self.name, self.sem, self.count)


class Buf:
    def __init__(self, name="", seed=None):
        self.name = name
        self.w = None
        self.r = list(seed) if seed else []

    def rdeps(self):
        return [self.w]

    def wdeps(self):
        return [self.w] + self.r

    def wrote(self, tok):
        self.w = tok
        self.r = []

    def read(self, tok):
        self.r.append(tok)
        if len(self.r) > 16:
            best = {}
            for t in self.r:
                if t is None:
                    continue
                if t[0] not in best or best[t[0]][2] < t[2]:
                    best[t[0]] = t
            self.r = list(best.values())

    def all_tokens(self):
        return [t for t in ([self.w] + self.r) if t is not None]


class PBuf(Buf):
    def rdeps(self):
        return self.wdeps()

    def read(self, tok):
        self.wrote(tok)


class FW:
    def __init__(self):
        self.nc = bass.Bass("TRN2", target_bir_lowering=False)
        nc = self.nc
        self.pe = Eng(self, "pe", nc.tensor)
        self.act = Eng(self, "act", nc.scalar)
        self.dve = Eng(self, "dve", nc.vector)
        self.pool = Eng(self, "pool", nc.gpsimd)
        self.sp = Eng(self, "sp", nc.sync)
        self.engs = [self.pe, self.act, self.dve, self.pool, self.sp]
        self.dsems = []

    def sb(self, name, shape, dt):
        return self.nc.alloc_sbuf_tensor(name, list(shape), dt)

    def op(self, E, fn, reads=(), writes=(), mark=True):
        deps = []
        for b in reads:
            deps += b.rdeps()
        for b in writes:
            deps += b.wdeps()
        E.wait(deps)
        inst = fn()
        if not mark:
            return None
        tok = E.mark(inst)
        for b in reads:
            b.read(tok)
        for b in writes:
            b.wrote(tok)
        return tok

    def dsem(self, name):
        s = [self.nc.alloc_semaphore(name), 0, "d_" + name]
        self.dsems.append(s)
        return s

    def dma(self, E, out_ap, in_ap, reads=(), writes=(), sem=None):
        deps = []
        for b in reads:
            deps += b.rdeps()
        for b in writes:
            deps += b.wdeps()
        E.wait(deps)
        inst = E.eng.dma_start(out=out_ap, in_=in_ap)
        sem[1] += 16
        inst.then_inc(sem[0], 16)
        tok = (sem[2], sem[0], sem[1])
        for b in reads:
            b.read(tok)
        for b in writes:
            b.wrote(tok)
        return tok

    def fence_tokens(self):
        toks = [e.now() for e in self.engs]
        toks += [(s[2], s[0], s[1]) for s in self.dsems if s[1] > 0]
        return [t for t in toks if t is not None]


class Rot:
    def __init__(self, fw, name, n, shape, dt, space="sb"):
        self.items = []
        for i in range(n):
            t = fw.sb(f"{name}{i}", shape, dt)
            self.items.append((t, Buf(f"{name}{i}")))
        self.i = 0

    def next(self):
        it = self.items[self.i % len(self.items)]
        self.i += 1
        return it


RET_W = 768
OFF_RQ, OFF_RK, OFF_RV, OFF_RG = 0, 768, 1536, 2304
OFF_PU = 3072
OFF_GQ, OFF_GK, OFF_GV, OFF_GR, OFF_GZ = 3584, 3968, 4352, 5120, 5888


def in_col_perm():
    cols = []
    for h in range(RH):
        cols += list(range(OFF_RQ + h * 128, OFF_RQ + (h + 1) * 128))
        cols += list(range(OFF_RK + h * 128, OFF_RK + (h + 1) * 128))
    for h in range(RH):
        cols += list(range(OFF_RG + h * 128, OFF_RG + (h + 1) * 128))
        cols += list(range(OFF_RV + h * 128, OFF_RV + (h + 1) * 128))
    cols += list(range(OFF_PU, OFF_PU + 512))
    for h in range(GH):
        cols += list(range(OFF_GQ + h * DK, OFF_GQ + (h + 1) * DK))
        cols += list(range(OFF_GK + h * DK, OFF_GK + (h + 1) * DK))
        cols += list(range(OFF_GV + h * DV, OFF_GV + (h + 1) * DV))
        cols += list(range(OFF_GR + h * DV, OFF_GR + (h + 1) * DV))
    cols += list(range(OFF_GZ, OFF_GZ + 16))
    assert len(cols) == IN_WIDTH and len(set(cols)) == IN_WIDTH
    return np.array(cols)


PC_RQK = lambda h: h * 256
PC_RGV = lambda h: 1536 + h * 256
PC_POOL = lambda s: 3072 + s * 256
PC_GQK = lambda h: 3584 + h * 576
PC_GV = lambda h: 3584 + h * 576 + 192
PC_GR = lambda h: 3584 + h * 576 + 384
PC_GZ = 5888


def out_row_map():
    rows = []
    for h in range(RH):
        rows += list(range(h * 128, (h + 1) * 128))
    for g in range(4):
        rows += list(range(768 + g * 128, 768 + (g + 1) * 128))
    for h in range(GH):
        base = 1280 + h * DV
        rows += list(range(base, base + 128))
        rows += list(range(base + 128, base + 192)) + [-1] * 64
    assert len(rows) == NKO * 128
    return np.array(rows)


def host_tables(pos0, ntok, seq_start):
    t = {}
    inv_freq = (10000.0 ** (-np.arange(0, 128, 2, dtype=np.float32) / np.float32(128))).astype(np.float32)
    pos = (pos0 + np.arange(ntok)).astype(np.float32)
    ang = (pos[None, :] * inv_freq[:, None]).astype(np.float32)
    cos = np.cos(ang.astype(np.float64)).astype(np.float32)
    sin = np.sin(ang.astype(np.float64)).astype(np.float32)
    t["cosT"] = np.ascontiguousarray(np.concatenate([cos, cos], 0))
    t["sinT"] = np.ascontiguousarray(np.concatenate([-sin, sin], 0))
    hh = np.arange(RH, dtype=np.float64)
    log_g = np.log1p(-np.exp2(-5.0 - hh))
    i = np.arange(128, dtype=np.float64)
    xi = np.exp((i[None, :] + 1.0) * log_g[:, None]) * (128.0 ** -0.5)
    t["xi"] = np.ascontiguousarray(np.broadcast_to(xi[None], (128, RH, 128))).astype(np.float32)
    jj = i[:, None, None]
    ii = i[None, None, :]
    d2 = np.where(jj <= ii, np.exp(-(jj + 1.0) * log_g[None, :, None]), 0.0)
    t["d2"] = np.ascontiguousarray(d2).astype(np.float32)
    zeta = np.exp((127.0 - i)[:, None] * log_g[None, :])
    t["zeta"] = np.ascontiguousarray(zeta).astype(np.float32)
    t["causal"] = (i[:, None] <= i[None, :]).astype(np.float32)
    t["ident"] = np.eye(128, dtype=np.float32)
    sw = np.zeros((128, 128), np.float32)
    for m in range(128):
        sw[(m + 64) % 128, m] = 1.0
    t["pswap"] = sw
    sm = np.ones((128, MT), np.float32)
    sm[:, ::CH] = 0.0
    t["scanmask"] = sm
    ic = np.zeros((128, 4, 16), np.float32)
    for g, w in enumerate((2, 4, 8, 16)):
        for tt in range(16):
            ic[:, g, tt] = 1.0 / (min(tt + 1, w) if seq_start else w)
    t["invcnt"] = ic
    return t


G128 = [float(np.exp(128.0 * np.log1p(-np.exp2(-5.0 - h)))) for h in range(RH)]


def build_program(NL, NMT, final_norm=True, stop=None):
    fw = FW()
    nc = fw.nc
    pe, act, dve, pool, sp = fw.pe, fw.act, fw.dve, fw.pool, fw.sp
    NTOK = NMT * MT

    def din(name, shape, dt=F32):
        return nc.dram_tensor(name, list(shape), dt, kind="ExternalInput").ap()

    xT_d = din("xT", [D, NTOK])
    c_d = din("c_fm", [128, KC])
    wada_d = din("w_ada", [NL, D, 6 * D])
    bada_d = din("b_ada_fm", [NL, 128, 96])
    gmix_d = din("g_mix_fm", [NL, 128, KC])
    gmlp_d = din("g_mlp_fm", [NL, 128, KC])
    win_d = din("w_in_p", [NL, D, IN_WIDTH])
    wg_d = din("w_gate_up", [NL, 16, 384])
    bg_d = din("b_gate_fm", [NL, 128, GH])
    wpool_d = din("w_pool", [NL, 4, 128, 128])
    spool_d = din("s_pool_fm", [NL, 128, 4])
    wout_d = din("w_out_p", [NL, NKO * 128, D])
    wup_d = din("w_up", [NL, D, DFF])
    wdn_d = din("w_down", [NL, DFF, D])
    gfin_d = din("g_final_fm", [128, KC])
    cos_d = din("cosT", [128, NTOK])
    sin_d = din("sinT", [128, NTOK])
    xi_d = din("xi", [128, RH, 128])
    d2_d = din("d2", [128, RH, 128])
    zeta_d = din("zeta", [128, RH])
    causal_d = din("causal", [128, 128])
    ident_d = din("ident", [128, 128])
    pswap_d = din("pswap", [128, 128])
    smask_d = din("scanmask", [128, MT])
    invcnt_d = din("invcnt", [128, 4, 16])
    yT_d = nc.dram_tensor("yT", [D, NTOK], F32, kind="ExternalOutput").ap()
    xs_d = nc.dram_tensor("xs", [D, NTOK], F32).ap()

    arena = fw.sb("arena", [128, KC * MT], F32)
    x_mt = arena[:, :].rearrange("p (k t) -> p k t", k=KC)
    B_x = Buf("x_mt")
    hT = fw.sb("hT", [128, KC, MT], BF16)
    B_h = [Buf(f"hT{t}") for t in range(NTT)]
    mixb = fw.sb("mix", [128, NKO, MT], BF16)
    B_mix = [Buf(f"mix{k}") for k in range(NKO)]
    NSLOT = 3
    wslot = [fw.sb(f"wslot{i}", [128, NKO, SLOTW], BF16) for i in range(NSLOT)]
    B_slot = [Buf(f"wslot{i}") for i in range(NSLOT)]
    S_slot = [fw.dsem(f"wsl{i}") for i in range(NSLOT)]
    slot_i = [0]

    cos_sb = fw.sb("cos_sb", [128, MT], F32)
    sin_sb = fw.sb("sin_sb", [128, MT], F32)
    B_cs = Buf("cossin")
    xi_sb = fw.sb("xi_sb", [128, RH, 128], F32)
    d2_sb = fw.sb("d2_sb", [128, RH, 128], F32)
    zeta_sb = fw.sb("zeta_sb", [128, RH], F32)
    causal_sb = fw.sb("causal_sb", [128, 128], F32)
    ident_sb = fw.sb("ident_sb", [128, 128], BF16)
    pswap_sb = fw.sb("pswap_sb", [128, 128], BF16)
    ones_sb = fw.sb("ones_sb", [128, 128], BF16)
    smask_sb = fw.sb("smask_sb", [128, MT], F32)
    invcnt_sb = fw.sb("invcnt_sb", [128, 4, 16], F32)
    eps_sb = fw.sb("eps_sb", [128, 1], F32)
    B_const = Buf("const")

    c_sb = fw.sb("c_sb", [128, KC], F32)
    sc_bf = fw.sb("sc_bf", [128, KC], BF16)
    gfin_sb = fw.sb("gfin_sb", [128, KC], F32)
    B_c = Buf("c")

    bada_sb = fw.sb("bada_sb", [128, 96], F32)
    gmix_sb = fw.sb("gmix_sb", [128, KC], F32)
    gmlp_sb = fw.sb("gmlp_sb", [128, KC], F32)
    wg_sb = fw.sb("wg_sb", [16, 384], BF16)
    nbg_sb = fw.sb("nbg_sb", [128, GH], F32)
    bg_sb = fw.sb("bg_sb", [128, GH], F32)
    wpool_sb = fw.sb("wpool_sb", [128, 4, 128], BF16)
    spool_sb = fw.sb("spool_sb", [128, 4], F32)
    wgz_sb = fw.sb("wgz_sb", [128, KC, 16], BF16)
    mod_sb = fw.sb("mod_sb", [128, 96], F32)
    modn_sb = fw.sb("modn_sb", [128, 96], F32)
    B_modn = Buf("modn")
    gmod1 = fw.sb("gmod1", [128, KC], F32)
    gmod2 = fw.sb("gmod2", [128, KC], F32)
    B_lp = Buf("layerparams")
    B_mod = Buf("mod")

    Sret = fw.sb("Sret", [128, RH, 128], F32)
    Sret_bf = fw.sb("Sret_bf", [128, RH, 128], BF16)
    Sgla = fw.sb("Sgla", [128, GH, DV], F32)
    Sgla_bf = fw.sb("Sgla_bf", [128, GH, DV], BF16)
    halo = fw.sb("halo", [128, 4, 16], F32)
    B_Sret = [Buf(f"Sret{h}") for h in range(RH)]
    B_Sretbf = [Buf(f"Sretbf{h}") for h in range(RH)]
    B_Sgla = [Buf(f"Sgla{h}") for h in range(GH)]
    B_Sglabf = [Buf(f"Sglabf{h}") for h in range(GH)]
    B_halo = [Buf(f"halo{g}") for g in range(4)]

    tmpf = Rot(fw, "tmpf", 3, [128, TT], F32)
    tmpb = Rot(fw, "tmpb", 2, [128, TT], BF16)
    sqpool = Rot(fw, "sqp", 4, [128, TT], BF16)
    rstd_t = Rot(fw, "rstd", 2, [128, TT], F32)

    pbank = [nc.alloc_psum_tensor(f"pb{i}", [128, 512], F32) for i in range(8)]
    B_pb = [PBuf(f"pb{i}") for i in range(8)]
    PZ = [0, 1]
    PROT, PSC, PROA, PROB, PSTAT, PU = 2, 3, 4, 5, 6, 7
    pz_i = [0]

    def next_pz():
        i = PZ[pz_i[0] % len(PZ)]
        pz_i[0] += 1
        return pbank[i], B_pb[i]

    B_sc = [B_pb[PSC]] * 4
    sc_i = [0]
    B_u = [B_pb[PU]] * 2
    u_i = [0]
    tr_ps_ret = pbank[PROB][:, 0:128].bitcast(BF16)
    tr_ps_gla = pbank[PROT][:, 0:128].bitcast(BF16)
    tr_i = [0]

    ld_sem = fw.dsem("ld")
    xs_sem = fw.dsem("xs")
    B_xs = [Buf(f"xs{m}") for m in range(NMT)]

    def mm_group(out_ap, pairs, reads, writes, flags=None):
        n = len(pairs)
        for i, (l, r) in enumerate(pairs):
            last = i == n - 1
            st = (i == 0) if flags is None else flags[0]
            sp_ = last if flags is None else (flags[1] and last)
            fw.op(pe, lambda l=l, r=r, st=st, sp_=sp_: nc.tensor.matmul(out_ap, l, r, start=st, stop=sp_),
                  reads=reads, writes=writes, mark=last)

    def load_slot(src_ap, nk, width):
        i = slot_i[0] % NSLOT
        slot_i[0] += 1
        fw.dma(pool, wslot[i][:, 0:nk, 0:width], src_ap.rearrange("(k p) c -> p k c", p=128),
               writes=[B_slot[i]], sem=S_slot[i])
        return wslot[i], B_slot[i]

    lp_sem = fw.dsem("lp")
    cs_sem = fw.dsem("cs")

    def ld(dst, src, B, eng=None, sem=None):
        fw.dma(eng or sp, dst, src, writes=[B], sem=sem or ld_sem)

    for dst, src in ((xi_sb[:], xi_d[:, :, :]), (d2_sb[:], d2_d[:, :, :]), (zeta_sb[:], zeta_d[:, :]),
                     (causal_sb[:], causal_d[:, :]), (smask_sb[:], smask_d[:, :]),
                     (invcnt_sb[:], invcnt_d[:, :, :]), (c_sb[:], c_d[:, :]), (gfin_sb[:], gfin_d[:, :])):
        ld(dst, src, B_const)
    ld(ident_sb[:], ident_d[:, :], B_const, eng=pool)
    ld(pswap_sb[:], pswap_d[:, :], B_const, eng=pool)
    fw.op(dve, lambda: nc.vector.memset(ones_sb[:], 1.0), writes=[B_const])
    fw.op(dve, lambda: nc.vector.memset(eps_sb[:], EPS), writes=[B_const])
    const_tok = (ld_sem[2], ld_sem[0], ld_sem[1])
    for e in (pe, act, dve):
        e.wait([const_tok, dve.now()])

    class _Ready(Buf):
        def rdeps(self):
            return []

        def read(self, tok):
            pass
    CONST = _Ready("CONST")
    fw.op(act, lambda: nc.scalar.activation(out=sc_bf[:], in_=c_sb[:], func=AF.Silu), reads=[CONST], writes=[B_c])
    fw.op(dve, lambda: nc.vector.memset(mixb[:], 0.0), writes=B_mix)

    N_ADA_SLOTS = 6 * D // SLOTW

    def ada_slot(l, s):
        pm, Bpm = pbank[PROT], B_pb[PROT]
        wsl, Bs = load_slot(wada_d[l, :, s * SLOTW:(s + 1) * SLOTW], KC, SLOTW)
        nb = SLOTW // 128
        for cb in range(nb):
            mm_group(pm[:, cb:cb + 1],
                     [(wsl[:, kc, cb * 128:(cb + 1) * 128], sc_bf[:, kc:kc + 1]) for kc in range(KC)],
                     reads=[Bs, B_c], writes=[Bpm])
        fw.op(act, lambda: nc.scalar.copy(out=modn_sb[:, s * nb:(s + 1) * nb], in_=pm[:, 0:nb]),
              reads=[Bpm], writes=[B_modn])

    def layer_prologue(l):
        ld(bada_sb[:], bada_d[l, :, :], B_lp, sem=lp_sem)
        ld(gmix_sb[:], gmix_d[l, :, :], B_lp, sem=lp_sem)
        ld(gmlp_sb[:], gmlp_d[l, :, :], B_lp, sem=lp_sem)
        ld(bg_sb[:], bg_d[l, :, :], B_lp, sem=lp_sem)
        ld(spool_sb[:], spool_d[l, :, :], B_lp, sem=lp_sem)
        ld(wg_sb[:], wg_d[l, :, :], B_lp, eng=pool, sem=lp_sem)
        ld(wpool_sb[:], wpool_d[l, :, :, :].rearrange("g c d -> c g d"), B_lp, eng=pool, sem=lp_sem)
        ld(wgz_sb[:], win_d[l, :, PC_GZ:PC_GZ + 16].rearrange("(k p) c -> p k c", p=128), B_lp, eng=pool, sem=lp_sem)
        B_lp.w = (lp_sem[2], lp_sem[0], lp_sem[1])
        fw.op(dve, lambda: nc.vector.tensor_scalar(nbg_sb[:], bg_sb[:], -1.0, None, ALU.mult), reads=[B_lp], writes=[B_lp])
        if l == 0:
            for s in range(N_ADA_SLOTS):
                ada_slot(0, s)
        fw.op(dve, lambda: nc.vector.tensor_tensor(mod_sb[:], modn_sb[:], bada_sb[:], ALU.add),
              reads=[B_modn, B_lp], writes=[B_mod])
        fw.op(dve, lambda: nc.vector.scalar_tensor_tensor(gmod1[:], mod_sb[:, 16:32], 1.0, gmix_sb[:], ALU.add, ALU.mult),
              reads=[B_mod, B_lp], writes=[B_mod])
        fw.op(dve, lambda: nc.vector.scalar_tensor_tensor(gmod2[:], mod_sb[:, 64:80], 1.0, gmlp_sb[:], ALU.add, ALU.mult),
              reads=[B_mod, B_lp], writes=[B_mod])
        for h in range(RH):
            fw.op(dve, lambda h=h: nc.vector.memset(Sret[:, h, :], 0.0), writes=[B_Sret[h]])
            fw.op(dve, lambda h=h: nc.vector.memset(Sret_bf[:, h, :], 0.0), writes=[B_Sretbf[h]])
        for h in range(GH):
            fw.op(dve, lambda h=h: nc.vector.memset(Sgla[:, h, :], 0.0), writes=[B_Sgla[h]])
            fw.op(dve, lambda h=h: nc.vector.memset(Sgla_bf[:, h, :], 0.0), writes=[B_Sglabf[h]])
        for g in range(4):
            fw.op(dve, lambda g=g: nc.vector.memset(halo[:, g, :], 0.0), writes=[B_halo[g]])

    SH1, SC1, GT1, SH2, SC2, GT2 = 0, 16, 32, 48, 64, 80

    def rms_to_h(gmod, shift_off):
        for tt in range(NTT):
            ts = slice(tt * TT, (tt + 1) * TT)
            ss, Bss = pbank[PSTAT], B_pb[PSTAT]
            for kc in range(KC):
                sq, Bsq = sqpool.next()
                if kc % 2 == 0:
                    fw.op(act, lambda kc=kc, sq=sq: nc.scalar.activation(out=sq[:], in_=x_mt[:, kc, ts], func=AF.Square),
                          reads=[B_x], writes=[Bsq])
                else:
                    fw.op(dve, lambda kc=kc, sq=sq: nc.vector.tensor_tensor(sq[:], x_mt[:, kc, ts], x_mt[:, kc, ts], ALU.mult),
                          reads=[B_x], writes=[Bsq])
                fw.op(pe, lambda kc=kc, sq=sq: nc.tensor.matmul(ss[:, :], ones_sb[:, :], sq[:], start=(kc == 0), stop=(kc == KC - 1)),
                      reads=[Bsq, CONST], writes=[Bss], mark=True)
            rs, Brs = rstd_t.next()
            fw.op(act, lambda: nc.scalar.activation(out=rs[:], in_=ss[:, :], func=AF.Ln, bias=eps_sb[:, 0:1], scale=1.0 / D),
                  reads=[Bss, CONST], writes=[Brs])
            fw.op(act, lambda: nc.scalar.activation(out=rs[:], in_=rs[:], func=AF.Exp, scale=-0.5),
                  reads=[Brs], writes=[Brs])
            for kc in range(KC):
                t1, Bt1 = tmpf.next()
                fw.op(dve, lambda kc=kc, t1=t1: nc.vector.scalar_tensor_tensor(t1[:], x_mt[:, kc, ts], gmod[:, kc:kc + 1], rs[:], ALU.mult, ALU.mult),
                      reads=[B_x, B_mod, Brs], writes=[Bt1])
                if shift_off is None:
                    pass
                else:
                    fw.op(act, lambda kc=kc, t1=t1: nc.scalar.activation(out=hT[:, kc, ts], in_=t1[:], func=AF.Identity,
                                                                         bias=mod_sb[:, shift_off + kc:shift_off + kc + 1], scale=1.0),
                          reads=[Bt1, B_mod], writes=[B_h[tt]])

    def proj_F(wsl, Bs, c0, M, tt, nk=KC, src=None, Bsrc=None):
        src = hT if src is None else src
        Bsrc = B_h[tt] if Bsrc is None else Bsrc
        pz, Bpz = next_pz()
        ts = slice(tt * TT, (tt + 1) * TT)
        mm_group(pz[0:M, :], [(wsl[:, kc, c0:c0 + M], src[:, kc, ts]) for kc in range(nk)],
                 reads=[Bs, Bsrc], writes=[Bpz])
        return pz, Bpz

    def proj_T(wsl, Bs, c0, N, ch_list, out_view_fn, Bout, evac_eng="act"):
        per_bank = max(1, 512 // N)
        for g0 in range(0, len(ch_list), per_bank):
            grp = ch_list[g0:g0 + per_bank]
            pz, Bpz = next_pz()
            for gi, ch in enumerate(grp):
                mm_group(pz[:, gi * N:(gi + 1) * N],
                         [(hT[:, kc, ch * CH:(ch + 1) * CH], wsl[:, kc, c0:c0 + N]) for kc in range(KC)],
                         reads=[Bs, B_h[(ch * CH) // TT]], writes=[Bpz])
            dst = out_view_fn(grp)
            srcv = pz[:, 0:len(grp) * N].rearrange("p (c n) -> p c n", n=N)
            fw.op(act, lambda dst=dst, srcv=srcv: nc.scalar.copy(out=dst, in_=srcv), reads=[Bpz], writes=[Bout])
            yield

    arena_bf = arena[:, :].bitcast(BF16)
    SET_BF = 8192

    def set_view(s, off, n):
        return arena_bf[:, s * SET_BF + off: s * SET_BF + off + n]

    def f32_view(off, n):
        return arena[:, 8192 + off: 8192 + off + n]

    def run_mt(l, m):
        tok0 = m * MT
        last_layer = (l == NL - 1)
        if stop == "prologue":
            return
        src = xT_d if l == 0 else xs_d
        fence = fw.fence_tokens()
        B_x.r += fence
        for half in range(2):
            ks = slice(half * 8, (half + 1) * 8)
            fw.dma(sp, x_mt[:, ks, :], src[:, tok0:tok0 + MT].rearrange("(k p) t -> p k t", p=128)[:, ks, :],
                   reads=[B_xs[m]], writes=[B_x], sem=xs_sem)
        B_x.w = (xs_sem[2], xs_sem[0], xs_sem[1])
        fw.dma(sp, cos_sb[:], cos_d[:, tok0:tok0 + MT], writes=[B_cs], sem=cs_sem)
        fw.dma(sp, sin_sb[:], sin_d[:, tok0:tok0 + MT], writes=[B_cs], sem=cs_sem)
        B_cs.w = (cs_sem[2], cs_sem[0], cs_sem[1])

        rms_to_h(gmod1, SH1)

        if stop == "norm1":
            return
        seed = B_x.all_tokens()
        set_prev = [list(seed), list(seed)]

        units = []
        LN_ROF = [(f32_view(1024, 512), Buf("rof0", seed)), (f32_view(1536, 512), Buf("rof1", seed))]
        LN_ROB = (f32_view(2048, 256).bitcast(BF16), Buf("rob", seed))
        LN_MEAN = (f32_view(2304, 512), Buf("lnmean", seed))
        LN_CEN = (f32_view(2816, 512), Buf("lncen", seed))
        LN_SQ = (f32_view(3328, 256).bitcast(BF16), Buf("lnsq", seed))
        LN_RS = (f32_view(3584, 512), Buf("lnrs", seed))
        kz_r = [(f32_view(0, 64).bitcast(BF16), Buf("kz0", seed)), (f32_view(64, 64).bitcast(BF16), Buf("kz1", seed))]
        sT_r = [(f32_view(128, 64).bitcast(BF16), Buf("sT0", seed)), (f32_view(192, 64).bitcast(BF16), Buf("sT1", seed))]
        kst_r = [(f32_view(256, 64).bitcast(BF16), Buf("kst0", seed)), (f32_view(320, 64).bitcast(BF16), Buf("kst1", seed))]
        gsT_r = [(f32_view(384, 64).bitcast(BF16), Buf("gsT0", seed)), (f32_view(448, 64).bitcast(BF16), Buf("gsT1", seed))]

        def make_ret(h, s):
            st = {}

            def A():
                sd = set_prev[s]
                qp = set_view(s, 0, MT); kp = set_view(s, MT, MT); sg = set_view(s, 2 * MT, MT)
                vtm = set_view(s, 3 * MT, MT).rearrange("p (c e) -> p c e", e=128)
                Bq, Bk, Bg, Bv = Buf("qp", sd), Buf("kp", sd), Buf("sg", sd), Buf("vtm", sd)
                st.update(qp=qp, kp=kp, sg=sg, vtm=vtm, Bq=Bq, Bk=Bk, Bg=Bg, Bv=Bv)
                wsl, Bs = load_slot(win_d[l, :, PC_RQK(h):PC_RQK(h) + 256], KC, 256)
                def finish(pz, Bpz, raw, Braw, ts, which):
                    pr, Bpr = pbank[PROT], B_pb[PROT]
                    fw.op(pe, lambda: nc.tensor.matmul(pr[:, :], pswap_sb[:, :], raw[:], start=True, stop=True),
                          reads=[Braw, CONST], writes=[Bpr])
                    t1, Bt1 = tmpf.next()
                    t2, Bt2 = tmpf.next()
                    fw.op(dve, lambda: nc.vector.tensor_tensor(t1[:], pz[:, :], cos_sb[:, ts], ALU.mult),
                          reads=[Bpz, B_cs], writes=[Bt1])
                    fw.op(dve, lambda: nc.vector.tensor_tensor(t2[:], pr[:, :], sin_sb[:, ts], ALU.mult),
                          reads=[Bpr, B_cs], writes=[Bt2])
                    if which == 0:
                        fw.op(dve, lambda: nc.vector.tensor_tensor(t1[:], t1[:], t2[:], ALU.add),
                              reads=[Bt2], writes=[Bt1])
                        fw.op(dve, lambda: nc.vector.tensor_tensor(
                            qp[:, ts].rearrange("p (c i) -> p c i", i=128),
                            t1[:].rearrange("p (c i) -> p c i", i=128),
                            xi_sb[:, h, :].unsqueeze(1).to_broadcast([128, TT // 128, 128]), ALU.mult),
                            reads=[Bt1, CONST], writes=[Bq])
                    else:
                        fw.op(dve, lambda: nc.vector.tensor_tensor(kp[:, ts], t1[:], t2[:], ALU.add),
                              reads=[Bt1, Bt2], writes=[Bk])

                pend = None
                for tt in range(NTT):
                    ts = slice(tt * TT, (tt + 1) * TT)
                    for which in range(2):
                        pz, Bpz = proj_F(wsl, Bs, which * 128, 128, tt)
                        raw, Braw = tmpb.next()
                        fw.op(act, lambda raw=raw, pz=pz: nc.scalar.copy(out=raw[:], in_=pz[:, :]), reads=[Bpz], writes=[Braw])
                        if pend is not None:
                            finish(*pend)
                        pend = (pz, Bpz, raw, Braw, ts, which)
                        yield
                wsl, Bs = load_slot(win_d[l, :, PC_RGV(h):PC_RGV(h) + 256], KC, 256)
                for tt in range(NTT):
                    ts = slice(tt * TT, (tt + 1) * TT)
                    pz, Bpz = proj_F(wsl, Bs, 0, 128, tt)
                    if pend is not None:
                        finish(*pend)
                        pend = None
                    fw.op(act, lambda pz=pz: nc.scalar.activation(out=sg[:, ts], in_=pz[:, :], func=AF.Silu), reads=[Bpz], writes=[Bg])
                    yield
                yield from proj_T(wsl, Bs, 128, 128, list(range(NCH)), lambda grp: vtm[:, grp[0]:grp[0] + len(grp), :], Bv)

            def B():
                qp, kp, sg, vtm = st["qp"], st["kp"], st["sg"], st["vtm"]
                Bq, Bk, Bg, Bv = st["Bq"], st["Bk"], st["Bg"], st["Bv"]
                pre = {}

                def emit_i(ch):
                    cs_ = slice(ch * CH, (ch + 1) * CH)
                    ti = tr_i[0] % 2; tr_i[0] += 1
                    trp = tr_ps_ret[:, ti * 128:(ti + 1) * 128]
                    Btr = B_pb[PROB]
                    fw.op(pe, lambda: nc.tensor.transpose(trp, kp[:, cs_], ident_sb[:, :]),
                          reads=[Bk, CONST], writes=[Btr])
                    kz, Bkz = kz_r[ch % 2]
                    fw.op(act, lambda: nc.scalar.mul(kz, trp, zeta_sb[:, h:h + 1]),
                          reads=[Btr, CONST], writes=[Bkz])
                    si = sc_i[0] % 4; sc_i[0] += 1
                    scp = pbank[PSC][:, si * 128:(si + 1) * 128]
                    fw.op(pe, lambda: nc.tensor.matmul(scp, kp[:, cs_], qp[:, cs_], start=True, stop=True),
                          reads=[Bk, Bq], writes=[B_sc[si]])
                    sT, BsT = sT_r[ch % 2]
                    fw.op(dve, lambda: nc.vector.tensor_tensor(sT, scp, d2_sb[:, h, :], ALU.mult),
                          reads=[B_sc[si], CONST], writes=[BsT])
                    pre[ch] = (kz, Bkz, sT, BsT)

                ro, Bro = pbank[PROA], B_pb[PROA]

                def ln_gen(grp):
                    gs = slice(grp * TT, (grp + 1) * TT)
                    rof, Brof = LN_ROF[grp % 2]
                    rob, Brob = LN_ROB
                    mean, Bmean = LN_MEAN
                    cen, Bcen = LN_CEN
                    sq, Bsq = LN_SQ
                    rs, Brs = LN_RS
                    stp, Bst = pbank[PSTAT], B_pb[PSTAT]
                    fw.op(act, lambda: nc.scalar.copy(out=rob, in_=ro[:, :]), reads=[Bro], writes=[Brob])
                    fw.op(act, lambda: nc.scalar.copy(out=rof, in_=ro[:, :]), reads=[Bro], writes=[Brof])
                    fw.op(pe, lambda: nc.tensor.matmul(stp[:, :], ones_sb[:, :], rob, start=True, stop=True),
                          reads=[Brob, CONST], writes=[Bst])
                    yield
                    fw.op(act, lambda: nc.scalar.mul(mean, stp[:, :], 1.0 / 128), reads=[Bst], writes=[Bmean])
                    fw.op(dve, lambda: nc.vector.tensor_tensor(cen, rof, mean, ALU.subtract),
                          reads=[Brof, Bmean], writes=[Bcen])
                    fw.op(act, lambda: nc.scalar.activation(out=sq, in_=cen, func=AF.Square), reads=[Bcen], writes=[Bsq])
                    fw.op(pe, lambda: nc.tensor.matmul(stp[:, :], ones_sb[:, :], sq, start=True, stop=True),
                          reads=[Bsq, CONST], writes=[Bst])
                    yield
                    fw.op(act, lambda: nc.scalar.activation(out=rs, in_=stp[:, :], func=AF.Ln, bias=eps_sb[:, 0:1], scale=1.0 / 128),
                          reads=[Bst, CONST], writes=[Brs])
                    fw.op(act, lambda: nc.scalar.activation(out=rs, in_=rs, func=AF.Exp, scale=-0.5), reads=[Brs], writes=[Brs])
                    fw.op(dve, lambda: nc.vector.tensor_tensor(cen, cen, rs, ALU.mult), reads=[Brs], writes=[Bcen])
                    fw.op(dve, lambda: nc.vector.tensor_tensor(mixb[:, h, gs], cen, sg[:, gs], ALU.mult),
                          reads=[Bcen, Bg], writes=[B_mix[h]])
                    yield

                pending_ln = iter(())
                emit_i(0)
                yield
                for grp in range(NCH // 4):
                    for ci in range(4):
                        ch = grp * 4 + ci
                        cs_ = slice(ch * CH, (ch + 1) * CH)
                        if ch + 1 < NCH:
                            emit_i(ch + 1)
                        kz, Bkz, sT, BsT = pre.pop(ch)
                        next(pending_ln, None)
                        rov = ro[:, ci * 128:(ci + 1) * 128]
                        fw.op(pe, lambda rov=rov, sT=sT: nc.tensor.matmul(rov, vtm[:, ch, :], sT, start=True, stop=False),
                              reads=[Bv, BsT], writes=[Bro], mark=False)
                        fw.op(pe, lambda rov=rov: nc.tensor.matmul(rov, Sret_bf[:, h, :], qp[:, cs_], start=False, stop=True),
                              reads=[Bv, BsT, B_Sretbf[h], Bq], writes=[Bro])
                        ui = u_i[0] % 2; u_i[0] += 1
                        up = pbank[PU][:, ui * 192:ui * 192 + 128]
                        fw.op(pe, lambda up=up, kz=kz: nc.tensor.matmul(up, kz, vtm[:, ch, :], start=True, stop=True),
                              reads=[Bkz, Bv], writes=[B_u[ui]])
                        fw.op(dve, lambda up=up: nc.vector.scalar_tensor_tensor(Sret[:, h, :], Sret[:, h, :], G128[h], up, ALU.mult, ALU.add),
                              reads=[B_u[ui]], writes=[B_Sret[h]])
                        fw.op(act, lambda: nc.scalar.copy(out=Sret_bf[:, h, :], in_=Sret[:, h, :]),
                              reads=[B_Sret[h]], writes=[B_Sretbf[h]])
                        yield
                    pending_ln = ln_gen(grp)
                    next(pending_ln)
                for _ in pending_ln:
                    yield
                toks = []
                for b in (Bq, Bk, Bg, Bv):
                    toks += b.all_tokens()
                set_prev[s] = toks
            return A, B

        def make_pool(s):
            st = {}

            def A():
                return
                yield

            def B():
                sd = set_prev[s]
                EXT = MT + 16
                bufA = set_view(s, 0, 2 * EXT).bitcast(F32)
                bufB = set_view(s, 2 * EXT, 2 * EXT).bitcast(F32)
                bufU = set_view(s, 4 * EXT, 2 * EXT).bitcast(F32)
                pooled = set_view(s, 6 * EXT, MT)
                BA, BB, BU, BP = Buf("pA", sd), Buf("pB", sd), Buf("pU", sd), Buf("pP", sd)
                for g in range(4):
                    if g % 2 == 0:
                        wsl, Bs = load_slot(win_d[l, :, PC_POOL(g // 2):PC_POOL(g // 2) + 256], KC, 256)
                        st["w"] = (wsl, Bs)
                    wsl, Bs = st["w"]
                    w = 2 << g
                    fw.op(act, lambda g=g: nc.scalar.copy(out=bufU[:, 0:16], in_=halo[:, g, :]), reads=[B_halo[g]], writes=[BU])
                    for tt in range(NTT):
                        pz, Bpz = proj_F(wsl, Bs, (g % 2) * 128, 128, tt)
                        fw.op(act, lambda pz=pz, tt=tt: nc.scalar.copy(out=bufU[:, 16 + tt * TT:16 + (tt + 1) * TT], in_=pz[:, :]),
                              reads=[Bpz], writes=[BU])
                        yield
                    fw.op(act, lambda g=g: nc.scalar.copy(out=halo[:, g, :], in_=bufU[:, MT:MT + 16]), reads=[BU], writes=[B_halo[g]])
                    cur, Bcur = bufU, BU
                    step = 1
                    pp = [(bufA, BA), (bufB, BB)]
                    k = 0
                    while step < w:
                        nxt, Bnxt = pp[k % 2]; k += 1
                        fw.op(dve, lambda cur=cur, nxt=nxt, step=step: nc.vector.tensor_tensor(nxt[:, step:EXT], cur[:, step:EXT], cur[:, 0:EXT - step], ALU.add),
                              reads=[Bcur], writes=[Bnxt])
                        cur, Bcur = nxt, Bnxt
                        step *= 2
                    fw.op(dve, lambda cur=cur, w=w: nc.vector.scalar_tensor_tensor(pooled[:, :], cur[:, 16:EXT], 1.0 / w, bufU[:, 16:EXT], ALU.mult, ALU.subtract),
                          reads=[Bcur, BU], writes=[BP])
                    if m == 0:
                        t1, Bt1 = tmpf.next()
                        fw.op(dve, lambda cur=cur, t1=t1, g=g: nc.vector.tensor_tensor(t1[:, 0:16], cur[:, 16:32], invcnt_sb[:, g, :], ALU.mult),
                              reads=[Bcur, CONST], writes=[Bt1])
                        fw.op(dve, lambda t1=t1: nc.vector.tensor_tensor(pooled[:, 0:16], t1[:, 0:16], bufU[:, 16:32], ALU.subtract),
                              reads=[Bt1, BU], writes=[BP])
                    for tt in range(NTT):
                        ts = slice(tt * TT, (tt + 1) * TT)
                        pz, Bpz = next_pz()
                        fw.op(pe, lambda pz=pz, g=g, ts=ts: nc.tensor.matmul(pz[:, :], wpool_sb[:, g, :], pooled[:, ts], start=True, stop=True),
                              reads=[BP, B_lp], writes=[Bpz])
                        fw.op(act, lambda pz=pz, g=g, ts=ts: nc.scalar.mul(mixb[:, RH + g, ts], pz[:, :], spool_sb[:, g:g + 1]),
                              reads=[Bpz, B_lp], writes=[B_mix[RH + g]])
                    yield
                toks = []
                for b in (BA, BB, BU, BP):
                    toks += b.all_tokens()
                set_prev[s] = toks
            return A, B

        def make_gla(h, s):
            st = {}

            def A():
                sd = set_prev[s]
                qd = set_view(s, 0, MT); ki = set_view(s, MT, MT); ks = set_view(s, 2 * MT, MT)
                sga = set_view(s, 3 * MT, MT); sgb = set_view(s, 4 * MT, MT)
                vtm = set_view(s, 5 * MT, NCH * DV).rearrange("p (c e) -> p c e", e=DV)
                gch = set_view(s, 5 * MT + NCH * DV, 2 * NCH).bitcast(F32)
                Bqd, Bki, Bks, Bsga, Bsgb, Bv, Bgch = (Buf(n, sd) for n in ("qd", "ki", "ks", "sga", "sgb", "gv", "gch"))
                st.update(qd=qd, ki=ki, ks=ks, sga=sga, sgb=sgb, vtm=vtm, gch=gch,
                          Bqd=Bqd, Bki=Bki, Bks=Bks, Bsga=Bsga, Bsgb=Bsgb, Bv=Bv, Bgch=Bgch)
                wsl, Bs = load_slot(win_d[l, :, PC_GQK(h):PC_GQK(h) + 192], KC, 192)
                for tt in range(NTT):
                    ts = slice(tt * TT, (tt + 1) * TT)
                    pg, Bpg = next_pz()
                    fw.op(pe, lambda pg=pg, ts=ts: nc.tensor.matmul(pg[0:DK, :], wg_sb[:, h * DK:(h + 1) * DK], st_gz[0][0:16, ts], start=True, stop=True),
                          reads=[B_lp, st_gz[1]], writes=[Bpg])
                    e1, Be1 = tmpf.next()
                    fw.op(act, lambda pg=pg, e1=e1: nc.scalar.activation(out=e1[0:DK, :], in_=pg[0:DK, :], func=AF.Exp, bias=nbg_sb[0:DK, h:h + 1], scale=-1.0),
                          reads=[Bpg, B_lp], writes=[Be1])
                    fw.op(act, lambda e1=e1: nc.scalar.activation(out=e1[0:DK, :], in_=e1[0:DK, :], func=AF.Ln, bias=1.0, scale=1.0),
                          reads=[Be1], writes=[Be1])
                    cs, Bcs = tmpf.next()
                    fw.op(dve, lambda e1=e1, cs=cs: nc.vector.tensor_tensor_scan(cs[0:DK, :], smask_sb[0:DK, 0:TT], e1[0:DK, :], 0.0, ALU.mult, ALU.add),
                          reads=[Be1, CONST], writes=[Bcs])
                    eb, Beb = tmpf.next()
                    fw.op(act, lambda eb=eb, cs=cs: nc.scalar.activation(out=eb[0:DK, :], in_=cs[0:DK, :], func=AF.Exp, scale=-1.0 / 16),
                          reads=[Bcs], writes=[Beb])
                    fw.op(act, lambda cs=cs: nc.scalar.activation(out=cs[0:DK, :], in_=cs[0:DK, :], func=AF.Exp, scale=1.0 / 16),
                          reads=[Beb], writes=[Bcs])
                    fw.op(act, lambda eb=eb, tt=tt: nc.scalar.copy(out=gch[0:DK, tt * 4:(tt + 1) * 4], in_=eb[0:DK, 127::128]),
                          reads=[Beb], writes=[Bgch])
                    yield
                    pq, Bpq = proj_F(wsl, Bs, 0, DK, tt)
                    fw.op(dve, lambda pq=pq, eb=eb, ts=ts: nc.vector.scalar_tensor_tensor(qd[0:DK, ts], pq[0:DK, :], float(DK) ** -0.5, eb[0:DK, :], ALU.mult, ALU.mult),
                          reads=[Bpq, Beb], writes=[Bqd])
                    yield
                    pk, Bpk = proj_F(wsl, Bs, DK, DK, tt)
                    fw.op(dve, lambda pk=pk, cs=cs, ts=ts: nc.vector.tensor_tensor(ki[0:DK, ts], pk[0:DK, :], cs[0:DK, :], ALU.mult),
                          reads=[Bpk, Bcs], writes=[Bki])
                    fw.op(dve, lambda eb=eb, ts=ts: nc.vector.tensor_tensor(
                        ks[0:DK, ts].rearrange("p (c i) -> p c i", i=128),
                        ki[0:DK, ts].rearrange("p (c i) -> p c i", i=128),
                        eb[0:DK, 127::128].unsqueeze(2).to_broadcast([DK, TT // 128, 128]), ALU.mult),
                        reads=[Bki, Beb], writes=[Bks])
                    yield
                wsl, Bs = load_slot(win_d[l, :, PC_GV(h):PC_GV(h) + 192], KC, 192)
                yield from proj_T(wsl, Bs, 0, DV, list(range(NCH)), lambda grp: vtm[:, grp[0]:grp[0] + len(grp), :], Bv)
                wsl, Bs = load_slot(win_d[l, :, PC_GR(h):PC_GR(h) + 192], KC, 192)
                for tt in range(NTT):
                    ts = slice(tt * TT, (tt + 1) * TT)
                    pz, Bpz = proj_F(wsl, Bs, 0, 128, tt)
                    fw.op(act, lambda pz=pz, ts=ts: nc.scalar.activation(out=sga[:, ts], in_=pz[:, :], func=AF.Silu), reads=[Bpz], writes=[Bsga])
                    yield
                    pz, Bpz = proj_F(wsl, Bs, 128, 64, tt)
                    fw.op(act, lambda pz=pz, ts=ts: nc.scalar.activation(out=sgb[0:64, ts], in_=pz[0:64, :], func=AF.Silu), reads=[Bpz], writes=[Bsgb])
                    yield

            def B():
                qd, ki, ks, sga, sgb, vtm, gch = (st[k] for k in ("qd", "ki", "ks", "sga", "sgb", "vtm", "gch"))
                Bqd, Bki, Bks, Bsga, Bsgb, Bv, Bgch = (st[k] for k in ("Bqd", "Bki", "Bks", "Bsga", "Bsgb", "Bv", "Bgch"))
                sT_r = gsT_r
                ka, kb = 10 + 2 * h, 11 + 2 * h
                pre = {}

                def emit_i(ch):
                    cs_ = slice(ch * CH, (ch + 1) * CH)
                    ti = tr_i[0] % 2; tr_i[0] += 1
                    trp = tr_ps_gla[:, ti * 128:ti * 128 + DK]
                    Btr = B_pb[PROT]
                    fw.op(pe, lambda: nc.tensor.transpose(trp, ks[0:DK, cs_], ident_sb[0:DK, 0:DK]),
                          reads=[Bks, CONST], writes=[Btr])
                    kst, Bkst = kst_r[ch % 2]
                    fw.op(act, lambda: nc.scalar.copy(out=kst[:, 0:DK], in_=trp), reads=[Btr], writes=[Bkst])
                    si = sc_i[0] % 4; sc_i[0] += 1
                    scp = pbank[PSC][:, si * 128:(si + 1) * 128]
                    fw.op(pe, lambda: nc.tensor.matmul(scp, ki[0:DK, cs_], qd[0:DK, cs_], start=True, stop=True),
                          reads=[Bki, Bqd], writes=[B_sc[si]])
                    sT, BsT = sT_r[ch % 2]
                    fw.op(dve, lambda: nc.vector.tensor_tensor(sT, scp, causal_sb[:, :], ALU.mult),
                          reads=[B_sc[si], CONST], writes=[BsT])
                    pre[ch] = (kst, Bkst, sT, BsT)

                emit_i(0)
                yield
                for grp in range(NCH // 4):
                    roA, BroA = pbank[PROA], B_pb[PROA]
                    roB, BroB = pbank[PROB], B_pb[PROB]
                    for ci in range(4):
                        ch = grp * 4 + ci
                        cs_ = slice(ch * CH, (ch + 1) * CH)
                        if ch + 1 < NCH:
                            emit_i(ch + 1)
                        kst, Bkst, sT, BsT = pre.pop(ch)
                        ra = roA[:, ci * 128:(ci + 1) * 128]
                        rb = roB[0:64, ci * 128:(ci + 1) * 128]
                        fw.op(pe, lambda ra=ra, sT=sT: nc.tensor.matmul(ra, vtm[:, ch, 0:128], sT, start=True, stop=False),
                              reads=[Bv, BsT], writes=[BroA], mark=False)
                        fw.op(pe, lambda ra=ra: nc.tensor.matmul(ra, Sgla_bf[0:DK, h, 0:128], qd[0:DK, cs_], start=False, stop=True),
                              reads=[Bv, BsT, B_Sglabf[h], Bqd], writes=[BroA])
                        fw.op(pe, lambda rb=rb, sT=sT: nc.tensor.matmul(rb, vtm[:, ch, 128:192], sT, start=True, stop=False),
                              reads=[Bv, BsT], writes=[BroB], mark=False)
                        fw.op(pe, lambda rb=rb: nc.tensor.matmul(rb, Sgla_bf[0:DK, h, 128:192], qd[0:DK, cs_], start=False, stop=True),
                              reads=[Bv, BsT, B_Sglabf[h], Bqd], writes=[BroB])
                        ui = u_i[0] % 2; u_i[0] += 1
                        up = pbank[PU][0:DK, ui * 192:(ui + 1) * 192]
                        fw.op(pe, lambda up=up, kst=kst: nc.tensor.matmul(up, kst[:, 0:DK], vtm[:, ch, :], start=True, stop=True),
                              reads=[Bkst, Bv], writes=[B_u[ui]])
                        fw.op(dve, lambda up=up, ch=ch: nc.vector.scalar_tensor_tensor(Sgla[0:DK, h, :], Sgla[0:DK, h, :], gch[0:DK, ch:ch + 1], up, ALU.mult, ALU.add),
                              reads=[B_u[ui], Bgch], writes=[B_Sgla[h]])
                        fw.op(act, lambda: nc.scalar.copy(out=Sgla_bf[0:DK, h, :], in_=Sgla[0:DK, h, :]),
                              reads=[B_Sgla[h]], writes=[B_Sglabf[h]])
                        yield
                    gs = slice(grp * TT, (grp + 1) * TT)
                    sqa, Bsqa = tmpb.next()
                    sqb, Bsqb = tmpb.next()
                    fw.op(act, lambda sqa=sqa: nc.scalar.activation(out=sqa[:], in_=roA[:, :], func=AF.Square), reads=[BroA], writes=[Bsqa])
                    fw.op(act, lambda sqb=sqb: nc.scalar.activation(out=sqb[0:64, :], in_=roB[0:64, :], func=AF.Square), reads=[BroB], writes=[Bsqb])
                    stp, Bst = pbank[PSTAT], B_pb[PSTAT]
                    fw.op(pe, lambda sqa=sqa: nc.tensor.matmul(stp[:, :], ones_sb[:, :], sqa[:], start=True, stop=False),
                          reads=[Bsqa, CONST], writes=[Bst], mark=False)
                    fw.op(pe, lambda sqb=sqb: nc.tensor.matmul(stp[:, :], ones_sb[0:64, :], sqb[0:64, :], start=False, stop=True),
                          reads=[Bsqa, Bsqb, CONST], writes=[Bst])
                    yield
                    rs, Brs = rstd_t.next()
                    fw.op(act, lambda rs=rs: nc.scalar.activation(out=rs[:], in_=stp[:, :], func=AF.Ln, bias=eps_sb[:, 0:1], scale=1.0 / DV),
                          reads=[Bst, CONST], writes=[Brs])
                    fw.op(act, lambda rs=rs: nc.scalar.activation(out=rs[:], in_=rs[:], func=AF.Exp, scale=-0.5), reads=[Brs], writes=[Brs])
                    t1, Bt1 = tmpf.next()
                    fw.op(dve, lambda t1=t1, rs=rs: nc.vector.tensor_tensor(t1[:], roA[:, :], rs[:], ALU.mult), reads=[BroA, Brs], writes=[Bt1])
                    fw.op(dve, lambda t1=t1: nc.vector.tensor_tensor(mixb[:, ka, gs], t1[:], sga[:, gs], ALU.mult), reads=[Bt1, Bsga], writes=[B_mix[ka]])
                    t2, Bt2 = tmpf.next()
                    fw.op(dve, lambda t2=t2, rs=rs: nc.vector.tensor_tensor(t2[0:64, :], roB[0:64, :], rs[0:64, :], ALU.mult), reads=[BroB, Brs], writes=[Bt2])
                    fw.op(dve, lambda t2=t2: nc.vector.tensor_tensor(mixb[0:64, kb, gs], t2[0:64, :], sgb[0:64, gs], ALU.mult), reads=[Bt2, Bsgb], writes=[B_mix[kb]])
                    yield
                toks = []
                for b in (Bqd, Bki, Bks, Bsga, Bsgb, Bv, Bgch):
                    toks += b.all_tokens()
                set_prev[s] = toks
            return A, B

        gzT = f32_view(512, MT // 2).bitcast(BF16)
        B_gz = Buf("gzT", seed)
        st_gz = (gzT, B_gz)

        def gz_A():
            for tt in range(NTT):
                ts = slice(tt * TT, (tt + 1) * TT)
                pz, Bpz = next_pz()
                mm_group(pz[0:16, :], [(wgz_sb[:, kc, :], hT[:, kc, ts]) for kc in range(KC)], reads=[B_lp, B_h[tt]], writes=[Bpz])
                fw.op(act, lambda pz=pz, ts=ts: nc.scalar.copy(out=gzT[0:16, ts], in_=pz[0:16, :]), reads=[Bpz], writes=[B_gz])
                yield

        ui_ = 0
        kinds = []
        for h in range(RH):
            units.append(make_ret(h, ui_ % 2)); ui_ += 1; kinds.append("ret")
        units.append(make_pool(ui_ % 2)); ui_ += 1; kinds.append("pool")
        gla_first = len(units)
        for h in range(GH):
            units.append(make_gla(h, ui_ % 2)); ui_ += 1; kinds.append("gla")

        if stop and stop.startswith("units"):
            units = units[:int(stop[5:].rstrip("A"))]
        NA = {"ret": 8, "pool": 0, "gla": 14}
        NB = {"ret": 11, "pool": 12, "gla": 13}

        def chain(*gens):
            for g in gens:
                yield from g

        for _ in units[0][0]():
            pass
        if stop and stop.endswith("A"):
            units = []
        for ui2 in range(len(units)):
            genB = units[ui2][1]()
            if ui2 + 1 < len(units):
                na = NA[kinds[ui2 + 1]]
                if ui2 + 1 == gla_first:
                    genA = chain(gz_A(), units[ui2 + 1][0]())
                    na += 2
                else:
                    genA = units[ui2 + 1][0]()
            else:
                genA = iter(())
                na = 0
            ratio = na / NB[kinds[ui2]]
            acc = 0.0
            for _ in genB:
                acc += ratio
                while acc >= 1.0:
                    acc -= 1.0
                    next(genA, None)
            for _ in genA:
                pass

        fence = fw.fence_tokens()
        B_x.r = list(fence)
        B_x.w = None
        for half in range(2):
            ks_ = slice(half * 8, (half + 1) * 8)
            fw.dma(sp, x_mt[:, ks_, :], src[:, tok0:tok0 + MT].rearrange("(k p) t -> p k t", p=128)[:, ks_, :],
                   reads=[B_xs[m]], writes=[B_x], sem=xs_sem)
        B_x.w = (xs_sem[2], xs_sem[0], xs_sem[1])

        if stop and (stop.startswith("units") or stop == "mixers"):
            for half in range(2):
                ks_ = slice(half * 8, (half + 1) * 8)
                fw.dma(sp, yT_d[:, tok0:tok0 + MT].rearrange("(k p) t -> p k t", p=128)[:, ks_, :], x_mt[:, ks_, :],
                       reads=[B_x], writes=[B_xs[m]], sem=xs_sem)
            return
        for sl in range(D // SLOTW):
            wsl, Bs = load_slot(wout_d[l, :, sl * SLOTW:(sl + 1) * SLOTW], NKO, SLOTW)
            for cb in range(SLOTW // 128):
                rb = sl * (SLOTW // 128) + cb
                for tt in range(NTT):
                    ts = slice(tt * TT, (tt + 1) * TT)
                    pz, Bpz = next_pz()
                    mm_group(pz[:, :], [(wsl[:, k, cb * 128:(cb + 1) * 128], mixb[:, k, ts]) for k in range(NKO)],
                             reads=[Bs] + B_mix, writes=[Bpz])
                    fw.op(dve, lambda pz=pz, rb=rb, ts=ts: nc.vector.scalar_tensor_tensor(
                        x_mt[:, rb, ts], pz[:, :], mod_sb[:, GT1 + rb:GT1 + rb + 1], x_mt[:, rb, ts], ALU.mult, ALU.add),
                        reads=[Bpz, B_mod], writes=[B_x])

        if stop == "outproj":
            for half in range(2):
                ks_ = slice(half * 8, (half + 1) * 8)
                fw.dma(sp, yT_d[:, tok0:tok0 + MT].rearrange("(k p) t -> p k t", p=128)[:, ks_, :], x_mt[:, ks_, :],
                       reads=[B_x], writes=[B_xs[m]], sem=xs_sem)
            return
        rms_to_h(gmod2, SH2)

        hid = mixb
        B_hid = B_mix
        ada_todo = []
        if l + 1 < NL:
            per = (N_ADA_SLOTS + NMT - 1) // NMT
            ada_todo = list(range(m * per, min(N_ADA_SLOTS, (m + 1) * per)))
        n_iter = 2 * (DFF // D) * (D // SLOTW)
        ada_every = max(1, n_iter // max(1, len(ada_todo))) if ada_todo else 0
        it_ctr = [0]

        def ada_tick():
            it_ctr[0] += 1
            if ada_todo and it_ctr[0] % ada_every == 0:
                ada_slot(l + 1, ada_todo.pop(0))

        for hb in range(DFF // D):
            for sl in range(D // SLOTW):
                c0 = hb * D + sl * SLOTW
                wsl, Bs = load_slot(wup_d[l, :, c0:c0 + SLOTW], KC, SLOTW)
                for cb in range(SLOTW // 128):
                    kk = sl * (SLOTW // 128) + cb
                    for tt in range(NTT):
                        ts = slice(tt * TT, (tt + 1) * TT)
                        pz, Bpz = proj_F(wsl, Bs, cb * 128, 128, tt)
                        r, Br = tmpf.next()
                        fw.op(act, lambda pz=pz, r=r: nc.scalar.activation(out=r[:], in_=pz[:, :], func=AF.Relu), reads=[Bpz], writes=[Br])
                        fw.op(dve, lambda r=r, kk=kk, ts=ts: nc.vector.tensor_tensor(hid[:, kk, ts], r[:], r[:], ALU.mult),
                              reads=[Br], writes=[B_hid[kk]])
                ada_tick()
            for sl in range(D // SLOTW):
                wsl, Bs = load_slot(wdn_d[l, hb * D:(hb + 1) * D, sl * SLOTW:(sl + 1) * SLOTW], KC, SLOTW)
                for cb in range(SLOTW // 128):
                    rb = sl * (SLOTW // 128) + cb
                    for tt in range(NTT):
                        ts = slice(tt * TT, (tt + 1) * TT)
                        pz, Bpz = next_pz()
                        mm_group(pz[:, :], [(wsl[:, k, cb * 128:(cb + 1) * 128], hid[:, k, ts]) for k in range(KC)],
                                 reads=[Bs] + B_hid[0:KC], writes=[Bpz])
                        fw.op(dve, lambda pz=pz, rb=rb, ts=ts: nc.vector.scalar_tensor_tensor(
                            x_mt[:, rb, ts], pz[:, :], mod_sb[:, GT2 + rb:GT2 + rb + 1], x_mt[:, rb, ts], ALU.mult, ALU.add),
                            reads=[Bpz, B_mod], writes=[B_x])
                ada_tick()
        while ada_todo:
            ada_slot(l + 1, ada_todo.pop(0))

        if last_layer and final_norm:
            for tt in range(NTT):
                ts = slice(tt * TT, (tt + 1) * TT)
                ss, Bss = pbank[PSTAT], B_pb[PSTAT]
                for kc in range(KC):
                    sq, Bsq = tmpb.next()
                    fw.op(act, lambda kc=kc, sq=sq: nc.scalar.activation(out=sq[:], in_=x_mt[:, kc, ts], func=AF.Square),
                          reads=[B_x], writes=[Bsq])
                    fw.op(pe, lambda kc=kc, sq=sq: nc.tensor.matmul(ss[:, :], ones_sb[:, :], sq[:], start=(kc == 0), stop=(kc == KC - 1)),
                          reads=[Bsq, CONST], writes=[Bss], mark=True)
                rs, Brs = rstd_t.next()
                fw.op(act, lambda rs=rs: nc.scalar.activation(out=rs[:], in_=ss[:, :], func=AF.Ln, bias=eps_sb[:, 0:1], scale=1.0 / D),
                      reads=[Bss, CONST], writes=[Brs])
                fw.op(act, lambda rs=rs: nc.scalar.activation(out=rs[:], in_=rs[:], func=AF.Exp, scale=-0.5), reads=[Brs], writes=[Brs])
                for kc in range(KC):
                    fw.op(dve, lambda kc=kc, rs=rs: nc.vector.scalar_tensor_tensor(x_mt[:, kc, ts], x_mt[:, kc, ts], gfin_sb[:, kc:kc + 1], rs[:], ALU.mult, ALU.mult),
                          reads=[Brs, CONST], writes=[B_x])
            dst = yT_d
        else:
            dst = yT_d if last_layer else xs_d
        for half in range(2):
            ks_ = slice(half * 8, (half + 1) * 8)
            fw.dma(sp, dst[:, tok0:tok0 + MT].rearrange("(k p) t -> p k t", p=128)[:, ks_, :], x_mt[:, ks_, :],
                   reads=[B_x], writes=[B_xs[m]], sem=xs_sem)
        B_xs[m].w = (xs_sem[2], xs_sem[0], xs_sem[1])

    for l in range(NL):
        layer_prologue(l)
        for m in range(NMT):
            run_mt(l, m)
    sp.wait(fw.fence_tokens())
    return nc


def prep_shared(inputs, NL):
    f = lambda a: np.ascontiguousarray(np.asarray(a, dtype=np.float32))
    perm = in_col_perm()
    rows = out_row_map()
    w_out = np.asarray(inputs["w_out"], dtype=np.float32)[:NL]
    w_out_p = np.zeros((NL, NKO * 128, D), np.float32)
    valid = rows >= 0
    w_out_p[:, valid, :] = w_out[:, rows[valid], :]
    sh = {
        "w_ada": f(np.asarray(inputs["w_ada"])[:NL]),
        "b_ada_fm": f(np.asarray(inputs["b_ada"])[:NL].reshape(NL, 96, 128).transpose(0, 2, 1)),
        "g_mix_fm": f(np.asarray(inputs["g_mix"])[:NL].reshape(NL, KC, 128).transpose(0, 2, 1)),
        "g_mlp_fm": f(np.asarray(inputs["g_mlp"])[:NL].reshape(NL, KC, 128).transpose(0, 2, 1)),
        "w_in_p": f(np.asarray(inputs["w_in"])[:NL][:, :, perm]),
        "w_gate_up": f(np.asarray(inputs["w_gate_up"])[:NL]),
        "w_pool": f(np.asarray(inputs["w_pool"])[:NL]),
        "s_pool_fm": f(np.asarray(inputs["s_pool"])[:NL].reshape(NL, 4, 128).transpose(0, 2, 1)),
        "w_out_p": w_out_p,
        "w_up": f(np.asarray(inputs["w_up"])[:NL]),
        "w_down": f(np.asarray(inputs["w_down"])[:NL]),
        "g_final_fm": f(np.asarray(inputs["g_final"]).reshape(KC, 128).T),
    }
    bg = np.zeros((NL, 128, GH), np.float32)
    bg[:, :DK, :] = np.asarray(inputs["b_gate"], dtype=np.float32)[:NL].reshape(NL, GH, DK).transpose(0, 2, 1)
    sh["b_gate_fm"] = bg
    return sh


def run_cores(nc, shared, x_list, c_list, pos0_list, seqstart_list):
    in_maps = []
    for x, c, p0, ss in zip(x_list, c_list, pos0_list, seqstart_list):
        m = dict(shared)
        m["xT"] = np.ascontiguousarray(np.asarray(x, dtype=np.float32).T)
        m["c_fm"] = np.ascontiguousarray(np.asarray(c, dtype=np.float32).reshape(KC, 128).T)
        m.update(host_tables(p0, x.shape[0], ss))
        in_maps.append(m)
    res = run_bass_kernel_spmd(nc, in_maps, core_ids=list(range(len(in_maps))))
    return [np.ascontiguousarray(r["yT"].T) for r in res.results]


_CACHE = {}


def kernel(x, c, w_ada, b_ada, g_mix, g_mlp, w_in, w_gate_up, b_gate, w_pool, s_pool,
           w_out, w_up, w_down, g_final):
    inputs = dict(w_ada=w_ada, b_ada=b_ada, g_mix=g_mix, g_mlp=g_mlp, w_in=w_in, w_gate_up=w_gate_up,
                  b_gate=b_gate, w_pool=w_pool, s_pool=s_pool, w_out=w_out, w_up=w_up, w_down=w_down,
                  g_final=g_final)
    x = np.asarray(x, dtype=np.float32)
    c = np.asarray(c, dtype=np.float32)
    B, S, _ = x.shape
    NL = 4
    NMT = S // MT
    key = (NL, NMT)
    if key not in _CACHE:
        _CACHE[key] = build_program(NL, NMT)
    nc = _CACHE[key]
    shared = prep_shared(inputs, NL)
    real = {0: 0, 2: 1, 4: 2, 6: 3}
    tabs = host_tables(0, S, True)
    zero_map = None
    in_maps = []
    for core in range(8):
        if core in real:
            b = real[core]
            mm = dict(shared)
            mm["xT"] = np.ascontiguousarray(x[b].T)
            mm["c_fm"] = np.ascontiguousarray(c[b].reshape(KC, 128).T)
            mm.update(tabs)
            in_maps.append(mm)
        else:
            if zero_map is None:
                ref = in_maps[0]
                zero_map = {k: np.zeros_like(v) for k, v in ref.items()}
            in_maps.append(zero_map)
    res = run_bass_kernel_spmd(nc, in_maps, core_ids=list(range(8)))
    outs = {b: np.ascontiguousarray(res.results[core]["yT"].T) for core, b in real.items()}
    return np.stack([outs[b] for b in range(B)], axis=0).astype(np.float32)
```

```python
import math
import numpy as np
import concourse.bass as bass
import concourse.mybir as mybir
from concourse.bass_utils import run_bass_kernel_spmd

F32 = mybir.dt.float32
BF16 = mybir.dt.bfloat16
AF = mybir.ActivationFunctionType
ALU = mybir.AluOpType

D = 2048
KC = 16
MT = 1024
TT = 512
NTT = MT // TT
CH = 128
NCH = MT // CH
RH = 6
GH = 4
DK = 96
DV = 192
NKO = 18
DFF = 8192
EPS = 1e-6
IN_WIDTH = 5904
SLOTW = 256


class Eng:
    def __init__(self, fw, name, eng):
        self.fw = fw
        self.name = name
        self.eng = eng
        self.sem = fw.nc.alloc_semaphore("sem_" + name)
        self.count = 0
        self.waited = {}

    def wait(self, toks):
        best = {}
        for t in toks:
            if t is None:
                continue
            key, sem, val = t
            if val <= self.waited.get(key, 0):
                continue
            if key == "pe" and self.name == "pe":
                continue
            if key not in best or best[key][1] < val:
                best[key] = (sem, val)
        for key, (sem, val) in best.items():
            self.eng.wait_ge(sem, val)
            self.waited[key] = val

    def mark(self, inst):
        inst.then_inc(self.sem, 1)
        self.count += 1
        return (self.name, self.sem, self.count)

    def now(self):
        if self.count == 0:
            return None
        return (self.name, self.sem, self.count)


class Buf:
    def __init__(self, name="", seed=None):
        self.name = name
        self.w = None
        self.r = list(seed) if seed else []

    def rdeps(self):
        return [self.w]

    def wdeps(self):
        return [self.w] + self.r

    def wrote(self, tok):
        self.w = tok
        self.r = []

    def read(self, tok):
        self.r.append(tok)
        if len(self.r) > 16:
            best = {}
            for t in self.r:
                if t is None:
                    continue
                if t[0] not in best or best[t[0]][2] < t[2]:
                    best[t[0]] = t
            self.r = list(best.values())

    def all_tokens(self):
        return [t for t in ([self.w] + self.r) if t is not None]


class PBuf(Buf):
    def rdeps(self):
        return self.wdeps()

    def read(self, tok):
        self.wrote(tok)


class FW:
    def __init__(self):
        self.nc = bass.Bass("TRN2", target_bir_lowering=False)
        nc = self.nc
        self.pe = Eng(self, "pe", nc.tensor)
        self.act = Eng(self, "act", nc.scalar)
        self.dve = Eng(self, "dve", nc.vector)
        self.pool = Eng(self, "pool", nc.gpsimd)
        self.sp = Eng(self, "sp", nc.sync)
        self.engs = [self.pe, self.act, self.dve, self.pool, self.sp]
        self.dsems = []

    def sb(self, name, shape, dt):
        return self.nc.alloc_sbuf_tensor(name, list(shape), dt)

    def op(self, E, fn, reads=(), writes=(), mark=True):
        deps = []
        for b in reads:
            deps += b.rdeps()
        for b in writes:
            deps += b.wdeps()
        E.wait(deps)
        inst = fn()
        if not mark:
            return None
        tok = E.mark(inst)
        for b in reads:
            b.read(tok)
        for b in writes:
            b.wrote(tok)
        return tok

    def dsem(self, name):
        s = [self.nc.alloc_semaphore(name), 0, "d_" + name]
        self.dsems.append(s)
        return s

    def dma(self, E, out_ap, in_ap, reads=(), writes=(), sem=None):
        deps = []
        for b in reads:
            deps += b.rdeps()
        for b in writes:
            deps += b.wdeps()
        E.wait(deps)
        inst = E.eng.dma_start(out=out_ap, in_=in_ap)
        sem[1] += 16
        inst.then_inc(sem[0], 16)
        tok = (sem[2], sem[0], sem[1])
        for b in reads:
            b.read(tok)
        for b in writes:
            b.wrote(tok)
        return tok

    def fence_tokens(self):
        toks = [e.now() for e in self.engs]
        toks += [(s[2], s[0], s[1]) for s in self.dsems if s[1] > 0]
        return [t for t in toks if t is not None]


class Rot:
    def __init__(self, fw, name, n, shape, dt, space="sb"):
        self.items = []
        for i in range(n):
            t = fw.sb(f"{name}{i}", shape, dt)
            self.items.append((t, Buf(f"{name}{i}")))
        self.i = 0

    def next(self):
        it = self.items[self.i % len(self.items)]
        self.i += 1
        return it


RET_W = 768
OFF_RQ, OFF_RK, OFF_RV, OFF_RG = 0, 768, 1536, 2304
OFF_PU = 3072
OFF_GQ, OFF_GK, OFF_GV, OFF_GR, OFF_GZ = 3584, 3968, 4352, 5120, 5888


def in_col_perm():
    cols = []
    for h in range(RH):
        cols += list(range(OFF_RQ + h * 128, OFF_RQ + (h + 1) * 128))
        cols += list(range(OFF_RK + h * 128, OFF_RK + (h + 1) * 128))
    for h in range(RH):
        cols += list(range(OFF_RG + h * 128, OFF_RG + (h + 1) * 128))
        cols += list(range(OFF_RV + h * 128, OFF_RV + (h + 1) * 128))
    cols += list(range(OFF_PU, OFF_PU + 512))
    for h in range(GH):
        cols += list(range(OFF_GQ + h * DK, OFF_GQ + (h + 1) * DK))
        cols += list(range(OFF_GK + h * DK, OFF_GK + (h + 1) * DK))
        cols += list(range(OFF_GV + h * DV, OFF_GV + (h + 1) * DV))
        cols += list(range(OFF_GR + h * DV, OFF_GR + (h + 1) * DV))
    cols += list(range(OFF_GZ, OFF_GZ + 16))
    assert len(cols) == IN_WIDTH and len(set(cols)) == IN_WIDTH
    return np.array(cols)


PC_RQK = lambda h: h * 256
PC_RGV = lambda h: 1536 + h * 256
PC_POOL = lambda s: 3072 + s * 256
PC_GQK = lambda h: 3584 + h * 576
PC_GV = lambda h: 3584 + h * 576 + 192
PC_GR = lambda h: 3584 + h * 576 + 384
PC_GZ = 5888


def out_row_map():
    rows = []
    for h in range(RH):
        rows += list(range(h * 128, (h + 1) * 128))
    for g in range(4):
        rows += list(range(768 + g * 128, 768 + (g + 1) * 128))
    for h in range(GH):
        base = 1280 + h * DV
        rows += list(range(base, base + 128))
        rows += list(range(base + 128, base + 192)) + [-1] * 64
    assert len(rows) == NKO * 128
    return np.array(rows)


def host_tables(pos0, ntok, seq_start):
    t = {}
    inv_freq = (10000.0 ** (-np.arange(0, 128, 2, dtype=np.float32) / np.float32(128))).astype(np.float32)
    pos = (pos0 + np.arange(ntok)).astype(np.float32)
    ang = (pos[None, :] * inv_freq[:, None]).astype(np.float32)
    cos = np.cos(ang.astype(np.float64)).astype(np.float32)
    sin = np.sin(ang.astype(np.float64)).astype(np.float32)
    t["cosT"] = np.ascontiguousarray(np.concatenate([cos, cos], 0))
    t["sinT"] = np.ascontiguousarray(np.concatenate([-sin, sin], 0))
    hh = np.arange(RH, dtype=np.float64)
    log_g = np.log1p(-np.exp2(-5.0 - hh))
    i = np.arange(128, dtype=np.float64)
    xi = np.exp((i[None, :] + 1.0) * log_g[:, None]) * (128.0 ** -0.5)
    t["xi"] = np.ascontiguousarray(np.broadcast_to(xi[None], (128, RH, 128))).astype(np.float32)
    jj = i[:, None, None]
    ii = i[None, None, :]
    d2 = np.where(jj <= ii, np.exp(-(jj + 1.0) * log_g[None, :, None]), 0.0)
    t["d2"] = np.ascontiguousarray(d2).astype(np.float32)
    zeta = np.exp((127.0 - i)[:, None] * log_g[None, :])
    t["zeta"] = np.ascontiguousarray(zeta).astype(np.float32)
    t["causal"] = (i[:, None] <= i[None, :]).astype(np.float32)
    t["ident"] = np.eye(128, dtype=np.float32)
    sw = np.zeros((128, 128), np.float32)
    for m in range(128):
        sw[(m + 64) % 128, m] = 1.0
    t["pswap"] = sw
    sm = np.ones((128, MT), np.float32)
    sm[:, ::CH] = 0.0
    t["scanmask"] = sm
    ic = np.zeros((128, 4, 16), np.float32)
    for g, w in enumerate((2, 4, 8, 16)):
        for tt in range(16):
            ic[:, g, tt] = 1.0 / (min(tt + 1, w) if seq_start else w)
    t["invcnt"] = ic
    return t


G128 = [float(np.exp(128.0 * np.log1p(-np.exp2(-5.0 - h)))) for h in range(RH)]


def build_program(NL, NMT, final_norm=True, stop=None):
    fw = FW()
    nc = fw.nc
    pe, act, dve, pool, sp = fw.pe, fw.act, fw.dve, fw.pool, fw.sp
    NTOK = NMT * MT

    def din(name, shape, dt=F32):
        return nc.dram_tensor(name, list(shape), dt, kind="ExternalInput").ap()

    xT_d = din("xT", [D, NTOK])
    c_d = din("c_fm", [128, KC])
    wada_d = din("w_ada", [NL, D, 6 * D])
    bada_d = din("b_ada_fm", [NL, 128, 96])
    gmix_d = din("g_mix_fm", [NL, 128, KC])
    gmlp_d = din("g_mlp_fm", [NL, 128, KC])
    win_d = din("w_in_p", [NL, D, IN_WIDTH])
    wg_d = din("w_gate_up", [NL, 16, 384])
    bg_d = din("b_gate_fm", [NL, 128, GH])
    wpool_d = din("w_pool", [NL, 4, 128, 128])
    spool_d = din("s_pool_fm", [NL, 128, 4])
    wout_d = din("w_out_p", [NL, NKO * 128, D])
    wup_d = din("w_up", [NL, D, DFF])
    wdn_d = din("w_down", [NL, DFF, D])
    gfin_d = din("g_final_fm", [128, KC])
    cos_d = din("cosT", [128, NTOK])
    sin_d = din("sinT", [128, NTOK])
    xi_d = din("xi", [128, RH, 128])
    d2_d = din("d2", [128, RH, 128])
    zeta_d = din("zeta", [128, RH])
    causal_d = din("causal", [128, 128])
    ident_d = din("ident", [128, 128])
    pswap_d = din("pswap", [128, 128])
    smask_d = din("scanmask", [128, MT])
    invcnt_d = din("invcnt", [128, 4, 16])
    yT_d = nc.dram_tensor("yT", [D, NTOK], F32, kind="ExternalOutput").ap()
    xs_d = nc.dram_tensor("xs", [D, NTOK], F32).ap()

    arena = fw.sb("arena", [128, KC * MT], F32)
    x_mt = arena[:, :].rearrange("p (k t) -> p k t", k=KC)
    B_x = Buf("x_mt")
    hT = fw.sb("hT", [128, KC, MT], BF16)
    B_h = [Buf(f"hT{t}") for t in range(NTT)]
    mixb = fw.sb("mix", [128, NKO, MT], BF16)
    B_mix = [Buf(f"mix{k}") for k in range(NKO)]
    NSLOT = 3
    wslot = [fw.sb(f"wslot{i}", [128, NKO, SLOTW], BF16) for i in range(NSLOT)]
    B_slot = [Buf(f"wslot{i}") for i in range(NSLOT)]
    S_slot = [fw.dsem(f"wsl{i}") for i in range(NSLOT)]
    slot_i = [0]

    cos_sb = fw.sb("cos_sb", [128, MT], F32)
    sin_sb = fw.sb("sin_sb", [128, MT], F32)
    B_cs = Buf("cossin")
    xi_sb = fw.sb("xi_sb", [128, RH, 128], F32)
    d2_sb = fw.sb("d2_sb", [128, RH, 128], F32)
    zeta_sb = fw.sb("zeta_sb", [128, RH], F32)
    causal_sb = fw.sb("causal_sb", [128, 128], F32)
    ident_sb = fw.sb("ident_sb", [128, 128], BF16)
    pswap_sb = fw.sb("pswap_sb", [128, 128], BF16)
    ones_sb = fw.sb("ones_sb", [128, 128], BF16)
    smask_sb = fw.sb("smask_sb", [128, MT], F32)
    invcnt_sb = fw.sb("invcnt_sb", [128, 4, 16], F32)
    eps_sb = fw.sb("eps_sb", [128, 1], F32)
    B_const = Buf("const")

    c_sb = fw.sb("c_sb", [128, KC], F32)
    sc_bf = fw.sb("sc_bf", [128, KC], BF16)
    gfin_sb = fw.sb("gfin_sb", [128, KC], F32)
    B_c = Buf("c")

    bada_sb = fw.sb("bada_sb", [128, 96], F32)
    gmix_sb = fw.sb("gmix_sb", [128, KC], F32)
    gmlp_sb = fw.sb("gmlp_sb", [128, KC], F32)
    wg_sb = fw.sb("wg_sb", [16, 384], BF16)
    nbg_sb = fw.sb("nbg_sb", [128, GH], F32)
    bg_sb = fw.sb("bg_sb", [128, GH], F32)
    wpool_sb = fw.sb("wpool_sb", [128, 4, 128], BF16)
    spool_sb = fw.sb("spool_sb", [128, 4], F32)
    wgz_sb = fw.sb("wgz_sb", [128, KC, 16], BF16)
    mod_sb = fw.sb("mod_sb", [128, 96], F32)
    modn_sb = fw.sb("modn_sb", [128, 96], F32)
    B_modn = Buf("modn")
    gmod1 = fw.sb("gmod1", [128, KC], F32)
    gmod2 = fw.sb("gmod2", [128, KC], F32)
    B_lp = Buf("layerparams")
    B_mod = Buf("mod")

    Sret = fw.sb("Sret", [128, RH, 128], F32)
    Sret_bf = fw.sb("Sret_bf", [128, RH, 128], BF16)
    Sgla = fw.sb("Sgla", [128, GH, DV], F32)
    Sgla_bf = fw.sb("Sgla_bf", [128, GH, DV], BF16)
    halo = fw.sb("halo", [128, 4, 16], F32)
    B_Sret = [Buf(f"Sret{h}") for h in range(RH)]
    B_Sretbf = [Buf(f"Sretbf{h}") for h in range(RH)]
    B_Sgla = [Buf(f"Sgla{h}") for h in range(GH)]
    B_Sglabf = [Buf(f"Sglabf{h}") for h in range(GH)]
    B_halo = [Buf(f"halo{g}") for g in range(4)]

    tmpf = Rot(fw, "tmpf", 3, [128, TT], F32)
    tmpb = Rot(fw, "tmpb", 2, [128, TT], BF16)
    sqpool = Rot(fw, "sqp", 4, [128, TT], BF16)
    rstd_t = Rot(fw, "rstd", 2, [128, TT], F32)

    pbank = [nc.alloc_psum_tensor(f"pb{i}", [128, 512], F32) for i in range(8)]
    B_pb = [PBuf(f"pb{i}") for i in range(8)]
    PZ = [0, 1]
    PROT, PSC, PROA, PROB, PSTAT, PU = 2, 3, 4, 5, 6, 7
    pz_i = [0]

    pz_cur = [list(PZ)]

    def next_pz():
        lst = pz_cur[0]
        i = lst[pz_i[0] % len(lst)]
        pz_i[0] += 1
        return pbank[i], B_pb[i]

    B_sc = [B_pb[PSC]] * 4
    sc_i = [0]
    B_u = [B_pb[PU]] * 2
    u_i = [0]
    tr_ps_ret = pbank[PROB][:, 0:128].bitcast(BF16)
    tr_ps_gla = pbank[PROT][:, 0:128].bitcast(BF16)
    tr_i = [0]

    ld_sem = fw.dsem("ld")
    xs_sem = fw.dsem("xs")
    B_xs = [Buf(f"xs{m}") for m in range(NMT)]

    def mm_group(out_ap, pairs, reads, writes, flags=None):
        n = len(pairs)
        for i, (l, r) in enumerate(pairs):
            last = i == n - 1
            st = (i == 0) if flags is None else flags[0]
            sp_ = last if flags is None else (flags[1] and last)
            fw.op(pe, lambda l=l, r=r, st=st, sp_=sp_: nc.tensor.matmul(out_ap, l, r, start=st, stop=sp_),
                  reads=reads, writes=writes, mark=last)

    def load_slot(src_ap, nk, width):
        i = slot_i[0] % NSLOT
        slot_i[0] += 1
        fw.dma(pool, wslot[i][:, 0:nk, 0:width], src_ap.rearrange("(k p) c -> p k c", p=128),
               writes=[B_slot[i]], sem=S_slot[i])
        return wslot[i], B_slot[i]

    lp_sem = fw.dsem("lp")
    cs_sem = fw.dsem("cs")

    def ld(dst, src, B, eng=None, sem=None):
        fw.dma(eng or sp, dst, src, writes=[B], sem=sem or ld_sem)

    for dst, src in ((xi_sb[:], xi_d[:, :, :]), (d2_sb[:], d2_d[:, :, :]), (zeta_sb[:], zeta_d[:, :]),
                     (causal_sb[:], causal_d[:, :]), (smask_sb[:], smask_d[:, :]),
                     (invcnt_sb[:], invcnt_d[:, :, :]), (c_sb[:], c_d[:, :]), (gfin_sb[:], gfin_d[:, :])):
        ld(dst, src, B_const)
    ld(ident_sb[:], ident_d[:, :], B_const, eng=pool)
    ld(pswap_sb[:], pswap_d[:, :], B_const, eng=pool)
    fw.op(dve, lambda: nc.vector.memset(ones_sb[:], 1.0), writes=[B_const])
    fw.op(dve, lambda: nc.vector.memset(eps_sb[:], EPS), writes=[B_const])
    const_tok = (ld_sem[2], ld_sem[0], ld_sem[1])
    for e in (pe, act, dve):
        e.wait([const_tok, dve.now()])

    class _Ready(Buf):
        def rdeps(self):
            return []

        def read(self, tok):
            pass
    CONST = _Ready("CONST")
    fw.op(act, lambda: nc.scalar.activation(out=sc_bf[:], in_=c_sb[:], func=AF.Silu), reads=[CONST], writes=[B_c])
    fw.op(dve, lambda: nc.vector.memset(mixb[:], 0.0), writes=B_mix)

    N_ADA_SLOTS = 6 * D // SLOTW

    def ada_slot(l, s):
        pm, Bpm = pbank[PROT], B_pb[PROT]
        wsl, Bs = load_slot(wada_d[l, :, s * SLOTW:(s + 1) * SLOTW], KC, SLOTW)
        nb = SLOTW // 128
        for cb in range(nb):
            mm_group(pm[:, cb:cb + 1],
                     [(wsl[:, kc, cb * 128:(cb + 1) * 128], sc_bf[:, kc:kc + 1]) for kc in range(KC)],
                     reads=[Bs, B_c], writes=[Bpm])
        fw.op(act, lambda: nc.scalar.copy(out=modn_sb[:, s * nb:(s + 1) * nb], in_=pm[:, 0:nb]),
              reads=[Bpm], writes=[B_modn])

    def layer_prologue(l):
        ld(bada_sb[:], bada_d[l, :, :], B_lp, sem=lp_sem)
        ld(gmix_sb[:], gmix_d[l, :, :], B_lp, sem=lp_sem)
        ld(gmlp_sb[:], gmlp_d[l, :, :], B_lp, sem=lp_sem)
        ld(bg_sb[:], bg_d[l, :, :], B_lp, sem=lp_sem)
        ld(spool_sb[:], spool_d[l, :, :], B_lp, sem=lp_sem)
        ld(wg_sb[:], wg_d[l, :, :], B_lp, eng=pool, sem=lp_sem)
        ld(wpool_sb[:], wpool_d[l, :, :, :].rearrange("g c d -> c g d"), B_lp, eng=pool, sem=lp_sem)
        ld(wgz_sb[:], win_d[l, :, PC_GZ:PC_GZ + 16].rearrange("(k p) c -> p k c", p=128), B_lp, eng=pool, sem=lp_sem)
        B_lp.w = (lp_sem[2], lp_sem[0], lp_sem[1])
        fw.op(dve, lambda: nc.vector.tensor_scalar(nbg_sb[:], bg_sb[:], -1.0, None, ALU.mult), reads=[B_lp], writes=[B_lp])
        if l == 0:
            for s in range(N_ADA_SLOTS):
                ada_slot(0, s)
        fw.op(dve, lambda: nc.vector.tensor_tensor(mod_sb[:], modn_sb[:], bada_sb[:], ALU.add),
              reads=[B_modn, B_lp], writes=[B_mod])
        fw.op(dve, lambda: nc.vector.scalar_tensor_tensor(gmod1[:], mod_sb[:, 16:32], 1.0, gmix_sb[:], ALU.add, ALU.mult),
              reads=[B_mod, B_lp], writes=[B_mod])
        fw.op(dve, lambda: nc.vector.scalar_tensor_tensor(gmod2[:], mod_sb[:, 64:80], 1.0, gmlp_sb[:], ALU.add, ALU.mult),
              reads=[B_mod, B_lp], writes=[B_mod])
        for h in range(RH):
            fw.op(dve, lambda h=h: nc.vector.memset(Sret[:, h, :], 0.0), writes=[B_Sret[h]])
            fw.op(dve, lambda h=h: nc.vector.memset(Sret_bf[:, h, :], 0.0), writes=[B_Sretbf[h]])
        for h in range(GH):
            fw.op(dve, lambda h=h: nc.vector.memset(Sgla[:, h, :], 0.0), writes=[B_Sgla[h]])
            fw.op(dve, lambda h=h: nc.vector.memset(Sgla_bf[:, h, :], 0.0), writes=[B_Sglabf[h]])
        for g in range(4):
            fw.op(dve, lambda g=g: nc.vector.memset(halo[:, g, :], 0.0), writes=[B_halo[g]])

    SH1, SC1, GT1, SH2, SC2, GT2 = 0, 16, 32, 48, 64, 80

    def rms_to_h(gmod, shift_off):
        for tt in range(NTT):
            ts = slice(tt * TT, (tt + 1) * TT)
            ss, Bss = pbank[PSTAT], B_pb[PSTAT]
            for kc in range(KC):
                sq, Bsq = sqpool.next()
                if kc % 2 == 0:
                    fw.op(act, lambda kc=kc, sq=sq: nc.scalar.activation(out=sq[:], in_=x_mt[:, kc, ts], func=AF.Square),
                          reads=[B_x], writes=[Bsq])
                else:
                    fw.op(dve, lambda kc=kc, sq=sq: nc.vector.tensor_tensor(sq[:], x_mt[:, kc, ts], x_mt[:, kc, ts], ALU.mult),
                          reads=[B_x], writes=[Bsq])
                fw.op(pe, lambda kc=kc, sq=sq: nc.tensor.matmul(ss[:, :], ones_sb[:, :], sq[:], start=(kc == 0), stop=(kc == KC - 1)),
                      reads=[Bsq, CONST], writes=[Bss], mark=True)
            rs, Brs = rstd_t.next()
            fw.op(act, lambda: nc.scalar.activation(out=rs[:], in_=ss[:, :], func=AF.Ln, bias=eps_sb[:, 0:1], scale=1.0 / D),
                  reads=[Bss, CONST], writes=[Brs])
            fw.op(act, lambda: nc.scalar.activation(out=rs[:], in_=rs[:], func=AF.Exp, scale=-0.5),
                  reads=[Brs], writes=[Brs])
            for kc in range(KC):
                t1, Bt1 = tmpf.next()
                fw.op(dve, lambda kc=kc, t1=t1: nc.vector.scalar_tensor_tensor(t1[:], x_mt[:, kc, ts], gmod[:, kc:kc + 1], rs[:], ALU.mult, ALU.mult),
                      reads=[B_x, B_mod, Brs], writes=[Bt1])
                if shift_off is None:
                    pass
                else:
                    fw.op(act, lambda kc=kc, t1=t1: nc.scalar.activation(out=hT[:, kc, ts], in_=t1[:], func=AF.Identity,
                                                                         bias=mod_sb[:, shift_off + kc:shift_off + kc + 1], scale=1.0),
                          reads=[Bt1, B_mod], writes=[B_h[tt]])

    def proj_F(wsl, Bs, c0, M, tt, nk=KC, src=None, Bsrc=None):
        src = hT if src is None else src
        Bsrc = B_h[tt] if Bsrc is None else Bsrc
        pz, Bpz = next_pz()
        ts = slice(tt * TT, (tt + 1) * TT)
        mm_group(pz[0:M, :], [(wsl[:, kc, c0:c0 + M], src[:, kc, ts]) for kc in range(nk)],
                 reads=[Bs, Bsrc], writes=[Bpz])
        return pz, Bpz

    def proj_T(wsl, Bs, c0, N, ch_list, out_view_fn, Bout, evac_eng="act"):
        per_bank = max(1, 512 // N)
        for g0 in range(0, len(ch_list), per_bank):
            grp = ch_list[g0:g0 + per_bank]
            pz, Bpz = next_pz()
            for gi, ch in enumerate(grp):
                mm_group(pz[:, gi * N:(gi + 1) * N],
                         [(hT[:, kc, ch * CH:(ch + 1) * CH], wsl[:, kc, c0:c0 + N]) for kc in range(KC)],
                         reads=[Bs, B_h[(ch * CH) // TT]], writes=[Bpz])
            dst = out_view_fn(grp)
            srcv = pz[:, 0:len(grp) * N].rearrange("p (c n) -> p c n", n=N)
            fw.op(act, lambda dst=dst, srcv=srcv: nc.scalar.copy(out=dst, in_=srcv), reads=[Bpz], writes=[Bout])
            yield

    arena_bf = arena[:, :].bitcast(BF16)
    SET_BF = 8192

    def set_view(s, off, n):
        return arena_bf[:, s * SET_BF + off: s * SET_BF + off + n]

    def f32_view(off, n):
        return arena[:, 8192 + off: 8192 + off + n]

    def run_mt(l, m):
        tok0 = m * MT
        last_layer = (l == NL - 1)
        if stop == "prologue":
            return
        src = xT_d if l == 0 else xs_d
        fence = fw.fence_tokens()
        B_x.r += fence
        for half in range(2):
            ks = slice(half * 8, (half + 1) * 8)
            fw.dma(sp, x_mt[:, ks, :], src[:, tok0:tok0 + MT].rearrange("(k p) t -> p k t", p=128)[:, ks, :],
                   reads=[B_xs[m]], writes=[B_x], sem=xs_sem)
        B_x.w = (xs_sem[2], xs_sem[0], xs_sem[1])
        fw.dma(sp, cos_sb[:], cos_d[:, tok0:tok0 + MT], writes=[B_cs], sem=cs_sem)
        fw.dma(sp, sin_sb[:], sin_d[:, tok0:tok0 + MT], writes=[B_cs], sem=cs_sem)
        B_cs.w = (cs_sem[2], cs_sem[0], cs_sem[1])

        pz_cur[0] = list(PZ)
        rms_to_h(gmod1, SH1)

        if stop == "norm1":
            return
        seed = B_x.all_tokens()
        set_prev = [list(seed), list(seed)]

        units = []
        G_ROA = (f32_view(4096, 512), Buf("groa", seed))
        G_ROB = (f32_view(4608, 512), Buf("grob", seed))
        G_SQA = (f32_view(5120, 256).bitcast(BF16), Buf("gsqa", seed))
        G_SQB = (f32_view(5376, 256).bitcast(BF16), Buf("gsqb", seed))
        G_RS = (f32_view(5632, 512), Buf("grs", seed))
        G_T = (f32_view(6144, 512), Buf("gt", seed))
        LN_ROF = [(f32_view(1024, 512), Buf("rof0", seed)), (f32_view(1536, 512), Buf("rof1", seed))]
        LN_ROB = (f32_view(2048, 256).bitcast(BF16), Buf("rob", seed))
        LN_MEAN = (f32_view(2304, 512), Buf("lnmean", seed))
        LN_CEN = (f32_view(2816, 512), Buf("lncen", seed))
        LN_SQ = (f32_view(3328, 256).bitcast(BF16), Buf("lnsq", seed))
        LN_RS = (f32_view(3584, 512), Buf("lnrs", seed))
        kz_r = [(f32_view(0, 64).bitcast(BF16), Buf("kz0", seed)), (f32_view(64, 64).bitcast(BF16), Buf("kz1", seed))]
        sT_r = [(f32_view(128, 64).bitcast(BF16), Buf("sT0", seed)), (f32_view(192, 64).bitcast(BF16), Buf("sT1", seed))]
        kst_r = [(f32_view(256, 64).bitcast(BF16), Buf("kst0", seed)), (f32_view(320, 64).bitcast(BF16), Buf("kst1", seed))]
        gsT_r = [(f32_view(384, 64).bitcast(BF16), Buf("gsT0", seed)), (f32_view(448, 64).bitcast(BF16), Buf("gsT1", seed))]

        def make_ret(h, s):
            st = {}

            def A():
                sd = set_prev[s]
                qp = set_view(s, 0, MT); kp = set_view(s, MT, MT); sg = set_view(s, 2 * MT, MT)
                vtm = set_view(s, 3 * MT, MT).rearrange("p (c e) -> p c e", e=128)
                Bq, Bk, Bg, Bv = Buf("qp", sd), Buf("kp", sd), Buf("sg", sd), Buf("vtm", sd)
                st.update(qp=qp, kp=kp, sg=sg, vtm=vtm, Bq=Bq, Bk=Bk, Bg=Bg, Bv=Bv)
                wsl, Bs = load_slot(win_d[l, :, PC_RQK(h):PC_RQK(h) + 256], KC, 256)
                def finish(pz, Bpz, raw, Braw, ts, which):
                    pr, Bpr = pbank[PROT], B_pb[PROT]
                    fw.op(pe, lambda: nc.tensor.matmul(pr[:, :], pswap_sb[:, :], raw[:], start=True, stop=True),
                          reads=[Braw, CONST], writes=[Bpr])
                    t1, Bt1 = tmpf.next()
                    t2, Bt2 = tmpf.next()
                    fw.op(dve, lambda: nc.vector.tensor_tensor(t1[:], pz[:, :], cos_sb[:, ts], ALU.mult),
                          reads=[Bpz, B_cs], writes=[Bt1])
                    fw.op(dve, lambda: nc.vector.tensor_tensor(t2[:], pr[:, :], sin_sb[:, ts], ALU.mult),
                          reads=[Bpr, B_cs], writes=[Bt2])
                    if which == 0:
                        fw.op(dve, lambda: nc.vector.tensor_tensor(t1[:], t1[:], t2[:], ALU.add),
                              reads=[Bt2], writes=[Bt1])
                        fw.op(dve, lambda: nc.vector.tensor_tensor(
                            qp[:, ts].rearrange("p (c i) -> p c i", i=128),
                            t1[:].rearrange("p (c i) -> p c i", i=128),
                            xi_sb[:, h, :].unsqueeze(1).to_broadcast([128, TT // 128, 128]), ALU.mult),
                            reads=[Bt1, CONST], writes=[Bq])
                    else:
                        fw.op(dve, lambda: nc.vector.tensor_tensor(kp[:, ts], t1[:], t2[:], ALU.add),
                              reads=[Bt1, Bt2], writes=[Bk])

                pend = None
                for tt in range(NTT):
                    ts = slice(tt * TT, (tt + 1) * TT)
                    for which in range(2):
                        pz, Bpz = proj_F(wsl, Bs, which * 128, 128, tt)
                        raw, Braw = tmpb.next()
                        fw.op(act, lambda raw=raw, pz=pz: nc.scalar.copy(out=raw[:], in_=pz[:, :]), reads=[Bpz], writes=[Braw])
                        if pend is not None:
                            finish(*pend)
                        pend = (pz, Bpz, raw, Braw, ts, which)
                        yield
                wsl, Bs = load_slot(win_d[l, :, PC_RGV(h):PC_RGV(h) + 256], KC, 256)
                for tt in range(NTT):
                    ts = slice(tt * TT, (tt + 1) * TT)
                    pz, Bpz = proj_F(wsl, Bs, 0, 128, tt)
                    if pend is not None:
                        finish(*pend)
                        pend = None
                    fw.op(act, lambda pz=pz: nc.scalar.activation(out=sg[:, ts], in_=pz[:, :], func=AF.Silu), reads=[Bpz], writes=[Bg])
                    yield
                yield from proj_T(wsl, Bs, 128, 128, list(range(NCH)), lambda grp: vtm[:, grp[0]:grp[0] + len(grp), :], Bv)

            def B():
                qp, kp, sg, vtm = st["qp"], st["kp"], st["sg"], st["vtm"]
                Bq, Bk, Bg, Bv = st["Bq"], st["Bk"], st["Bg"], st["Bv"]
                pre = {}

                def emit_i(ch):
                    cs_ = slice(ch * CH, (ch + 1) * CH)
                    ti = tr_i[0] % 2; tr_i[0] += 1
                    trp = tr_ps_ret[:, ti * 128:(ti + 1) * 128]
                    Btr = B_pb[PROB]
                    fw.op(pe, lambda: nc.tensor.transpose(trp, kp[:, cs_], ident_sb[:, :]),
                          reads=[Bk, CONST], writes=[Btr])
                    kz, Bkz = kz_r[ch % 2]
                    fw.op(act, lambda: nc.scalar.mul(kz, trp, zeta_sb[:, h:h + 1]),
                          reads=[Btr, CONST], writes=[Bkz])
                    si = sc_i[0] % 4; sc_i[0] += 1
                    scp = pbank[PSC][:, si * 128:(si + 1) * 128]
                    fw.op(pe, lambda: nc.tensor.matmul(scp, kp[:, cs_], qp[:, cs_], start=True, stop=True),
                          reads=[Bk, Bq], writes=[B_sc[si]])
                    sT, BsT = sT_r[ch % 2]
                    fw.op(dve, lambda: nc.vector.tensor_tensor(sT, scp, d2_sb[:, h, :], ALU.mult),
                          reads=[B_sc[si], CONST], writes=[BsT])
                    pre[ch] = (kz, Bkz, sT, BsT)

                ro, Bro = pbank[PROA], B_pb[PROA]

                def ln_gen(grp):
                    gs = slice(grp * TT, (grp + 1) * TT)
                    rof, Brof = LN_ROF[grp % 2]
                    rob, Brob = LN_ROB
                    mean, Bmean = LN_MEAN
                    cen, Bcen = LN_CEN
                    sq, Bsq = LN_SQ
                    rs, Brs = LN_RS
                    stp, Bst = pbank[PSTAT], B_pb[PSTAT]
                    fw.op(act, lambda: nc.scalar.copy(out=rob, in_=ro[:, :]), reads=[Bro], writes=[Brob])
                    fw.op(act, lambda: nc.scalar.copy(out=rof, in_=ro[:, :]), reads=[Bro], writes=[Brof])
                    fw.op(pe, lambda: nc.tensor.matmul(stp[:, :], ones_sb[:, :], rob, start=True, stop=True),
                          reads=[Brob, CONST], writes=[Bst])
                    yield
                    fw.op(act, lambda: nc.scalar.mul(mean, stp[:, :], 1.0 / 128), reads=[Bst], writes=[Bmean])
                    fw.op(dve, lambda: nc.vector.tensor_tensor(cen, rof, mean, ALU.subtract),
                          reads=[Brof, Bmean], writes=[Bcen])
                    fw.op(act, lambda: nc.scalar.activation(out=sq, in_=cen, func=AF.Square), reads=[Bcen], writes=[Bsq])
                    fw.op(pe, lambda: nc.tensor.matmul(stp[:, :], ones_sb[:, :], sq, start=True, stop=True),
                          reads=[Bsq, CONST], writes=[Bst])
                    yield
                    fw.op(act, lambda: nc.scalar.activation(out=rs, in_=stp[:, :], func=AF.Ln, bias=eps_sb[:, 0:1], scale=1.0 / 128),
                          reads=[Bst, CONST], writes=[Brs])
                    fw.op(act, lambda: nc.scalar.activation(out=rs, in_=rs, func=AF.Exp, scale=-0.5), reads=[Brs], writes=[Brs])
                    fw.op(dve, lambda: nc.vector.tensor_tensor(cen, cen, rs, ALU.mult), reads=[Brs], writes=[Bcen])
                    fw.op(dve, lambda: nc.vector.tensor_tensor(mixb[:, h, gs], cen, sg[:, gs], ALU.mult),
                          reads=[Bcen, Bg], writes=[B_mix[h]])
                    yield

                pending_ln = iter(())
                emit_i(0)
                yield
                for grp in range(NCH // 4):
                    for ci in range(4):
                        ch = grp * 4 + ci
                        cs_ = slice(ch * CH, (ch + 1) * CH)
                        if ch + 1 < NCH:
                            emit_i(ch + 1)
                        kz, Bkz, sT, BsT = pre.pop(ch)
                        next(pending_ln, None)
                        rov = ro[:, ci * 128:(ci + 1) * 128]
                        fw.op(pe, lambda rov=rov, sT=sT: nc.tensor.matmul(rov, vtm[:, ch, :], sT, start=True, stop=False),
                              reads=[Bv, BsT], writes=[Bro], mark=False)
                        fw.op(pe, lambda rov=rov: nc.tensor.matmul(rov, Sret_bf[:, h, :], qp[:, cs_], start=False, stop=True),
                              reads=[Bv, BsT, B_Sretbf[h], Bq], writes=[Bro])
                        ui = u_i[0] % 2; u_i[0] += 1
                        up = pbank[PU][:, ui * 192:ui * 192 + 128]
                        fw.op(pe, lambda up=up, kz=kz: nc.tensor.matmul(up, kz, vtm[:, ch, :], start=True, stop=True),
                              reads=[Bkz, Bv], writes=[B_u[ui]])
                        fw.op(dve, lambda up=up: nc.vector.scalar_tensor_tensor(Sret[:, h, :], Sret[:, h, :], G128[h], up, ALU.mult, ALU.add),
                              reads=[B_u[ui]], writes=[B_Sret[h]])
                        fw.op(act, lambda: nc.scalar.copy(out=Sret_bf[:, h, :], in_=Sret[:, h, :]),
                              reads=[B_Sret[h]], writes=[B_Sretbf[h]])
                        yield
                    pending_ln = ln_gen(grp)
                    next(pending_ln)
                for _ in pending_ln:
                    yield
                toks = []
                for b in (Bq, Bk, Bg, Bv):
                    toks += b.all_tokens()
                set_prev[s] = toks
            return A, B

        def make_pool(s):
            st = {}

            def A():
                return
                yield

            def B():
                sd = set_prev[s]
                EXT = MT + 16
                bufA = set_view(s, 0, 2 * EXT).bitcast(F32)
                bufB = set_view(s, 2 * EXT, 2 * EXT).bitcast(F32)
                bufU = set_view(s, 4 * EXT, 2 * EXT).bitcast(F32)
                pooled = set_view(s, 6 * EXT, MT)
                BA, BB, BU, BP = Buf("pA", sd), Buf("pB", sd), Buf("pU", sd), Buf("pP", sd)
                for g in range(4):
                    if g % 2 == 0:
                        wsl, Bs = load_slot(win_d[l, :, PC_POOL(g // 2):PC_POOL(g // 2) + 256], KC, 256)
                        st["w"] = (wsl, Bs)
                    wsl, Bs = st["w"]
                    w = 2 << g
                    fw.op(act, lambda g=g: nc.scalar.copy(out=bufU[:, 0:16], in_=halo[:, g, :]), reads=[B_halo[g]], writes=[BU])
                    for tt in range(NTT):
                        pz, Bpz = proj_F(wsl, Bs, (g % 2) * 128, 128, tt)
                        fw.op(act, lambda pz=pz, tt=tt: nc.scalar.copy(out=bufU[:, 16 + tt * TT:16 + (tt + 1) * TT], in_=pz[:, :]),
                              reads=[Bpz], writes=[BU])
                        yield
                    fw.op(act, lambda g=g: nc.scalar.copy(out=halo[:, g, :], in_=bufU[:, MT:MT + 16]), reads=[BU], writes=[B_halo[g]])
                    cur, Bcur = bufU, BU
                    step = 1
                    pp = [(bufA, BA), (bufB, BB)]
                    k = 0
                    while step < w:
                        nxt, Bnxt = pp[k % 2]; k += 1
                        fw.op(dve, lambda cur=cur, nxt=nxt, step=step: nc.vector.tensor_tensor(nxt[:, step:EXT], cur[:, step:EXT], cur[:, 0:EXT - step], ALU.add),
                              reads=[Bcur], writes=[Bnxt])
                        cur, Bcur = nxt, Bnxt
                        step *= 2
                    fw.op(dve, lambda cur=cur, w=w: nc.vector.scalar_tensor_tensor(pooled[:, :], cur[:, 16:EXT], 1.0 / w, bufU[:, 16:EXT], ALU.mult, ALU.subtract),
                          reads=[Bcur, BU], writes=[BP])
                    if m == 0:
                        t1, Bt1 = tmpf.next()
                        fw.op(dve, lambda cur=cur, t1=t1, g=g: nc.vector.tensor_tensor(t1[:, 0:16], cur[:, 16:32], invcnt_sb[:, g, :], ALU.mult),
                              reads=[Bcur, CONST], writes=[Bt1])
                        fw.op(dve, lambda t1=t1: nc.vector.tensor_tensor(pooled[:, 0:16], t1[:, 0:16], bufU[:, 16:32], ALU.subtract),
                              reads=[Bt1, BU], writes=[BP])
                    for tt in range(NTT):
                        ts = slice(tt * TT, (tt + 1) * TT)
                        pz, Bpz = next_pz()
                        fw.op(pe, lambda pz=pz, g=g, ts=ts: nc.tensor.matmul(pz[:, :], wpool_sb[:, g, :], pooled[:, ts], start=True, stop=True),
                              reads=[BP, B_lp], writes=[Bpz])
                        fw.op(act, lambda pz=pz, g=g, ts=ts: nc.scalar.mul(mixb[:, RH + g, ts], pz[:, :], spool_sb[:, g:g + 1]),
                              reads=[Bpz, B_lp], writes=[B_mix[RH + g]])
                    yield
                toks = []
                for b in (BA, BB, BU, BP):
                    toks += b.all_tokens()
                set_prev[s] = toks
            return A, B

        def make_gla(h, s):
            st = {}

            def A():
                sd = set_prev[s]
                qd = set_view(s, 0, MT); ki = set_view(s, MT, MT); ks = set_view(s, 2 * MT, MT)
                sga = set_view(s, 3 * MT, MT); sgb = set_view(s, 4 * MT, MT)
                vtm = set_view(s, 5 * MT, NCH * DV).rearrange("p (c e) -> p c e", e=DV)
                gch = set_view(s, 5 * MT + NCH * DV, 2 * NCH).bitcast(F32)
                Bqd, Bki, Bks, Bsga, Bsgb, Bv, Bgch = (Buf(n, sd) for n in ("qd", "ki", "ks", "sga", "sgb", "gv", "gch"))
                st.update(qd=qd, ki=ki, ks=ks, sga=sga, sgb=sgb, vtm=vtm, gch=gch,
                          Bqd=Bqd, Bki=Bki, Bks=Bks, Bsga=Bsga, Bsgb=Bsgb, Bv=Bv, Bgch=Bgch)
                wsl, Bs = load_slot(win_d[l, :, PC_GQK(h):PC_GQK(h) + 192], KC, 192)
                for tt in range(NTT):
                    ts = slice(tt * TT, (tt + 1) * TT)
                    pg, Bpg = next_pz()
                    fw.op(pe, lambda pg=pg, ts=ts: nc.tensor.matmul(pg[0:DK, :], wg_sb[:, h * DK:(h + 1) * DK], st_gz[0][0:16, ts], start=True, stop=True),
                          reads=[B_lp, st_gz[1]], writes=[Bpg])
                    e1, Be1 = tmpf.next()
                    fw.op(act, lambda pg=pg, e1=e1: nc.scalar.activation(out=e1[0:DK, :], in_=pg[0:DK, :], func=AF.Exp, bias=nbg_sb[0:DK, h:h + 1], scale=-1.0),
                          reads=[Bpg, B_lp], writes=[Be1])
                    fw.op(act, lambda e1=e1: nc.scalar.activation(out=e1[0:DK, :], in_=e1[0:DK, :], func=AF.Ln, bias=1.0, scale=1.0),
                          reads=[Be1], writes=[Be1])
                    cs, Bcs = tmpf.next()
                    fw.op(dve, lambda e1=e1, cs=cs: nc.vector.tensor_tensor_scan(cs[0:DK, :], smask_sb[0:DK, 0:TT], e1[0:DK, :], 0.0, ALU.mult, ALU.add),
                          reads=[Be1, CONST], writes=[Bcs])
                    eb, Beb = tmpf.next()
                    fw.op(act, lambda eb=eb, cs=cs: nc.scalar.activation(out=eb[0:DK, :], in_=cs[0:DK, :], func=AF.Exp, scale=-1.0 / 16),
                          reads=[Bcs], writes=[Beb])
                    fw.op(act, lambda cs=cs: nc.scalar.activation(out=cs[0:DK, :], in_=cs[0:DK, :], func=AF.Exp, scale=1.0 / 16),
                          reads=[Beb], writes=[Bcs])
                    fw.op(act, lambda eb=eb, tt=tt: nc.scalar.copy(out=gch[0:DK, tt * 4:(tt + 1) * 4], in_=eb[0:DK, 127::128]),
                          reads=[Beb], writes=[Bgch])
                    yield
                    pq, Bpq = proj_F(wsl, Bs, 0, DK, tt)
                    fw.op(dve, lambda pq=pq, eb=eb, ts=ts: nc.vector.scalar_tensor_tensor(qd[0:DK, ts], pq[0:DK, :], float(DK) ** -0.5, eb[0:DK, :], ALU.mult, ALU.mult),
                          reads=[Bpq, Beb], writes=[Bqd])
                    yield
                    pk, Bpk = proj_F(wsl, Bs, DK, DK, tt)
                    fw.op(dve, lambda pk=pk, cs=cs, ts=ts: nc.vector.tensor_tensor(ki[0:DK, ts], pk[0:DK, :], cs[0:DK, :], ALU.mult),
                          reads=[Bpk, Bcs], writes=[Bki])
                    fw.op(dve, lambda eb=eb, ts=ts: nc.vector.tensor_tensor(
                        ks[0:DK, ts].rearrange("p (c i) -> p c i", i=128),
                        ki[0:DK, ts].rearrange("p (c i) -> p c i", i=128),
                        eb[0:DK, 127::128].unsqueeze(2).to_broadcast([DK, TT // 128, 128]), ALU.mult),
                        reads=[Bki, Beb], writes=[Bks])
                    yield
                wsl, Bs = load_slot(win_d[l, :, PC_GV(h):PC_GV(h) + 192], KC, 192)
                yield from proj_T(wsl, Bs, 0, DV, list(range(NCH)), lambda grp: vtm[:, grp[0]:grp[0] + len(grp), :], Bv)
                wsl, Bs = load_slot(win_d[l, :, PC_GR(h):PC_GR(h) + 192], KC, 192)
                for tt in range(NTT):
                    ts = slice(tt * TT, (tt + 1) * TT)
                    pz, Bpz = proj_F(wsl, Bs, 0, 128, tt)
                    fw.op(act, lambda pz=pz, ts=ts: nc.scalar.activation(out=sga[:, ts], in_=pz[:, :], func=AF.Silu), reads=[Bpz], writes=[Bsga])
                    yield
                    pz, Bpz = proj_F(wsl, Bs, 128, 64, tt)
                    fw.op(act, lambda pz=pz, ts=ts: nc.scalar.activation(out=sgb[0:64, ts], in_=pz[0:64, :], func=AF.Silu), reads=[Bpz], writes=[Bsgb])
                    yield

            def B():
                qd, ki, ks, sga, sgb, vtm, gch = (st[k] for k in ("qd", "ki", "ks", "sga", "sgb", "vtm", "gch"))
                Bqd, Bki, Bks, Bsga, Bsgb, Bv, Bgch = (st[k] for k in ("Bqd", "Bki", "Bks", "Bsga", "Bsgb", "Bv", "Bgch"))
                sT_r = gsT_r
                ka, kb = 10 + 2 * h, 11 + 2 * h
                pre = {}

                def emit_i(ch):
                    cs_ = slice(ch * CH, (ch + 1) * CH)
                    ti = tr_i[0] % 2; tr_i[0] += 1
                    trp = tr_ps_gla[:, ti * 128:ti * 128 + DK]
                    Btr = B_pb[PROT]
                    fw.op(pe, lambda: nc.tensor.transpose(trp, ks[0:DK, cs_], ident_sb[0:DK, 0:DK]),
                          reads=[Bks, CONST], writes=[Btr])
                    kst, Bkst = kst_r[ch % 2]
                    fw.op(act, lambda: nc.scalar.copy(out=kst[:, 0:DK], in_=trp), reads=[Btr], writes=[Bkst])
                    si = sc_i[0] % 4; sc_i[0] += 1
                    scp = pbank[PSC][:, si * 128:(si + 1) * 128]
                    fw.op(pe, lambda: nc.tensor.matmul(scp, ki[0:DK, cs_], qd[0:DK, cs_], start=True, stop=True),
                          reads=[Bki, Bqd], writes=[B_sc[si]])
                    sT, BsT = sT_r[ch % 2]
                    fw.op(dve, lambda: nc.vector.tensor_tensor(sT, scp, causal_sb[:, :], ALU.mult),
                          reads=[B_sc[si], CONST], writes=[BsT])
                    pre[ch] = (kst, Bkst, sT, BsT)

                roA, BroA = pbank[PROA], B_pb[PROA]
                roB, BroB = pbank[PROB], B_pb[PROB]

                def rms_gen(grp):
                    gs = slice(grp * TT, (grp + 1) * TT)
                    raf, Braf = G_ROA
                    rbf, Brbf = G_ROB
                    sqa, Bsqa = G_SQA
                    sqb, Bsqb = G_SQB
                    rs, Brs = G_RS
                    t1, Bt1 = G_T
                    stp, Bst = pbank[PSTAT], B_pb[PSTAT]
                    fw.op(act, lambda: nc.scalar.activation(out=sqa, in_=roA[:, :], func=AF.Square), reads=[BroA], writes=[Bsqa])
                    fw.op(act, lambda: nc.scalar.activation(out=sqb[0:64, :], in_=roB[0:64, :], func=AF.Square), reads=[BroB], writes=[Bsqb])
                    fw.op(act, lambda: nc.scalar.copy(out=raf, in_=roA[:, :]), reads=[BroA], writes=[Braf])
                    fw.op(act, lambda: nc.scalar.copy(out=rbf[0:64, :], in_=roB[0:64, :]), reads=[BroB], writes=[Brbf])
                    fw.op(pe, lambda: nc.tensor.matmul(stp[:, :], ones_sb[:, :], sqa, start=True, stop=False),
                          reads=[Bsqa, CONST], writes=[Bst], mark=False)
                    fw.op(pe, lambda: nc.tensor.matmul(stp[:, :], ones_sb[0:64, :], sqb[0:64, :], start=False, stop=True),
                          reads=[Bsqa, Bsqb, CONST], writes=[Bst])
                    yield
                    fw.op(act, lambda: nc.scalar.activation(out=rs, in_=stp[:, :], func=AF.Ln, bias=eps_sb[:, 0:1], scale=1.0 / DV),
                          reads=[Bst, CONST], writes=[Brs])
                    fw.op(act, lambda: nc.scalar.activation(out=rs, in_=rs, func=AF.Exp, scale=-0.5), reads=[Brs], writes=[Brs])
                    yield
                    fw.op(dve, lambda: nc.vector.tensor_tensor(t1, raf, rs, ALU.mult), reads=[Braf, Brs], writes=[Bt1])
                    fw.op(dve, lambda: nc.vector.tensor_tensor(mixb[:, ka, gs], t1, sga[:, gs], ALU.mult), reads=[Bt1, Bsga], writes=[B_mix[ka]])
                    fw.op(dve, lambda: nc.vector.tensor_tensor(t1[0:64, :], rbf[0:64, :], rs[0:64, :], ALU.mult), reads=[Brbf, Brs], writes=[Bt1])
                    fw.op(dve, lambda: nc.vector.tensor_tensor(mixb[0:64, kb, gs], t1[0:64, :], sgb[0:64, gs], ALU.mult), reads=[Bt1, Bsgb], writes=[B_mix[kb]])
                    yield

                pending_rms = iter(())
                emit_i(0)
                yield
                for grp in range(NCH // 4):
                    for ci in range(4):
                        ch = grp * 4 + ci
                        cs_ = slice(ch * CH, (ch + 1) * CH)
                        if ch + 1 < NCH:
                            emit_i(ch + 1)
                        kst, Bkst, sT, BsT = pre.pop(ch)
                        next(pending_rms, None)
                        ra = roA[:, ci * 128:(ci + 1) * 128]
                        rb = roB[0:64, ci * 128:(ci + 1) * 128]
                        fw.op(pe, lambda ra=ra, sT=sT: nc.tensor.matmul(ra, vtm[:, ch, 0:128], sT, start=True, stop=False),
                              reads=[Bv, BsT], writes=[BroA], mark=False)
                        fw.op(pe, lambda ra=ra: nc.tensor.matmul(ra, Sgla_bf[0:DK, h, 0:128], qd[0:DK, cs_], start=False, stop=True),
                              reads=[Bv, BsT, B_Sglabf[h], Bqd], writes=[BroA])
                        fw.op(pe, lambda rb=rb, sT=sT: nc.tensor.matmul(rb, vtm[:, ch, 128:192], sT, start=True, stop=False),
                              reads=[Bv, BsT], writes=[BroB], mark=False)
                        fw.op(pe, lambda rb=rb: nc.tensor.matmul(rb, Sgla_bf[0:DK, h, 128:192], qd[0:DK, cs_], start=False, stop=True),
                              reads=[Bv, BsT, B_Sglabf[h], Bqd], writes=[BroB])
                        ui = u_i[0] % 2; u_i[0] += 1
                        up = pbank[PU][0:DK, ui * 192:(ui + 1) * 192]
                        fw.op(pe, lambda up=up, kst=kst: nc.tensor.matmul(up, kst[:, 0:DK], vtm[:, ch, :], start=True, stop=True),
                              reads=[Bkst, Bv], writes=[B_u[ui]])
                        fw.op(dve, lambda up=up, ch=ch: nc.vector.scalar_tensor_tensor(Sgla[0:DK, h, :], Sgla[0:DK, h, :], gch[0:DK, ch:ch + 1], up, ALU.mult, ALU.add),
                              reads=[B_u[ui], Bgch], writes=[B_Sgla[h]])
                        fw.op(act, lambda: nc.scalar.copy(out=Sgla_bf[0:DK, h, :], in_=Sgla[0:DK, h, :]),
                              reads=[B_Sgla[h]], writes=[B_Sglabf[h]])
                        yield
                    pending_rms = rms_gen(grp)
                    next(pending_rms)
                for _ in pending_rms:
                    yield
                toks = []
                for b in (Bqd, Bki, Bks, Bsga, Bsgb, Bv, Bgch):
                    toks += b.all_tokens()
                set_prev[s] = toks
            return A, B

        gzT = f32_view(512, MT // 2).bitcast(BF16)
        B_gz = Buf("gzT", seed)
        st_gz = (gzT, B_gz)

        def gz_A():
            for tt in range(NTT):
                ts = slice(tt * TT, (tt + 1) * TT)
                pz, Bpz = next_pz()
                mm_group(pz[0:16, :], [(wgz_sb[:, kc, :], hT[:, kc, ts]) for kc in range(KC)], reads=[B_lp, B_h[tt]], writes=[Bpz])
                fw.op(act, lambda pz=pz, ts=ts: nc.scalar.copy(out=gzT[0:16, ts], in_=pz[0:16, :]), reads=[Bpz], writes=[B_gz])
                yield

        ui_ = 0
        kinds = []
        for h in range(RH):
            units.append(make_ret(h, ui_ % 2)); ui_ += 1; kinds.append("ret")
        units.append(make_pool(ui_ % 2)); ui_ += 1; kinds.append("pool")
        gla_first = len(units)
        for h in range(GH):
            units.append(make_gla(h, ui_ % 2)); ui_ += 1; kinds.append("gla")

        if stop and stop.startswith("units"):
            units = units[:int(stop[5:].rstrip("A"))]
        NA = {"ret": 8, "pool": 0, "gla": 14}
        NB = {"ret": 11, "pool": 12, "gla": 11}

        def chain(*gens):
            for g in gens:
                yield from g

        for _ in units[0][0]():
            pass
        if stop and stop.endswith("A"):
            units = []
        for ui2 in range(len(units)):
            genB = units[ui2][1]()
            if ui2 + 1 < len(units):
                na = NA[kinds[ui2 + 1]]
                if ui2 + 1 == gla_first:
                    genA = chain(gz_A(), units[ui2 + 1][0]())
                    na += 2
                else:
                    genA = units[ui2 + 1][0]()
            else:
                genA = iter(())
                na = 0
            ratio = na / NB[kinds[ui2]]
            acc = 0.0
            for _ in genB:
                acc += ratio
                while acc >= 1.0:
                    acc -= 1.0
                    next(genA, None)
            for _ in genA:
                pass

        fence = fw.fence_tokens()
        B_x.r = list(fence)
        B_x.w = None
        for half in range(2):
            ks_ = slice(half * 8, (half + 1) * 8)
            fw.dma(sp, x_mt[:, ks_, :], src[:, tok0:tok0 + MT].rearrange("(k p) t -> p k t", p=128)[:, ks_, :],
                   reads=[B_xs[m]], writes=[B_x], sem=xs_sem)
        B_x.w = (xs_sem[2], xs_sem[0], xs_sem[1])

        if stop and (stop.startswith("units") or stop == "mixers"):
            for half in range(2):
                ks_ = slice(half * 8, (half + 1) * 8)
                fw.dma(sp, yT_d[:, tok0:tok0 + MT].rearrange("(k p) t -> p k t", p=128)[:, ks_, :], x_mt[:, ks_, :],
                       reads=[B_x], writes=[B_xs[m]], sem=xs_sem)
            return
        pz_cur[0] = [0, 1, PSC, PROA, PROB, PU]
        for sl in range(D // SLOTW):
            wsl, Bs = load_slot(wout_d[l, :, sl * SLOTW:(sl + 1) * SLOTW], NKO, SLOTW)
            for cb in range(SLOTW // 128):
                rb = sl * (SLOTW // 128) + cb
                for tt in range(NTT):
                    ts = slice(tt * TT, (tt + 1) * TT)
                    pz, Bpz = next_pz()
                    mm_group(pz[:, :], [(wsl[:, k, cb * 128:(cb + 1) * 128], mixb[:, k, ts]) for k in range(NKO)],
                             reads=[Bs] + B_mix, writes=[Bpz])
                    fw.op(dve, lambda pz=pz, rb=rb, ts=ts: nc.vector.scalar_tensor_tensor(
                        x_mt[:, rb, ts], pz[:, :], mod_sb[:, GT1 + rb:GT1 + rb + 1], x_mt[:, rb, ts], ALU.mult, ALU.add),
                        reads=[Bpz, B_mod], writes=[B_x])

        if stop == "outproj":
            for half in range(2):
                ks_ = slice(half * 8, (half + 1) * 8)
                fw.dma(sp, yT_d[:, tok0:tok0 + MT].rearrange("(k p) t -> p k t", p=128)[:, ks_, :], x_mt[:, ks_, :],
                       reads=[B_x], writes=[B_xs[m]], sem=xs_sem)
            return
        rms_to_h(gmod2, SH2)

        hid = mixb
        B_hid = B_mix
        ada_todo = []
        if l + 1 < NL:
            per = (N_ADA_SLOTS + NMT - 1) // NMT
            ada_todo = list(range(m * per, min(N_ADA_SLOTS, (m + 1) * per)))
        n_iter = 2 * (DFF // D) * (D // SLOTW)
        ada_every = max(1, n_iter // max(1, len(ada_todo))) if ada_todo else 0
        it_ctr = [0]

        def ada_tick():
            it_ctr[0] += 1
            if ada_todo and it_ctr[0] % ada_every == 0:
                ada_slot(l + 1, ada_todo.pop(0))

        for hb in range(DFF // D):
            for sl in range(D // SLOTW):
                c0 = hb * D + sl * SLOTW
                wsl, Bs = load_slot(wup_d[l, :, c0:c0 + SLOTW], KC, SLOTW)
                for cb in range(SLOTW // 128):
                    kk = sl * (SLOTW // 128) + cb
                    for tt in range(NTT):
                        ts = slice(tt * TT, (tt + 1) * TT)
                        pz, Bpz = proj_F(wsl, Bs, cb * 128, 128, tt)
                        r, Br = tmpf.next()
                        fw.op(act, lambda pz=pz, r=r: nc.scalar.activation(out=r[:], in_=pz[:, :], func=AF.Relu), reads=[Bpz], writes=[Br])
                        fw.op(dve, lambda r=r, kk=kk, ts=ts: nc.vector.tensor_tensor(hid[:, kk, ts], r[:], r[:], ALU.mult),
                              reads=[Br], writes=[B_hid[kk]])
                ada_tick()
            for sl in range(D // SLOTW):
                wsl, Bs = load_slot(wdn_d[l, hb * D:(hb + 1) * D, sl * SLOTW:(sl + 1) * SLOTW], KC, SLOTW)
                for cb in range(SLOTW // 128):
                    rb = sl * (SLOTW // 128) + cb
                    for tt in range(NTT):
                        ts = slice(tt * TT, (tt + 1) * TT)
                        pz, Bpz = next_pz()
                        mm_group(pz[:, :], [(wsl[:, k, cb * 128:(cb + 1) * 128], hid[:, k, ts]) for k in range(KC)],
                                 reads=[Bs] + B_hid[0:KC], writes=[Bpz])
                        fw.op(dve, lambda pz=pz, rb=rb, ts=ts: nc.vector.scalar_tensor_tensor(
                            x_mt[:, rb, ts], pz[:, :], mod_sb[:, GT2 + rb:GT2 + rb + 1], x_mt[:, rb, ts], ALU.mult, ALU.add),
                            reads=[Bpz, B_mod], writes=[B_x])
                ada_tick()
        while ada_todo:
            ada_slot(l + 1, ada_todo.pop(0))

        if last_layer and final_norm:
            for tt in range(NTT):
                ts = slice(tt * TT, (tt + 1) * TT)
                ss, Bss = pbank[PSTAT], B_pb[PSTAT]
                for kc in range(KC):
                    sq, Bsq = tmpb.next()
                    fw.op(act, lambda kc=kc, sq=sq: nc.scalar.activation(out=sq[:], in_=x_mt[:, kc, ts], func=AF.Square),
                          reads=[B_x], writes=[Bsq])
                    fw.op(pe, lambda kc=kc, sq=sq: nc.tensor.matmul(ss[:, :], ones_sb[:, :], sq[:], start=(kc == 0), stop=(kc == KC - 1)),
                          reads=[Bsq, CONST], writes=[Bss], mark=True)
                rs, Brs = rstd_t.next()
                fw.op(act, lambda rs=rs: nc.scalar.activation(out=rs[:], in_=ss[:, :], func=AF.Ln, bias=eps_sb[:, 0:1], scale=1.0 / D),
                      reads=[Bss, CONST], writes=[Brs])
                fw.op(act, lambda rs=rs: nc.scalar.activation(out=rs[:], in_=rs[:], func=AF.Exp, scale=-0.5), reads=[Brs], writes=[Brs])
                for kc in range(KC):
                    fw.op(dve, lambda kc=kc, rs=rs: nc.vector.scalar_tensor_tensor(x_mt[:, kc, ts], x_mt[:, kc, ts], gfin_sb[:, kc:kc + 1], rs[:], ALU.mult, ALU.mult),
                          reads=[Brs, CONST], writes=[B_x])
            dst = yT_d
        else:
            dst = yT_d if last_layer else xs_d
        for half in range(2):
            ks_ = slice(half * 8, (half + 1) * 8)
            fw.dma(sp, dst[:, tok0:tok0 + MT].rearrange("(k p) t -> p k t", p=128)[:, ks_, :], x_mt[:, ks_, :],
                   reads=[B_x], writes=[B_xs[m]], sem=xs_sem)
        B_xs[m].w = (xs_sem[2], xs_sem[0], xs_sem[1])

    for l in range(NL):
        layer_prologue(l)
        for m in range(NMT):
            run_mt(l, m)
    sp.wait(fw.fence_tokens())
    return nc


def prep_shared(inputs, NL):
    f = lambda a: np.ascontiguousarray(np.asarray(a, dtype=np.float32))
    perm = in_col_perm()
    rows = out_row_map()
    w_out = np.asarray(inputs["w_out"], dtype=np.float32)[:NL]
    w_out_p = np.zeros((NL, NKO * 128, D), np.float32)
    valid = rows >= 0
    w_out_p[:, valid, :] = w_out[:, rows[valid], :]
    sh = {
        "w_ada": f(np.asarray(inputs["w_ada"])[:NL]),
        "b_ada_fm": f(np.asarray(inputs["b_ada"])[:NL].reshape(NL, 96, 128).transpose(0, 2, 1)),
        "g_mix_fm": f(np.asarray(inputs["g_mix"])[:NL].reshape(NL, KC, 128).transpose(0, 2, 1)),
        "g_mlp_fm": f(np.asarray(inputs["g_mlp"])[:NL].reshape(NL, KC, 128).transpose(0, 2, 1)),
        "w_in_p": f(np.asarray(inputs["w_in"])[:NL][:, :, perm]),
        "w_gate_up": f(np.asarray(inputs["w_gate_up"])[:NL]),
        "w_pool": f(np.asarray(inputs["w_pool"])[:NL]),
        "s_pool_fm": f(np.asarray(inputs["s_pool"])[:NL].reshape(NL, 4, 128).transpose(0, 2, 1)),
        "w_out_p": w_out_p,
        "w_up": f(np.asarray(inputs["w_up"])[:NL]),
        "w_down": f(np.asarray(inputs["w_down"])[:NL]),
        "g_final_fm": f(np.asarray(inputs["g_final"]).reshape(KC, 128).T),
    }
    bg = np.zeros((NL, 128, GH), np.float32)
    bg[:, :DK, :] = np.asarray(inputs["b_gate"], dtype=np.float32)[:NL].reshape(NL, GH, DK).transpose(0, 2, 1)
    sh["b_gate_fm"] = bg
    return sh


def run_cores(nc, shared, x_list, c_list, pos0_list, seqstart_list):
    in_maps = []
    for x, c, p0, ss in zip(x_list, c_list, pos0_list, seqstart_list):
        m = dict(shared)
        m["xT"] = np.ascontiguousarray(np.asarray(x, dtype=np.float32).T)
        m["c_fm"] = np.ascontiguousarray(np.asarray(c, dtype=np.float32).reshape(KC, 128).T)
        m.update(host_tables(p0, x.shape[0], ss))
        in_maps.append(m)
    res = run_bass_kernel_spmd(nc, in_maps, core_ids=list(range(len(in_maps))))
    return [np.ascontiguousarray(r["yT"].T) for r in res.results]


_CACHE = {}


def kernel(x, c, w_ada, b_ada, g_mix, g_mlp, w_in, w_gate_up, b_gate, w_pool, s_pool,
           w_out, w_up, w_down, g_final):
    inputs = dict(w_ada=w_ada, b_ada=b_ada, g_mix=g_mix, g_mlp=g_mlp, w_in=w_in, w_gate_up=w_gate_up,
                  b_gate=b_gate, w_pool=w_pool, s_pool=s_pool, w_out=w_out, w_up=w_up, w_down=w_down,
                  g_final=g_final)
    x = np.asarray(x, dtype=np.float32)
    c = np.asarray(c, dtype=np.float32)
    B, S, _ = x.shape
    NL = 4
    NMT = S // MT
    key = (NL, NMT)
    if key not in _CACHE:
        _CACHE[key] = build_program(NL, NMT)
    nc = _CACHE[key]
    shared = prep_shared(inputs, NL)
    real = {0: 0, 2: 1, 4: 2, 6: 3}
    tabs = host_tables(0, S, True)
    zero_map = None
    in_maps = []
    for core in range(8):
        if core in real:
            b = real[core]
            mm = dict(shared)
            mm["xT"] = np.ascontiguousarray(x[b].T)
            mm["c_fm"] = np.ascontiguousarray(c[b].reshape(KC, 128).T)
            mm.update(tabs)
            in_maps.append(mm)
        else:
            if zero_map is None:
                ref = in_maps[0]
                zero_map = {k: np.zeros_like(v) for k, v in ref.items()}
            in_maps.append(zero_map)
    res = run_bass_kernel_spmd(nc, in_maps, core_ids=list(range(8)))
    outs = {b: np.ascontiguousarray(res.results[core]["yT"].T) for core, b in real.items()}
    return np.stack([outs[b] for b in range(B)], axis=0).astype(np.float32)
```
